# Optimizing a Trainium2 kernel written in Bass

```python
import math
import jax, jax.numpy as jnp
from jax import lax
import numpy as np

D_MODEL = 1024
BATCH = 8
SEQ = 4096
DEPTH = 2

N_A_LAYERS = DEPTH // 2
N_B_LAYERS = DEPTH - N_A_LAYERS
PLE_DIM = 256
NORM_EPS = 1e-6
GATED_NORM_EPS = 1e-5
SSM_EXPAND = 2
D_INNER = SSM_EXPAND * D_MODEL
SSM_HEADDIM = 64
SSM_HEADS = D_INNER // SSM_HEADDIM
SSM_GROUPS = 8
SSM_HPG = SSM_HEADS // SSM_GROUPS
SSM_STATE = 128
CONV_K = 4
CONV_DIM = D_INNER + 2 * SSM_GROUPS * SSM_STATE
D_IN_PROJ = D_INNER + CONV_DIM + SSM_HEADS
SSD_CHUNK = 128
HEAD_DIM = 64
N_Q_HEADS = D_MODEL // HEAD_DIM
N_KV_HEADS = 2
Q_PER_KV = N_Q_HEADS // N_KV_HEADS
WINDOW = 128
ROT_DIM = HEAD_DIM // 4
ROPE_THETA = 500000.0
KV_DIM = N_KV_HEADS * HEAD_DIM
PEER_HEADS = 8
N_KEYS = 128
N_EXPERTS = N_KEYS * N_KEYS
PEER_DK = 256
PEER_HALF = PEER_DK // 2
PEER_TOPK = 16
PEER_BLOCK = 128

kernel_name = 'hybrid_ssd_swa_sink_peer_yoco'


def rmsnorm(x, g, eps=NORM_EPS):
    xf = x.astype(jnp.float32)
    xf = xf * lax.rsqrt(jnp.mean(xf * xf, axis=-1, keepdims=True) + eps)
    return (xf * g.astype(jnp.float32)).astype(x.dtype)


def rope_tables(positions):
    inv = jnp.power(ROPE_THETA, -jnp.arange(0, ROT_DIM, 2, dtype=jnp.float32) / ROT_DIM)
    ang = positions.astype(jnp.float32)[..., None] * inv
    return jnp.cos(ang)[:, :, None, :], jnp.sin(ang)[:, :, None, :]


def apply_partial_rope(t, cos, sin):
    half = ROT_DIM // 2
    t1 = t[..., :half].astype(jnp.float32)
    t2 = t[..., half:ROT_DIM].astype(jnp.float32)
    rot = jnp.concatenate([t1 * cos - t2 * sin, t2 * cos + t1 * sin], axis=-1).astype(t.dtype)
    return jnp.concatenate([rot, t[..., ROT_DIM:]], axis=-1)


def causal_conv(u, w, b):
    S = u.shape[1]
    up = jnp.pad(u, ((0, 0), (CONV_K - 1, 0), (0, 0)))
    out = b
    for k in range(CONV_K):
        out = out + up[:, k:k + S] * w[k]
    return out


def ssd_chunked(xs, dt, A, Bm, Cm):
    Bsz, S = xs.shape[0], xs.shape[1]
    nc = S // SSD_CHUNK
    f32 = jnp.float32
    X = (xs.astype(f32) * dt[..., None]).reshape(Bsz, nc, SSD_CHUNK, SSM_GROUPS, SSM_HPG, SSM_HEADDIM)
    a = (dt * A).reshape(Bsz, nc, SSD_CHUNK, SSM_GROUPS, SSM_HPG)
    Bc = Bm.astype(f32).reshape(Bsz, nc, SSD_CHUNK, SSM_GROUPS, SSM_STATE)
    Cc = Cm.astype(f32).reshape(Bsz, nc, SSD_CHUNK, SSM_GROUPS, SSM_STATE)
    a_cs = jnp.cumsum(a, axis=2)
    causal = jnp.tril(jnp.ones((SSD_CHUNK, SSD_CHUNK), dtype=bool))[:, :, None, None]
    seg = a_cs[:, :, :, None] - a_cs[:, :, None, :]
    L = jnp.exp(jnp.where(causal, seg, -jnp.inf))
    cb = jnp.einsum('bclgn,bcsgn->bclsg', Cc, Bc)
    y_diag = jnp.einsum('bclsg,bclsgr,bcsgrp->bclgrp', cb, L, X)
    decay_to_end = jnp.exp(a_cs[:, :, -1:] - a_cs)
    states = jnp.einsum('bclgn,bclgr,bclgrp->bcgrpn', Bc, decay_to_end, X)
    chunk_decay = jnp.exp(a_cs[:, :, -1])

    def step(h, inp):
        st, dc = inp
        return h * dc[..., None, None] + st, h

    h0 = jnp.zeros((Bsz, SSM_GROUPS, SSM_HPG, SSM_HEADDIM, SSM_STATE), f32)
    _, prev = lax.scan(step, h0, (jnp.moveaxis(states, 1, 0), jnp.moveaxis(chunk_decay, 1, 0)))
    prev = jnp.moveaxis(prev, 0, 1)
    y_off = jnp.einsum('bclgn,bcgrpn,bclgr->bclgrp', Cc, prev, jnp.exp(a_cs))
    return (y_diag + y_off).reshape(Bsz, S, SSM_HEADS, SSM_HEADDIM)


def mamba2_mixer(x, norm_g, in_w, conv_w, conv_b, dt_bias, A_log, D_skip, gate_norm, out_w):
    Bsz, S, _ = x.shape
    f32 = jnp.float32
    h = rmsnorm(x, norm_g)
    zxbcdt = h @ in_w
    z = zxbcdt[..., :D_INNER]
    xbc = zxbcdt[..., D_INNER:D_INNER + CONV_DIM]
    dt_raw = zxbcdt[..., D_INNER + CONV_DIM:]
    xbc = jax.nn.silu(causal_conv(xbc, conv_w, conv_b))
    gn = SSM_GROUPS * SSM_STATE
    xs = xbc[..., :D_INNER].reshape(Bsz, S, SSM_HEADS, SSM_HEADDIM)
    Bm = xbc[..., D_INNER:D_INNER + gn].reshape(Bsz, S, SSM_GROUPS, SSM_STATE)
    Cm = xbc[..., D_INNER + gn:].reshape(Bsz, S, SSM_GROUPS, SSM_STATE)
    dt = jax.nn.softplus(dt_raw.astype(f32) + dt_bias.astype(f32))
    A = -jnp.exp(A_log.astype(f32))
    y = ssd_chunked(xs, dt, A, Bm, Cm)
    y = y + xs.astype(f32) * D_skip.astype(f32)[:, None]
    y = y.reshape(Bsz, S, D_INNER) * jax.nn.silu(z.astype(f32))
    y = y.reshape(Bsz, S, SSM_GROUPS, D_INNER // SSM_GROUPS)
    y = y * lax.rsqrt(jnp.mean(y * y, axis=-1, keepdims=True) + GATED_NORM_EPS)
    y = (y.reshape(Bsz, S, D_INNER) * gate_norm.astype(f32)).astype(x.dtype)
    return y @ out_w


def shared_kv(x, norm_g, kv_w, kv_b, cos, sin):
    Bsz, S, _ = x.shape
    kv = rmsnorm(x, norm_g) @ kv_w + kv_b
    k = kv[..., :KV_DIM].reshape(Bsz, S, N_KV_HEADS, HEAD_DIM)
    v = kv[..., KV_DIM:].reshape(Bsz, S, N_KV_HEADS, HEAD_DIM)
    return apply_partial_rope(k, cos, sin), v


def swa_sink_attention(x, norm_g, q_w, q_b, sinks, o_w, o_b, k, v, cos, sin):
    Bsz, S, _ = x.shape
    nb = S // WINDOW
    f32 = jnp.float32
    h = rmsnorm(x, norm_g)
    q = (h @ q_w + q_b).reshape(Bsz, S, N_Q_HEADS, HEAD_DIM)
    q = apply_partial_rope(q, cos, sin)
    qb = q.reshape(Bsz, nb, WINDOW, N_KV_HEADS, Q_PER_KV, HEAD_DIM)

    def band(t):
        tp = jnp.concatenate([jnp.zeros_like(t[:, :WINDOW]), t], axis=1)
        tp = tp.reshape(Bsz, nb + 1, WINDOW, N_KV_HEADS, HEAD_DIM)
        return jnp.concatenate([tp[:, :-1], tp[:, 1:]], axis=2)

    kb, vb = band(k), band(v)
    sink_logits = sinks.astype(f32).reshape(N_KV_HEADS, Q_PER_KV)
    qi = jnp.arange(WINDOW)[:, None]
    kj = jnp.arange(2 * WINDOW)[None, :]
    in_window = (kj > qi) & (kj <= qi + WINDOW)
    scale = HEAD_DIM ** -0.5

    def block(inp):
        qblk, kblk, vblk, n = inp
        s = jnp.einsum('bqgrd,bkgd->bgrqk', qblk, kblk).astype(f32) * scale
        valid = in_window & (n * WINDOW - WINDOW + kj >= 0)
        s = jnp.where(valid, s, -jnp.inf)
        sink = jnp.broadcast_to(sink_logits[None, :, :, None, None], s.shape[:-1] + (1,))
        pr = jax.nn.softmax(jnp.concatenate([s, sink], axis=-1), axis=-1)[..., :-1]
        return jnp.einsum('bgrqk,bkgd->bqgrd', pr.astype(vblk.dtype), vblk)

    o = lax.map(block, (jnp.moveaxis(qb, 1, 0), jnp.moveaxis(kb, 1, 0), jnp.moveaxis(vb, 1, 0),
                        jnp.arange(nb)))
    o = jnp.moveaxis(o, 0, 1).reshape(Bsz, S, N_Q_HEADS * HEAD_DIM)
    return o @ o_w + o_b


def peer_ffn(x, norm_g, q_w, sub_keys, u, v):
    Bsz, S, D = x.shape
    T = Bsz * S
    h = rmsnorm(x, norm_g).reshape(T, D)
    q = (h @ q_w).reshape(T, PEER_HEADS, 2, PEER_HALF)
    s = jnp.einsum('thcd,ckd->thck', q, sub_keys).astype(jnp.float32)
    s1, i1 = lax.top_k(s[:, :, 0], PEER_TOPK)
    s2, i2 = lax.top_k(s[:, :, 1], PEER_TOPK)
    cand_s = (s1[..., :, None] + s2[..., None, :]).reshape(T, PEER_HEADS, PEER_TOPK * PEER_TOPK)
    cand_i = (i1[..., :, None] * N_KEYS + i2[..., None, :]).reshape(T, PEER_HEADS, PEER_TOPK * PEER_TOPK)
    best_s, best_pos = lax.top_k(cand_s, PEER_TOPK)
    idx = jnp.take_along_axis(cand_i, best_pos, axis=-1)
    gate = jax.nn.softmax(best_s, axis=-1).astype(x.dtype)
    nblk = T // PEER_BLOCK

    def block(inp):
        hb, ib, gb = inp
        a = jnp.einsum('nd,nhkd->nhk', hb, u[ib])
        w = gb * jax.nn.gelu(a, approximate=False)
        return jnp.einsum('nhk,nhkd->nd', w, v[ib])

    out = lax.map(block, (h.reshape(nblk, PEER_BLOCK, D),
                          idx.reshape(nblk, PEER_BLOCK, PEER_HEADS, PEER_TOPK),
                          gate.reshape(nblk, PEER_BLOCK, PEER_HEADS, PEER_TOPK)))
    return out.reshape(Bsz, S, D)


def ple_add(x, p_i, norm_g, proj, gate_w):
    gate = jax.nn.sigmoid(rmsnorm(x, norm_g) @ gate_w)
    return x + (p_i @ proj) * gate


def setup_inputs(seed: int = 0) -> dict:
    key = jax.random.key(seed)
    ks = jax.random.split(key, 32)
    f32 = jnp.float32

    def nrm(k, shape, scale):
        return jax.random.normal(k, shape, f32) * scale

    def gain(k, shape):
        return 1.0 + 0.05 * jax.random.normal(k, shape, f32)

    dt0 = jnp.exp(jax.random.uniform(ks[6], (N_A_LAYERS, SSM_HEADS), f32,
                                     minval=math.log(1e-3), maxval=math.log(1e-1)))
    inp = {
        'x': nrm(ks[0], (BATCH, SEQ, D_MODEL), 1.0),
        'p': nrm(ks[1], (DEPTH, BATCH, SEQ, PLE_DIM), 1.0),
        'positions': jnp.arange(SEQ, dtype=jnp.int32)[None, :]
                     + jax.random.randint(ks[2], (BATCH, 1), 0, 4096, dtype=jnp.int32),
        'ssm_norm': gain(ks[3], (N_A_LAYERS, D_MODEL)),
        'ssm_in_w': nrm(ks[4], (N_A_LAYERS, D_MODEL, D_IN_PROJ), D_MODEL ** -0.5),
        'ssm_conv_w': nrm(ks[5], (N_A_LAYERS, CONV_K, CONV_DIM), CONV_K ** -0.5),
        'ssm_conv_b': nrm(ks[7], (N_A_LAYERS, CONV_DIM), 0.02),
        'ssm_dt_bias': dt0 + jnp.log(-jnp.expm1(-dt0)),
        'ssm_A_log': jnp.log(jax.random.uniform(ks[8], (N_A_LAYERS, SSM_HEADS), f32, minval=1.0, maxval=16.0)),
        'ssm_D': gain(ks[9], (N_A_LAYERS, SSM_HEADS)),
        'ssm_gate_norm': gain(ks[10], (N_A_LAYERS, D_INNER)),
        'ssm_out_w': nrm(ks[11], (N_A_LAYERS, D_INNER, D_MODEL), D_INNER ** -0.5),
        'kv_norm': gain(ks[12], (D_MODEL,)),
        'kv_w': nrm(ks[13], (D_MODEL, 2 * KV_DIM), D_MODEL ** -0.5),
        'kv_b': nrm(ks[14], (2 * KV_DIM,), 0.02),
        'attn_norm': gain(ks[15], (N_B_LAYERS, D_MODEL)),
        'q_w': nrm(ks[16], (N_B_LAYERS, D_MODEL, N_Q_HEADS * HEAD_DIM), D_MODEL ** -0.5),
        'q_b': nrm(ks[17], (N_B_LAYERS, N_Q_HEADS * HEAD_DIM), 0.02),
        'sinks': nrm(ks[18], (N_B_LAYERS, N_Q_HEADS), 0.5),
        'o_w': nrm(ks[19], (N_B_LAYERS, N_Q_HEADS * HEAD_DIM, D_MODEL), (N_Q_HEADS * HEAD_DIM) ** -0.5),
        'o_b': nrm(ks[20], (N_B_LAYERS, D_MODEL), 0.02),
        'peer_norm': gain(ks[21], (DEPTH, D_MODEL)),
        'peer_q_w': nrm(ks[22], (DEPTH, D_MODEL, PEER_HEADS * PEER_DK), D_MODEL ** -0.5),
        'peer_sub_keys': nrm(ks[23], (DEPTH, 2, N_KEYS, PEER_HALF), PEER_HALF ** -0.5),
        'peer_u': nrm(ks[24], (DEPTH, N_EXPERTS, D_MODEL), D_MODEL ** -0.5),
        'peer_v': nrm(ks[25], (DEPTH, N_EXPERTS, D_MODEL), PEER_HEADS ** -0.5),
        'ple_norm': gain(ks[26], (DEPTH, D_MODEL)),
        'ple_proj': nrm(ks[27], (DEPTH, PLE_DIM, D_MODEL), PLE_DIM ** -0.5),
        'ple_gate_w': nrm(ks[28], (DEPTH, D_MODEL, D_MODEL), D_MODEL ** -0.5),
        'final_norm': gain(ks[29], (D_MODEL,)),
    }
    return inp


def reference(x, p, positions, ssm_norm, ssm_in_w, ssm_conv_w, ssm_conv_b, ssm_dt_bias, ssm_A_log,
              ssm_D, ssm_gate_norm, ssm_out_w, kv_norm, kv_w, kv_b, attn_norm, q_w, q_b, sinks,
              o_w, o_b, peer_norm, peer_q_w, peer_sub_keys, peer_u, peer_v, ple_norm, ple_proj,
              ple_gate_w, final_norm):
    cos, sin = rope_tables(positions)
    k_shared, v_shared = None, None
    for i in range(DEPTH):
        if i < N_A_LAYERS:
            j = i
            x = x + mamba2_mixer(x, ssm_norm[j], ssm_in_w[j], ssm_conv_w[j], ssm_conv_b[j],
                                 ssm_dt_bias[j], ssm_A_log[j], ssm_D[j], ssm_gate_norm[j], ssm_out_w[j])
        else:
            j = i - N_A_LAYERS
            x = x + swa_sink_attention(x, attn_norm[j], q_w[j], q_b[j], sinks[j], o_w[j], o_b[j],
                                       k_shared, v_shared, cos, sin)
        x = x + peer_ffn(x, peer_norm[i], peer_q_w[i], peer_sub_keys[i], peer_u[i], peer_v[i])
        x = ple_add(x, p[i], ple_norm[i], ple_proj[i], ple_gate_w[i])
        if i == N_A_LAYERS - 1:
            k_shared, v_shared = shared_kv(x, kv_norm, kv_w, kv_b, cos, sin)
    return rmsnorm(x, final_norm)
```

```python
import numpy as np
from contextlib import ExitStack
import concourse.bass as bass
import concourse.mybir as mybir
from concourse.bass_utils import run_bass_kernel_spmd

F32 = mybir.dt.float32
BF16 = mybir.dt.bfloat16
I32 = mybir.dt.int32
U32 = mybir.dt.uint32
AF = mybir.ActivationFunctionType
ALU = mybir.AluOpType
AX = mybir.AxisListType

SYNC_SAME_ENGINE = True
OPT_BF16_ONEHOT = False
OPT_NOSYNC = True
OPT_PUMP = 1
OPT_POOLMULT = 0
PAD1A = 3
DEBUG = False
S = 4096
NCH = 32


class Buf:
    def __init__(self, t, name, parent=None):
        self.t = t
        self.name = name
        self.parent = parent
        self._lw = None
        self._rd = []

    @property
    def lw(self):
        return self.parent.lw if self.parent is not None else self._lw

    @lw.setter
    def lw(self, v):
        if self.parent is not None:
            self.parent.lw = v
        else:
            self._lw = v

    @property
    def rd(self):
        return self.parent.rd if self.parent is not None else self._rd

    @rd.setter
    def rd(self, v):
        if self.parent is not None:
            self.parent.rd = v
        else:
            self._rd = v

    def __getitem__(self, idx):
        return self.t[idx]


class KB:
    ENG = ('pe', 'act', 'dve', 'pool', 'sp')

    def __init__(self, nc, n_dma_sems=16):
        self.nc = nc
        self.es = ExitStack()
        self.ops = {e: [] for e in self.ENG}
        self.cnt = {e: 0 for e in self.ENG}
        self.sem = {}
        self.ekey = {}
        self.epoch = 0
        for e in ('pe', 'act', 'dve', 'pool'):
            self.ekey[e] = e + '@0'
            self.sem[self.ekey[e]] = self.es.enter_context(nc.semaphore('s_' + e + '0'))
        self.ekey['sp'] = 'sp@0'
        self.dma_sems = []
        self.dma_pool = {'hw': [], 'sw': []}
        self.dma_pool['cv'] = []
        for i in range(n_dma_sems + 8 + 4):
            s = self.es.enter_context(nc.semaphore(f'd{i}'))
            self.sem[f'd{i}'] = s
            ent = [f'd{i}', 0]
            pk = 'hw' if i < n_dma_sems else ('sw' if i < n_dma_sems + 8 else 'cv')
            if pk != 'cv':
                self.dma_sems.append(ent)
            self.dma_pool[pk].append(ent)
        self.dma_rr = {'hw': 0, 'sw': 0, 'cv': 0}
        self.waited = {e: {} for e in self.ENG}
        self.final_tokens = []
        self.banks = []
        self.bank_rr = 0
        self.scope = None
        self.pending = {e: [] for e in self.ENG}

    def new_epoch(self):
        self.epoch += 1
        for e in ('pe', 'act', 'dve', 'pool'):
            self.ekey[e] = f'{e}@{self.epoch}'
            self.sem[self.ekey[e]] = self.es.enter_context(self.nc.semaphore(f's_{e}{self.epoch}'))
            self.cnt[e] = 0

    def barrier(self):
        cur = {self.ekey[e]: self.cnt[e] for e in ('pe', 'act', 'dve', 'pool')}
        for k, v in self.dma_sems:
            cur[k] = v
        for e in self.ENG:
            for k, v in cur.items():
                if v > 0 and self.waited[e].get(k, 0) < v:
                    self.waited[e][k] = v
                    self.pending[e].append((k, v))

    def sb(self, name, shape, dtype):
        es = self.scope if self.scope is not None else self.es
        self.uid = getattr(self, 'uid', 0) + 1
        name = f'{name}_u{self.uid}'
        t = es.enter_context(self.nc.sbuf_tensor(name, list(shape), dtype))
        return Buf(t, name)

    def ps(self, name, shape, dtype):
        t = self.es.enter_context(self.nc.psum_tensor(name, list(shape), dtype))
        return Buf(t, name)

    def dram(self, name, shape, dtype, kind):
        t = self.nc.dram_tensor(name, list(shape), dtype, kind=kind)
        return Buf(t.ap(), name)

    def scratch(self, name, shape, dtype):
        return self.dram(name, shape, dtype, "ExternalOutput" if DEBUG else "Internal")

    def dbg(self, name, buf, shape, dtype):
        if not DEBUG:
            return
        d = self.dram('dbg_' + name, shape, dtype, "ExternalOutput")
        self.dma('sp', d[:], buf[:], reads=[buf], writes=[d])

    def bank(self, pool=None):
        bs = self.banks if pool is None else pool
        b = bs[self.bank_rr % len(bs)]
        self.bank_rr += 1
        return b

    def _deps(self, eng, reads, writes, nosync=False, after=()):
        toks = {}

        def add(tok, force=False):
            if tok is None:
                return
            k, v = tok
            if k == self.ekey[eng] and not force and (nosync or not (SYNC_SAME_ENGINE and eng in ('act', 'dve', 'pool'))):
                return
            if toks.get(k, 0) < v:
                toks[k] = v
        for b in reads:
            add(b.lw)
        for b in writes:
            add(b.lw)
            for r in b.rd:
                add(r)
        for t in after:
            add(t, True)
        w = self.waited[eng]
        out = []
        for k, v in toks.items():
            if w.get(k, 0) < v:
                w[k] = v
                out.append((k, v))
        return out

    def _commit(self, tok, reads, writes):
        for b in reads:
            b.rd.append(tok)
            if len(b.rd) > 32:
                m = {}
                for k, v in b.rd:
                    m[k] = max(m.get(k, 0), v)
                b.rd = list(m.items())
        for b in writes:
            b.lw = tok
            b.rd = []

    def op(self, eng, fn, reads=(), writes=(), nosync=False, after=()):
        waits = self.pending[eng] + self._deps(eng, reads, writes, nosync and OPT_NOSYNC, after)
        self.pending[eng] = []
        self.cnt[eng] += 1
        tok = (self.ekey[eng], self.cnt[eng])
        self.ops[eng].append((waits, fn, (self.ekey[eng], 1)))
        self._commit(tok, reads, writes)
        return tok

    def dma(self, q, out_ap, in_ap, reads=(), writes=(), **kw):
        pk = kw.pop('sempool', None) or ('sw' if q == 'pool' else 'hw')
        pl = self.dma_pool[pk]
        ent = pl[self.dma_rr[pk] % len(pl)]
        self.dma_rr[pk] += 1
        key = ent[0]
        waits = self.pending[q] + self._deps(q, reads, writes)
        self.pending[q] = []
        if ent[1] > 0 and self.waited[q].get(key, 0) < ent[1]:
            self.waited[q][key] = ent[1]
            waits.append((key, ent[1]))
        ent[1] += 16
        tok = (key, ent[1])

        def fn(e, out_ap=out_ap, in_ap=in_ap, kw=kw):
            return e.dma_start(out=out_ap, in_=in_ap, **kw)
        self.ops[q].append((waits, fn, (key, 16)))
        self._commit(tok, reads, writes)
        return tok

    def finish(self, tokens):
        self.final_tokens = list(tokens)

    def emit(self):
        nc = self.nc
        with nc.Block() as block:
            def body(ename):
                def _f(e):
                    for waits, fn, (sk, inc) in self.ops[ename]:
                        for k, v in waits:
                            e.wait_ge(self.sem[k], v)
                        ins = fn(e)
                        ins.then_inc(self.sem[sk], inc)
                    if ename == 'sp':
                        for k, v in self.final_tokens:
                            e.wait_ge(self.sem[k], v)
                return _f
            block.tensor(body('pe'))
            block.scalar(body('act'))
            block.vector(body('dve'))
            block.gpsimd(body('pool'))
            block.sync(body('sp'))
        self.es.close()


def make_consts():
    k = np.arange(128)
    c = {}
    c['c_ident'] = np.eye(128, dtype=np.float32)
    c['c_triu'] = (k[:, None] <= k[None, :]).astype(np.float32)
    c['c_gt'] = (k[:, None] > k[None, :]).astype(np.float32)
    c['c_ones'] = np.ones((128, 128), np.float32)
    c['c_iota'] = np.broadcast_to(k[None, :].astype(np.float32), (128, 128)).copy()
    return c


def load_consts(kb, names=('c_ident', 'c_triu', 'c_gt', 'c_ones', 'c_iota')):
    C = {}
    for n in names:
        d = kb.dram(n, [128, 128], F32, "ExternalInput")
        sb = kb.sb(n + '_bf', [128, 128], BF16)
        kb.dma('pool', sb[:], d[:], reads=[d], writes=[sb])
        C[n] = sb
        sf = kb.sb(n + '_f', [128, 128], F32)
        kb.dma('sp', sf[:], d[:], reads=[d], writes=[sf])
        C[n + '_f'] = sf
    return C


def bcast_load(kb, name, dram_ap_1xn, n, dbuf):
    sb = kb.sb(name, [128, n], F32)
    kb.dma('sp', sb[:], dram_ap_1xn.partition_broadcast(128), reads=[dbuf], writes=[sb])
    return sb


def rmsnorm_bf(kb, x, gbc, hb, D, eps, junk, ssq, rstd, eng2='dve'):
    kb.op('act', lambda e: e.activation(out=junk[:, 0:D], in_=x[:, 0:D], func=AF.Square, accum_out=ssq[:, 0:1]), [x], [junk, ssq])
    kb.op('dve', lambda e: e.tensor_scalar(out=rstd[:, 0:1], in0=ssq[:, 0:1], scalar1=1.0 / D, scalar2=eps, op0=ALU.mult, op1=ALU.add), [ssq], [rstd])
    kb.op('act', lambda e: e.activation(out=rstd[:, 0:1], in_=rstd[:, 0:1], func=AF.Ln), [rstd], [rstd])
    kb.op('act', lambda e: e.activation(out=rstd[:, 0:1], in_=rstd[:, 0:1], func=AF.Exp, scale=-0.5), [rstd], [rstd])
    kb.op(eng2, lambda e: e.scalar_tensor_tensor(out=hb[:, 0:D], in0=x[:, 0:D], scalar=rstd[:, 0:1], in1=gbc[:, 0:D], op0=ALU.mult, op1=ALU.mult), [x, rstd, gbc], [hb])


def transpose_to(kb, src, ncols, dst_ap, dst, ident, ptr, evac='act'):
    n = ncols // 128
    for c in range(n):
        kb.op('pe', lambda e, c=c: e.transpose(out=ptr[:, c * 128:(c + 1) * 128], in_=src[:, c * 128:(c + 1) * 128], identity=ident[:]), [src, ident], [ptr])
    pv = ptr[:, 0:ncols].rearrange('p (c t) -> p c t', t=128)
    if evac == 'act':
        kb.op('act', lambda e: e.copy(out=dst_ap, in_=pv), [ptr], [dst])
    else:
        kb.op(evac, lambda e: e.tensor_copy(out=dst_ap, in_=pv), [ptr], [dst])


def phase1(kb, C, x_d, x1_d, W, after_loads=None):
    nc = kb.nc
    XB = kb.scratch('sc_XB', [S, 3072], BF16)
    BTs = kb.scratch('sc_BT', [NCH, 128, 8, 128], BF16)
    CTs = kb.scratch('sc_CT', [NCH, 128, 8, 128], BF16)
    ZS = kb.scratch('sc_ZS', [S, 2048], BF16)
    DT = kb.scratch('sc_DT', [S, 32], F32)
    ident = C['c_ident']
    ptr = kb.ptr
    with ExitStack() as sc:
        kb.scope = sc
        inw = kb.sb('inw', [128, 8, 6176], BF16)
        padbank = kb.banks[5]
        for dc in range(8):
            kb.dma('pool', inw[:, dc, :], W['in_w'][dc * 128:(dc + 1) * 128, :], reads=[W['in_w']], writes=[inw])
        gbc = bcast_load(kb, 'gbc1', W['ssm_norm'][0:1, :], 1024, W['ssm_norm'])
        dtb = bcast_load(kb, 'dtb', W['dt_bias'][0:1, :], 32, W['dt_bias'])
        cw = kb.sb('cw', [128, 32, 4], F32)
        kb.dma('sp', cw[:], W['conv_wT'].t.rearrange('(f p) k -> p f k', p=128), reads=[W['conv_wT']], writes=[cw])
        cbias = kb.sb('cbias', [128, 32], F32)
        kb.dma('sp', cbias[:], W['conv_b2'][:, :], reads=[W['conv_b2']], writes=[cbias])
        Uall = kb.sb('Uall', [128, 32, 516], BF16)
        kb.op('dve', lambda e: e.memset(Uall[:], 0.0), [], [Uall])
        Us = [Buf(Uall.t[:, f, :], f'U{f}') for f in range(32)]
        for u_ in Us:
            u_.lw = Uall.lw
        xt = [kb.sb(f'xt{i}', [128, 1024], F32) for i in range(2)]
        junk = kb.sb('junk', [128, 1024], F32)
        ssq = kb.sb('ssq', [128, 1], F32)
        rstd = kb.sb('rstd', [128, 1], F32)
        hb = [kb.sb(f'hb{i}', [128, 1024], BF16) for i in range(2)]
        hT = kb.sb('hT', [128, 8, 512], BF16)
        acc = [kb.sb(f'acc{i}', [128, 512], F32) for i in range(3)]
        xbcf = [kb.sb(f'xbcf{i}', [128, 512], BF16) for i in range(5)]
        XBo = kb.sb('XBo', [128, 4, 3072], BF16)
        zo = [kb.sb(f'zo{i}', [128, 2048], BF16) for i in range(2)]
        dtt = [kb.sb(f'dtt{i}', [128, 32], F32) for i in range(2)]
        if after_loads is not None:
            after_loads()
        for st in range(8):
            for j in range(4):
                xx = xt[j % 2]
                r0 = st * 512 + j * 128
                kb.dma('sp', xx[:], x_d[r0:r0 + 128, :], reads=[x_d], writes=[xx])
                h = hb[j % 2]
                rmsnorm_bf(kb, xx, gbc, h, 1024, 1e-6, junk, ssq, rstd)
                transpose_to(kb, h, 1024, hT[:, :, j * 128:(j + 1) * 128], hT, ident, ptr[j % 2])
            for f in range(32):
                pb = kb.bank(kb.banks[0:5])
                for dc in range(8):
                    kb.op('pe', lambda e, pb=pb, dc=dc, f=f: e.matmul(pb[:, 0:512], lhsT=inw[:, dc, 2048 + f * 128:2048 + (f + 1) * 128], rhs=hT[:, dc, :], start=(dc == 0), stop=(dc == 7)), [inw, hT], [pb])
                for dd in range(PAD1A):
                    kb.op('pe', lambda e, dd=dd, f=f: e.matmul(padbank[:, 0:512], lhsT=inw[:, dd, 2048 + f * 128:2048 + (f + 1) * 128], rhs=hT[:, dd, :], start=True, stop=True), [inw, hT], [padbank])
                U = Us[f]
                kb.op('act', lambda e, U=U, pb=pb: e.copy(out=U[:, 3:515], in_=pb[:, 0:512]), [pb], [U])
                a = acc[f % 3]
                kb.op('act', lambda e, U=U, a=a, f=f: e.activation(out=a[:], in_=U[:, 0:512], func=AF.Identity, scale=cw[:, f, 0:1], bias=cbias[:, f:f + 1]), [U, cw, cbias], [a])
                for k in range(1, 4):
                    kb.op('dve', lambda e, U=U, a=a, f=f, k=k: e.scalar_tensor_tensor(out=a[:], in0=U[:, k:k + 512], scalar=cw[:, f, k:k + 1], in1=a[:], op0=ALU.mult, op1=ALU.add), [U, cw, a], [a])
                kb.op('dve', lambda e, U=U: e.tensor_copy(out=U[:, 0:3], in_=U[:, 512:515]), [U], [U])

                def _silu(ff, st=st):
                    a2 = acc[ff % 3]
                    xo = xbcf[ff % 5]
                    kb.op('act', lambda e, xo=xo, a2=a2: e.activation(out=xo[:], in_=a2[:], func=AF.Silu), [a2], [xo])
                    if ff >= 16:
                        dst = BTs if ff < 24 else CTs
                        g = (ff - 16) % 8
                        kb.dma('sp', dst.t[st * 4:(st + 1) * 4, :, g, :].rearrange('c n t -> n c t'), xo[:].rearrange('p (c t) -> p c t', t=128), reads=[xo], writes=[dst])

                def _tr(ff):
                    xo2 = xbcf[ff % 5]
                    pt = ptr[ff % 2]
                    for j in range(4):
                        kb.op('pe', lambda e, pt=pt, xo2=xo2, j=j: e.transpose(out=pt[:, j * 128:(j + 1) * 128], in_=xo2[:, j * 128:(j + 1) * 128], identity=ident[:]), [xo2, ident], [pt])
                    kb.op('act', lambda e, pt=pt, ff=ff: e.copy(out=XBo[:, :, ff * 128:(ff + 1) * 128], in_=pt[:, 0:512].rearrange('p (j c) -> p j c', c=128)), [pt], [XBo])
                if f >= 1:
                    _silu(f - 1)
                if 3 <= f < 27:
                    _tr(f - 3)
            _silu(31)
            kb.dma('sp', XB.t[st * 512:(st + 1) * 512, :].rearrange('(j p) c -> p j c', p=128), XBo[:], reads=[XBo], writes=[XB])
            for j in range(4):
                z = zo[j % 2]
                for q in range(4):
                    pb = kb.bank(kb.banks[0:5])
                    for dc in range(8):
                        kb.op('pe', lambda e, pb=pb, dc=dc, q=q, j=j: e.matmul(pb[:, 0:512], lhsT=hT[:, dc, j * 128:(j + 1) * 128], rhs=inw[:, dc, q * 512:(q + 1) * 512], start=(dc == 0), stop=(dc == 7)), [inw, hT], [pb])
                    kb.op('act', lambda e, pb=pb, z=z, q=q: e.activation(out=z[:, q * 512:(q + 1) * 512], in_=pb[:, 0:512], func=AF.Silu), [pb], [z])
                r0 = st * 512 + j * 128
                kb.dma('sp', ZS[r0:r0 + 128, :], z[:], reads=[z], writes=[ZS])
                pb = kb.bank(kb.banks[0:5])
                for dc in range(8):
                    kb.op('pe', lambda e, pb=pb, dc=dc, j=j: e.matmul(pb[:, 0:32], lhsT=hT[:, dc, j * 128:(j + 1) * 128], rhs=inw[:, dc, 6144:6176], start=(dc == 0), stop=(dc == 7)), [inw, hT], [pb])
                d = dtt[j % 2]
                kb.op('dve', lambda e, pb=pb, d=d: e.tensor_tensor(out=d[:], in0=pb[:, 0:32], in1=dtb[:], op=ALU.add), [pb, dtb], [d])
                kb.op('act', lambda e, d=d: e.activation(out=d[:], in_=d[:], func=AF.Exp), [d], [d])
                kb.op('act', lambda e, d=d: e.activation(out=d[:], in_=d[:], func=AF.Ln, bias=1.0), [d], [d])
                kb.dma('sp', DT[r0:r0 + 128, :], d[:], reads=[d], writes=[DT])
    kb.barrier()
    kb.new_epoch()
    with ExitStack() as sc:
        kb.scope = sc
        outw = kb.sb('outw', [128, 16, 1024], BF16)
        for ic in range(16):
            kb.dma('pool', outw[:, ic, :], W['out_w'][ic * 128:(ic + 1) * 128, :], reads=[W['out_w']], writes=[outw])
        gnb = bcast_load(kb, 'gnb', W['gate_norm'][0:1, :], 2048, W['gate_norm'])
        Abc = bcast_load(kb, 'Abc', W['A_log'][0:1, :], 32, W['A_log'])
        Dbc = bcast_load(kb, 'Dbc', W['D'][0:1, :], 32, W['D'])
        kb.op('act', lambda e: e.activation(out=Abc[:], in_=Abc[:], func=AF.Exp), [Abc], [Abc])
        kb.op('dve', lambda e: e.tensor_scalar(out=Abc[:], in0=Abc[:], scalar1=-1.0, scalar2=None, op0=ALU.mult), [Abc], [Abc])
        triu, gt, ones = C['c_triu'], C['c_gt'], C['c_ones']
        triu_f, ones_f = C['c_triu_f'], C['c_ones_f']
        xbt = [kb.sb(f'xbt{i}', [128, 3072], BF16) for i in range(2)]
        bt = [kb.sb(f'bt{i}', [128, 8, 128], BF16) for i in range(2)]
        ct = [kb.sb(f'ct{i}', [128, 8, 128], BF16) for i in range(2)]
        dtl = [kb.sb(f'dtl{i}', [128, 32], F32) for i in range(2)]
        zl = [kb.sb(f'zl{i}', [128, 2048], BF16) for i in range(2)]
        xl = [kb.sb(f'xl{i}', [128, 1024], F32) for i in range(2)]
        a_t = kb.sb('a_t', [128, 32], F32)
        acs = kb.sb('acs', [128, 32], F32)
        dte = kb.sb('dte', [128, 32], F32)
        cd = kb.sb('cd', [128, 32], F32)
        Xdt = kb.sb('Xdt', [128, 32, 64], BF16)
        Xd = kb.sb('Xd', [128, 32, 64], BF16)
        rhs_all = kb.sb('rhs_all', [128, 32, 128], BF16)
        cbTm = kb.sb('cbTm', [128, 8, 128], F32)
        LT = [kb.sb(f'LT{i}', [128, 4, 128], F32) for i in range(2)]
        decT = [kb.sb(f'decT{i}', [128, 4, 128], BF16) for i in range(2)]
        MT = [kb.sb(f'MT{i}', [128, 4, 128], BF16) for i in range(2)]
        CsT = [kb.sb(f'CsT{i}', [128, 4, 128], BF16) for i in range(2)]
        prevT = kb.sb('prevT', [128, 32, 64], F32)
        prevB = kb.sb('prevB', [128, 32, 64], BF16)
        kb.op('dve', lambda e: e.memset(prevT[:], 0.0), [], [prevT])
        kb.op('dve', lambda e: e.memset(prevB[:], 0.0), [], [prevB])
        t1s = [kb.sb(f't1_{i}', [128, 2048], F32) for i in range(2)]
        y2 = kb.sb('y2', [128, 2048], F32)
        sq = kb.sb('sq', [128, 2048], F32)
        ssq8 = kb.sb('ssq8', [128, 8], F32)
        y3 = kb.sb('y3', [128, 2048], BF16)
        ynT = kb.sb('ynT', [128, 16, 128], BF16)
        xo = [kb.sb(f'xo{i}', [128, 1024], F32) for i in range(2)]
        def genA(c):
            r0 = c * 128
            xb_, b_, c_, d_, z_, x_ = xbt[c % 2], bt[c % 2], ct[c % 2], dtl[c % 2], zl[c % 2], xl[c % 2]
            kb.dma('sp', xb_[:], XB[r0:r0 + 128, :], reads=[XB], writes=[xb_])
            kb.dma('sp', b_[:], BTs[c], reads=[BTs], writes=[b_])
            kb.dma('sp', c_[:], CTs[c], reads=[CTs], writes=[c_])
            kb.dma('sp', d_[:], DT[r0:r0 + 128, :], reads=[DT], writes=[d_])
            kb.dma('sp', z_[:], ZS[r0:r0 + 128, :], reads=[ZS], writes=[z_])
            kb.dma('sp', x_[:], x_d[r0:r0 + 128, :], reads=[x_d], writes=[x_])
            yield
            kb.op('dve', lambda e, d_=d_: e.tensor_tensor(out=a_t[:], in0=d_[:], in1=Abc[:], op=ALU.mult), [d_, Abc], [a_t])
            pb = kb.bank(kb.banks[4:6])
            kb.op('pe', lambda e, pb=pb: e.matmul(pb[:, 0:32], lhsT=triu_f[:], rhs=a_t[:], start=True, stop=True), [triu_f, a_t], [pb])
            kb.op('pe', lambda e, pb=pb: e.matmul(pb[:, 32:64], lhsT=ones_f[:], rhs=a_t[:], start=True, stop=True), [ones_f, a_t], [pb])
            kb.op('act', lambda e, pb=pb: e.copy(out=acs[:], in_=pb[:, 0:32]), [pb], [acs])
            kb.op('dve', lambda e, pb=pb: e.tensor_tensor(out=dte[:], in0=pb[:, 32:64], in1=acs[:], op=ALU.subtract), [pb, acs], [dte])
            kb.op('act', lambda e: e.activation(out=dte[:], in_=dte[:], func=AF.Exp), [dte], [dte])
            kb.op('act', lambda e, pb=pb: e.activation(out=cd[:], in_=pb[:, 32:64], func=AF.Exp), [pb], [cd])
            yield
            xs3 = xb_[:, 0:2048].rearrange('p (h d) -> p h d', d=64)
            kb.op('dve', lambda e, xs3=xs3, d_=d_: e.tensor_tensor(out=Xdt[:], in0=xs3, in1=d_[:].unsqueeze(2).to_broadcast([128, 32, 64]), op=ALU.mult), [xb_, d_], [Xdt])
            kb.op('dve', lambda e: e.tensor_tensor(out=Xd[:], in0=Xdt[:], in1=dte[:].unsqueeze(2).to_broadcast([128, 32, 64]), op=ALU.mult), [Xdt, dte], [Xd])
            yield
            for h_ in range(32):
                if h_ % 8 == 7:
                    yield
                kb.op('act', lambda e, h_=h_: e.activation(out=rhs_all[:, h_, :], in_=triu_f[:], func=AF.Copy, scale=a_t[:, h_:h_ + 1]), [a_t, triu_f], [rhs_all], nosync=(h_ > 0))
            for half in range(2):
                pb = kb.bank(kb.banks[4:6])
                for gg in range(4):
                    g = half * 4 + gg
                    kb.op('pe', lambda e, pb=pb, g=g, gg=gg, b_=b_, c_=c_: e.matmul(pb[:, gg * 128:(gg + 1) * 128], lhsT=b_[:, g, :], rhs=c_[:, g, :], start=True, stop=True), [b_, c_], [pb])
                kb.op('dve', lambda e, pb=pb, half=half: e.tensor_tensor(out=cbTm[:, half * 4:(half + 1) * 4, :], in0=pb[:, 0:512].rearrange('p (g l) -> p g l', l=128), in1=triu_f[:].unsqueeze(1).to_broadcast([128, 4, 128]), op=ALU.mult), [pb, triu_f], [cbTm])
            yield
            ybanks = kb.banks[0:4]
            sdb = [kb.banks[4], kb.banks[5], kb.xbanks[0], kb.xbanks[1]]

            def _segdec(hg):
                rh = rhs_all[:, hg * 4:(hg + 1) * 4, :].rearrange('p h l -> p (h l)')
                pseg = sdb[(hg % 2) * 2]
                pdec = sdb[(hg % 2) * 2 + 1]
                kb.op('pe', lambda e, pseg=pseg, rh=rh: e.matmul(pseg[:, 0:512], lhsT=gt[:], rhs=rh, start=True, stop=True), [gt, rhs_all], [pseg])
                kb.op('pe', lambda e, pdec=pdec, rh=rh: e.matmul(pdec[:, 0:512], lhsT=ones[:], rhs=rh, start=True, stop=True), [ones, rhs_all], [pdec])
                lt, dc_, mt, cs = LT[hg % 2], decT[hg % 2], MT[hg % 2], CsT[hg % 2]
                g = hg
                kb.op('act', lambda e, pseg=pseg, lt=lt: e.activation(out=lt[:].rearrange('p h l -> p (h l)'), in_=pseg[:, 0:512], func=AF.Exp), [pseg], [lt])
                kb.op('act', lambda e, pdec=pdec, dc_=dc_: e.activation(out=dc_[:].rearrange('p h l -> p (h l)'), in_=pdec[:, 0:512], func=AF.Exp), [pdec], [dc_])
                kb.op('dve', lambda e, lt=lt, mt=mt, g=g: e.tensor_tensor(out=mt[:], in0=lt[:], in1=cbTm[:, g:g + 1, :].to_broadcast([128, 4, 128]), op=ALU.mult), [lt, cbTm], [mt])
                kb.op('dve', lambda e, dc_=dc_, cs=cs, g=g, c_=c_: e.tensor_tensor(out=cs[:], in0=dc_[:], in1=c_[:, g:g + 1, :].to_broadcast([128, 4, 128]), op=ALU.mult), [dc_, c_], [cs])
            _segdec(0)
            for hg in range(8):
                if hg + 1 < 8:
                    _segdec(hg + 1)
                mt, cs = MT[hg % 2], CsT[hg % 2]
                for hh in range(4):
                    h = hg * 4 + hh
                    yb = ybanks[h // 8]
                    col = (h % 8) * 64
                    kb.op('pe', lambda e, yb=yb, col=col, mt=mt, hh=hh, h=h: e.matmul(yb[:, col:col + 64], lhsT=mt[:, hh, :], rhs=Xdt[:, h, :], start=True, stop=False), [mt, Xdt], [yb])
                    kb.op('pe', lambda e, yb=yb, col=col, cs=cs, hh=hh, h=h: e.matmul(yb[:, col:col + 64], lhsT=cs[:, hh, :], rhs=prevB[:, h, :], start=False, stop=True), [cs, prevB], [yb])
                yield
            t1 = t1s[c % 2]
            kb.op('dve', lambda e, xs3=xs3: e.tensor_tensor(out=t1[:].rearrange('p (h d) -> p h d', d=64), in0=xs3, in1=Dbc[:].unsqueeze(2).to_broadcast([128, 32, 64]), op=ALU.mult), [xb_, Dbc], [t1])
            for q in range(4):
                kb.op('dve', lambda e, q=q, yb=ybanks[q]: e.tensor_tensor(out=t1[:, q * 512:(q + 1) * 512], in0=yb[:, 0:512], in1=t1[:, q * 512:(q + 1) * 512], op=ALU.add), [ybanks[q], t1], [t1])
            yield
            sbanks = kb.banks[0:4]
            for h in range(32):
                g = h // 4
                sbk = sbanks[h // 8]
                col = (h % 8) * 64
                kb.op('pe', lambda e, sbk=sbk, col=col, g=g, h=h, xb_=xb_: e.matmul(sbk[:, col:col + 64], lhsT=xb_[:, 2048 + g * 128:2048 + (g + 1) * 128], rhs=Xd[:, h, :], start=True, stop=True), [xb_, Xd], [sbk])
            yield
            kb.op('dve', lambda e: e.tensor_tensor(out=prevT[:], in0=prevT[:], in1=cd[:].unsqueeze(2).to_broadcast([128, 32, 64]), op=ALU.mult), [prevT, cd], [prevT])
            pf = prevT[:].rearrange('p h d -> p (h d)')
            for q in range(4):
                kb.op('dve', lambda e, q=q, sbk=sbanks[q], pf=pf: e.tensor_tensor(out=pf[:, q * 512:(q + 1) * 512], in0=sbk[:, 0:512], in1=pf[:, q * 512:(q + 1) * 512], op=ALU.add), [sbanks[q], prevT], [prevT])
            kb.op('act', lambda e: e.copy(out=prevB[:], in_=prevT[:]), [prevT], [prevB])
            yield

        def genB(c):
            r0 = c * 128
            z_, x_ = zl[c % 2], xl[c % 2]
            t1 = t1s[c % 2]
            kb.op('dve', lambda e, z_=z_: e.tensor_tensor(out=y2[:], in0=t1[:], in1=z_[:], op=ALU.mult), [t1, z_], [y2])
            yield
            for g_ in range(8):
                kb.op('act', lambda e, g_=g_: e.activation(out=sq[:, g_ * 256:(g_ + 1) * 256], in_=y2[:, g_ * 256:(g_ + 1) * 256], func=AF.Square, accum_out=ssq8[:, g_:g_ + 1]), [y2], [sq, ssq8], nosync=(g_ > 0))
            yield
            kb.op('dve', lambda e: e.tensor_scalar(out=ssq8[:], in0=ssq8[:], scalar1=1.0 / 256, scalar2=1e-5, op0=ALU.mult, op1=ALU.add), [ssq8], [ssq8])
            kb.op('act', lambda e: e.activation(out=ssq8[:], in_=ssq8[:], func=AF.Ln), [ssq8], [ssq8])
            kb.op('act', lambda e: e.activation(out=ssq8[:], in_=ssq8[:], func=AF.Exp, scale=-0.5), [ssq8], [ssq8])
            yield
            for g_ in range(8):
                kb.op('dve', lambda e, g_=g_: e.scalar_tensor_tensor(out=y3[:, g_ * 256:(g_ + 1) * 256], in0=y2[:, g_ * 256:(g_ + 1) * 256], scalar=ssq8[:, g_:g_ + 1], in1=gnb[:, g_ * 256:(g_ + 1) * 256], op0=ALU.mult, op1=ALU.mult), [y2, ssq8, gnb], [y3], nosync=(g_ > 0))
            yield
            for hf in range(2):
                pt = ptr[hf]
                for ic in range(8):
                    kb.op('pe', lambda e, pt=pt, ic=ic, hf=hf: e.transpose(out=pt[:, ic * 128:(ic + 1) * 128], in_=y3[:, (hf * 8 + ic) * 128:(hf * 8 + ic + 1) * 128], identity=ident[:]), [y3, ident], [pt])
                kb.op('act', lambda e, pt=pt, hf=hf: e.copy(out=ynT[:, hf * 8:(hf + 1) * 8, :], in_=pt[:, 0:1024].rearrange('p (c t) -> p c t', t=128)), [pt], [ynT])
            yield
            o = xo[c % 2]
            for hf in range(2):
                yield
                pb = kb.bank(kb.banks[4:6])
                for ic in range(16):
                    kb.op('pe', lambda e, pb=pb, ic=ic, hf=hf: e.matmul(pb[:, 0:512], lhsT=ynT[:, ic, :], rhs=outw[:, ic, hf * 512:(hf + 1) * 512], start=(ic == 0), stop=(ic == 15)), [ynT, outw], [pb])
                kb.op('dve', lambda e, pb=pb, hf=hf, o=o, x_=x_: e.tensor_tensor(out=o[:, hf * 512:(hf + 1) * 512], in0=pb[:, 0:512], in1=x_[:, hf * 512:(hf + 1) * 512], op=ALU.add), [pb, x_], [o])
            kb.dma('sp', x1_d[r0:r0 + 128, :], o[:], reads=[o], writes=[x1_d])

        def _adv(g):
            if g is None:
                return None
            try:
                next(g)
                return g
            except StopIteration:
                return None
        gA = genA(0)
        while gA is not None:
            gA = _adv(gA)
        for c in range(NCH):
            gB = genB(c)
            gA = genA(c + 1) if c + 1 < NCH else None
            k_ = 0
            while gA is not None or gB is not None:
                gA = _adv(gA)
                if k_ % 2 == 1 or gA is None:
                    gB = _adv(gB)
                k_ += 1
    kb.scope = None
    kb.barrier()
    kb.new_epoch()


def new_kb():
    nc = bass.Bass("TRN2", target_bir_lowering=False)
    kb = KB(nc)
    allb = [kb.ps(f'bank{i}', [128, 512], F32) for i in range(8)]
    kb.banks = allb[0:6]
    kb.xbanks = allb[6:8]
    kb.ptr = [Buf(b.t[:].bitcast(BF16), f'ptrv{i}', parent=b) for i, b in enumerate(kb.xbanks)]
    return nc, kb


def peer_convert(kb, W, L, lazy=False):
    U16 = kb.scratch(f'sc_U16_{L}', [128, 128, 1024], BF16)
    V16 = kb.scratch(f'sc_V16_{L}', [128, 128, 1024], BF16)

    def gen():
        for q in range(64):
            for (src, dst) in ((W['peer_uT'], U16), (W['peer_v'], V16)):
                kb.dma('pool', dst.t[q * 2:(q + 1) * 2].rearrange('c p n -> p c n'), src.t[q * 2:(q + 1) * 2].rearrange('c p n -> p c n'), reads=[src], writes=[dst], sempool='cv')
                yield
    g = gen()
    if lazy:
        return U16, V16, g
    for _ in g:
        pass
    return U16, V16


def phase_peer(kb, C, x_in, x_out, W, U16, V16, NT=256, nst=None, extra_gen=None):
    ident, ident_f = C['c_ident'], C['c_ident_f']
    iota = C['c_iota_f']
    iota_b = C['c_iota'] if OPT_BF16_ONEHOT else C['c_iota_f']
    ptr = kb.ptr
    nsub = NT // 128
    NST = (S // NT) if nst is None else nst
    with ExitStack() as sc:
        kb.scope = sc
        qw = kb.sb('qw', [128, 8, 2048], BF16)
        for dc in range(8):
            kb.dma('pool', qw[:, dc, :], W['peer_q_w'][dc * 128:(dc + 1) * 128, :], reads=[W['peer_q_w']], writes=[qw])
        skT = kb.sb('skT', [128, 2, 128], BF16)
        kb.dma('pool', skT[:], W['peer_skT'].t.rearrange('c d k -> d c k'), reads=[W['peer_skT']], writes=[skT])
        gbc = bcast_load(kb, 'gbcp', W['peer_norm'][0:1, :], 1024, W['peer_norm'])
        G = kb.sb('G', [128, 128, NT], BF16)
        uvb = [(kb.sb(f'ub{i}', [128, 2, 1024], BF16), kb.sb(f'vb{i}', [128, 2, 1024], BF16)) for i in range(3)]
        arA = kb.sb('arA', [128, 2048], F32)
        arB = kb.sb('arB', [128, 2048], F32)
        arC = kb.sb('arC', [128, 2048], F32)
        qT = kb.sb('qT', [128, 16, NT], BF16)
        hTs = [kb.sb(f'hTp{i}', [128, 8, NT], BF16) for i in range(2)]
        hb = [kb.sb(f'hbp{i}', [128, 1024], BF16) for i in range(2)]
        xt = [kb.sb(f'xtp{i}', [128, 1024], F32) for i in range(2)]
        ssq = kb.sb('ssqp', [128, 1], F32)
        rstd = kb.sb('rstdp', [128, 1], F32)
        v16 = kb.sb('v16', [128, 16, 16], F32)
        idx = kb.sb('idx', [128, 16, 16], U32)
        idxf = kb.sb('idxf', [128, 16, 16], F32)
        c8 = kb.sb('c8', [128, 8, 16], F32)
        pos = kb.sb('pos', [128, 8, 16], U32)
        posf = kb.sb('posf', [128, 8, 16], F32)
        r1f = kb.sb('r1f', [128, 8, 16], F32)
        r2f = kb.sb('r2f', [128, 8, 16], F32)
        ge = kb.sb('ge', [128, 8, 16], F32)
        zz = kb.sb('zz', [128, 8], F32)
        IJG = kb.sb('IJG', [128, 3, 128], F32)
        IJGT = [kb.sb(f'IJGT{i}', [128, 3, 128], BF16 if OPT_BF16_ONEHOT else F32) for i in range(nsub)]
        XYb = [kb.sb(f'XYb{i}', [128, 8, 2, 128], BF16) for i in range(2)]
        gl = [kb.sb(f'gl{i}', [128, NT], BF16) for i in range(3)]
        wT = [kb.sb(f'wT{i}', [128, NT], BF16) for i in range(3)]
        thr = kb.sb('thr', [128, 16], F32)
        kb.op('dve', lambda e: e.tensor_scalar(out=thr[:], in0=iota[:, 0:16], scalar1=16.0, scalar2=16.0, op0=ALU.mult, op1=ALU.add), [iota], [thr])
        ob = kb.banks[0:4]
        aslots = [kb.banks[4], kb.banks[5], kb.xbanks[1]]
        rbank = kb.xbanks[0]
        LOOK = 2

        def routing(stile, qbank=None):
            t0 = stile * NT
            hT = hTs[stile % 2]
            for sub in range(nsub):
                xx = xt[sub % 2]
                r0 = t0 + sub * 128
                kb.dma('sp', xx[:], x_in[r0:r0 + 128, :], reads=[x_in], writes=[xx])
                h = hb[sub % 2]
                rmsnorm_bf(kb, xx, gbc, h, 1024, 1e-6, arC, ssq, rstd)
                yield
                transpose_to(kb, h, 1024, hT[:, :, sub * 128:(sub + 1) * 128], hT, ident, ptr[1])
                yield
            for fc in range(16):
                pb = rbank if qbank is None else qbank[fc % len(qbank)]
                for dc in range(8):
                    kb.op('pe', lambda e, pb=pb, dc=dc, fc=fc: e.matmul(pb[:, 0:NT], lhsT=qw[:, dc, fc * 128:(fc + 1) * 128], rhs=hT[:, dc, :], start=(dc == 0), stop=(dc == 7)), [qw, hT], [pb])
                kb.op('act', lambda e, pb=pb, fc=fc: e.copy(out=qT[:, fc, :], in_=pb[:, 0:NT]), [pb], [qT])
                yield
            for sub in range(nsub):
                s_sb = arA
                for q4 in range(4):
                    pb = rbank
                    for i4 in range(4):
                        fc = q4 * 4 + i4
                        kb.op('pe', lambda e, pb=pb, fc=fc, i4=i4, sub=sub: e.matmul(pb[:, i4 * 128:(i4 + 1) * 128], lhsT=qT[:, fc, sub * 128:(sub + 1) * 128], rhs=skT[:, fc % 2, :], start=True, stop=True), [qT, skT], [pb])
                    kb.op('act', lambda e, pb=pb, q4=q4: e.copy(out=s_sb[:, q4 * 512:(q4 + 1) * 512], in_=pb[:, 0:512]), [pb], [s_sb])
                    yield
                tmp = arC
                last = None
                for sg in range(16):
                    ss = s_sb[:, sg * 128:(sg + 1) * 128]
                    last = kb.op('dve', lambda e, ss=ss, sg=sg: e.max(out=v16[:, sg, 0:8], in_=ss), [s_sb], [v16], nosync=(sg > 0))
                    if sg % 4 == 3:
                        yield
                for sg in range(16):
                    ss = s_sb[:, sg * 128:(sg + 1) * 128]
                    t_ = kb.op('dve', lambda e, ss=ss, sg=sg: e.max_index(out=idx[:, sg, 0:8], in_max=v16[:, sg, 0:8], in_values=ss), [s_sb, v16], [idx], nosync=(sg > 0), after=([last] if sg == 0 else []))
                    if sg == 15:
                        last = t_
                    if sg % 4 == 3:
                        yield
                for sg in range(16):
                    ss = s_sb[:, sg * 128:(sg + 1) * 128]
                    t_ = kb.op('dve', lambda e, ss=ss, sg=sg: e.match_replace(out=tmp[:, sg * 128:(sg + 1) * 128], in_to_replace=v16[:, sg, 0:8], in_values=ss, imm_value=-1e30), [s_sb, v16], [tmp], nosync=(sg > 0), after=([last] if sg == 0 else []))
                    if sg == 15:
                        last = t_
                    if sg % 4 == 3:
                        yield
                for sg in range(16):
                    t_ = kb.op('dve', lambda e, sg=sg: e.max(out=v16[:, sg, 8:16], in_=tmp[:, sg * 128:(sg + 1) * 128]), [tmp], [v16], nosync=(sg > 0), after=([last] if sg == 0 else []))
                    if sg == 15:
                        last = t_
                    if sg % 4 == 3:
                        yield
                for sg in range(16):
                    t_ = kb.op('dve', lambda e, sg=sg: e.max_index(out=idx[:, sg, 8:16], in_max=v16[:, sg, 8:16], in_values=tmp[:, sg * 128:(sg + 1) * 128]), [tmp, v16], [idx], nosync=(sg > 0), after=([last] if sg == 0 else []))
                    if sg == 15:
                        last = t_
                    if sg % 4 == 3:
                        yield
                kb.op('dve', lambda e: e.tensor_copy(out=idxf[:], in_=idx[:]), [idx], [idxf])
                cand = arB
                v4 = v16[:].rearrange('p (h c) r -> p h c r', c=2)
                kb.op('dve', lambda e: e.tensor_tensor(out=cand[:].rearrange('p (h a b) -> p h a b', a=16, b=16), in0=v4[:, :, 0, :].unsqueeze(3).to_broadcast([128, 8, 16, 16]), in1=v4[:, :, 1, :].unsqueeze(2).to_broadcast([128, 8, 16, 16]), op=ALU.add), [v16], [cand])
                yield
                tmp2 = arA
                last = None
                for hh in range(8):
                    cs_ = cand[:, hh * 256:(hh + 1) * 256]
                    last = kb.op('dve', lambda e, cs_=cs_, hh=hh: e.max(out=c8[:, hh, 0:8], in_=cs_), [cand], [c8], nosync=(hh > 0))
                yield
                for hh in range(8):
                    cs_ = cand[:, hh * 256:(hh + 1) * 256]
                    t_ = kb.op('dve', lambda e, cs_=cs_, hh=hh: e.max_index(out=pos[:, hh, 0:8], in_max=c8[:, hh, 0:8], in_values=cs_), [cand, c8], [pos], nosync=(hh > 0), after=([last] if hh == 0 else []))
                    if hh == 7:
                        last = t_
                yield
                for hh in range(8):
                    cs_ = cand[:, hh * 256:(hh + 1) * 256]
                    t_ = kb.op('dve', lambda e, cs_=cs_, hh=hh: e.match_replace(out=tmp2[:, hh * 256:(hh + 1) * 256], in_to_replace=c8[:, hh, 0:8], in_values=cs_, imm_value=-1e30), [cand, c8], [tmp2], nosync=(hh > 0), after=([last] if hh == 0 else []))
                    if hh == 7:
                        last = t_
                yield
                for hh in range(8):
                    t_ = kb.op('dve', lambda e, hh=hh: e.max(out=c8[:, hh, 8:16], in_=tmp2[:, hh * 256:(hh + 1) * 256]), [tmp2], [c8], nosync=(hh > 0), after=([last] if hh == 0 else []))
                    if hh == 7:
                        last = t_
                yield
                for hh in range(8):
                    t_ = kb.op('dve', lambda e, hh=hh: e.max_index(out=pos[:, hh, 8:16], in_max=c8[:, hh, 8:16], in_values=tmp2[:, hh * 256:(hh + 1) * 256]), [tmp2, c8], [pos], nosync=(hh > 0), after=([last] if hh == 0 else []))
                    if hh == 7:
                        last = t_
                yield
                kb.op('dve', lambda e: e.tensor_tensor(out=ge[:], in0=c8[:], in1=c8[:, :, 0:1].to_broadcast([128, 8, 16]), op=ALU.subtract), [c8], [ge])
                kb.op('act', lambda e: e.activation(out=ge[:], in_=ge[:], func=AF.Exp), [ge], [ge])
                kb.op('dve', lambda e: e.tensor_reduce(out=zz[:], in_=ge[:], axis=AX.X, op=ALU.add), [ge], [zz])
                kb.op('dve', lambda e: e.reciprocal(out=zz[:], in_=zz[:]), [zz], [zz])
                kb.op('dve', lambda e: e.tensor_tensor(out=IJG[:, 2, :].rearrange('p (h k) -> p h k', k=16), in0=ge[:], in1=zz[:].unsqueeze(2).to_broadcast([128, 8, 16]), op=ALU.mult), [ge, zz], [IJG])
                yield
                kb.op('dve', lambda e: e.tensor_copy(out=posf[:], in_=pos[:]), [pos], [posf])
                kb.op('dve', lambda e: e.tensor_tensor(out=arA[:].rearrange('p (h k r) -> p h k r', k=16, r=16), in0=posf[:].unsqueeze(3).to_broadcast([128, 8, 16, 16]), in1=thr[:].unsqueeze(1).unsqueeze(1).to_broadcast([128, 8, 16, 16]), op=ALU.is_ge), [posf, thr], [arA])
                yield
                kb.op('dve', lambda e: e.tensor_reduce(out=r1f[:].rearrange('p h k -> p (h k)'), in_=arA[:].rearrange('p (m r) -> p m r', r=16), axis=AX.X, op=ALU.add), [arA], [r1f])
                kb.op('dve', lambda e: e.scalar_tensor_tensor(out=r2f[:], in0=r1f[:], scalar=-16.0, in1=posf[:], op0=ALU.mult, op1=ALU.add), [r1f, posf], [r2f])
                yield
                i4v = idxf[:].rearrange('p (h c) r -> p h c r', c=2)
                for which, rf in ((0, r1f), (1, r2f)):
                    eq = arA[:].rearrange('p (h k r) -> p h k r', k=16, r=16)
                    kb.op('dve', lambda e, rf=rf, eq=eq: e.tensor_tensor(out=eq, in0=iota[:, 0:16].unsqueeze(1).unsqueeze(1).to_broadcast([128, 8, 16, 16]), in1=rf[:].unsqueeze(3).to_broadcast([128, 8, 16, 16]), op=ALU.is_equal), [rf, iota], [arA])
                    yield
                    kb.op('dve', lambda e, eq=eq, which=which: e.tensor_tensor(out=arC[:].rearrange('p (h k r) -> p h k r', k=16, r=16), in0=eq, in1=i4v[:, :, which, :].unsqueeze(2).to_broadcast([128, 8, 16, 16]), op=ALU.mult), [arA, idxf], [arC])
                    yield
                    kb.op('dve', lambda e, which=which: e.tensor_reduce(out=IJG[:, which, :], in_=arC[:].rearrange('p (m r) -> p m r', r=16), axis=AX.X, op=ALU.add), [arC], [IJG])
                    yield
                pb = rbank
                for w3 in range(3):
                    kb.op('pe', lambda e, pb=pb, w3=w3: e.transpose(out=pb[:, w3 * 128:(w3 + 1) * 128], in_=IJG[:, w3, :], identity=ident_f[:]), [IJG, ident_f], [pb])
                ijgt = IJGT[sub]
                kb.op('act', lambda e, pb=pb, ijgt=ijgt: e.copy(out=ijgt[:].rearrange('p a t -> p (a t)'), in_=pb[:, 0:384]), [pb], [ijgt])
                yield

        def ggen(stile, gen=None):
            pbs = [rbank, kb.xbanks[1]]
            k = 0
            npumped = 0
            for sub in range(nsub):
                ijgt = IJGT[sub]
                for blk in range(16):
                    if extra_gen is not None and blk < 4:
                        try:
                            next(extra_gen)
                        except StopIteration:
                            pass
                    if gen is not None and npumped < 20:
                        npumped += 1
                        try:
                            next(gen)
                        except StopIteration:
                            pass
                    XY = XYb[blk % 2]
                    tb = blk * 8
                    kb.op('dve', lambda e, XY=XY, tb=tb, ijgt=ijgt: e.tensor_tensor(out=XY[:], in0=iota_b[:].unsqueeze(1).unsqueeze(1).to_broadcast([128, 8, 2, 128]), in1=ijgt[:, 0:2, tb:tb + 8].rearrange('p w t -> p t w').unsqueeze(3).to_broadcast([128, 8, 2, 128]), op=ALU.is_equal), [iota_b, ijgt], [XY], nosync=True)
                    kb.op('dve', lambda e, XY=XY, tb=tb, ijgt=ijgt: e.tensor_tensor(out=XY[:, :, 1, :], in0=XY[:, :, 1, :], in1=ijgt[:, 2, tb:tb + 8].unsqueeze(2).to_broadcast([128, 8, 128]), op=ALU.mult), [XY, ijgt], [XY])
                    for t4 in range(2):
                        pb = pbs[k % 2]
                        k += 1
                        for ti in range(4):
                            t = t4 * 4 + ti
                            kb.op('pe', lambda e, pb=pb, ti=ti, t=t, XY=XY: e.matmul(pb[:, ti * 128:(ti + 1) * 128], lhsT=XY[:, t, 1, :], rhs=XY[:, t, 0, :], start=True, stop=True), [XY], [pb])
                        tok = sub * 128 + tb + t4 * 4
                        kb.op('act', lambda e, pb=pb, tok=tok: e.copy(out=G[:, :, tok:tok + 4], in_=pb[:, 0:512].rearrange('p (t i) -> p i t', i=128)), [pb], [G])

        def stageF(stile, gen):
            hT = hTs[stile % 2]

            def pump(n):
                if gen is None:
                    return
                for _ in range(n):
                    try:
                        next(gen)
                    except StopIteration:
                        return

            def load(cg):
                ub, vb = uvb[cg % 3]
                kb.dma('sp', ub[:], U16.t[cg * 2:(cg + 1) * 2].rearrange('c p n -> p c n'), reads=[U16], writes=[ub])
                kb.dma('sp', vb[:], V16.t[cg * 2:(cg + 1) * 2].rearrange('c p n -> p c n'), reads=[V16], writes=[vb])

            def emit_a(c):
                ub, vb = uvb[(c // 2) % 3]
                pa = aslots[c % 3]
                ci = c % 2
                for dc in range(8):
                    kb.op('pe', lambda e, pa=pa, dc=dc, ci=ci, ub=ub: e.matmul(pa[:, 0:NT], lhsT=ub[:, ci, dc * 128:(dc + 1) * 128], rhs=hT[:, dc, :], start=(dc == 0), stop=(dc == 7)), [ub, hT], [pa])
                g_ = gl[c % 3]
                w_ = wT[c % 3]
                kb.op('act', lambda e, pa=pa, g_=g_: e.activation(out=g_[:], in_=pa[:, 0:NT], func=AF.Gelu), [pa], [g_])
                kb.op('pool' if (OPT_POOLMULT and c % OPT_POOLMULT == OPT_POOLMULT - 1) else 'dve', lambda e, g_=g_, w_=w_, c=c: e.tensor_tensor(out=w_[:], in0=g_[:], in1=G[:, c, :], op=ALU.mult), [g_, G], [w_])

            def emit_s2(c):
                ub, vb = uvb[(c // 2) % 3]
                w_ = wT[c % 3]
                ci = c % 2
                for tt in range(nsub):
                    for dh in range(2):
                        o = ob[tt * 2 + dh]
                        kb.op('pe', lambda e, o=o, tt=tt, dh=dh, ci=ci, vb=vb, w_=w_, c=c: e.matmul(o[:, 0:512], lhsT=w_[:, tt * 128:(tt + 1) * 128], rhs=vb[:, ci, dh * 512:(dh + 1) * 512], start=(c == 0), stop=(c == 127)), [w_, vb], [o])
            load(0)
            load(1)
            for c0 in range(LOOK):
                emit_a(c0)
            for c in range(128):
                if c % 2 == 0 and c // 2 + 2 < 64:
                    load(c // 2 + 2)
                if c + LOOK < 128:
                    emit_a(c + LOOK)
                emit_s2(c)
                pump(OPT_PUMP)
            pump(100000)

        def epilogue(stile):
            t0 = stile * NT
            for sub in range(nsub):
                xx = xt[sub % 2]
                r0 = t0 + sub * 128
                kb.dma('sp', xx[:], x_in[r0:r0 + 128, :], reads=[x_in], writes=[xx])
                for dh in range(2):
                    kb.op('dve', lambda e, xx=xx, dh=dh, o=ob[sub * 2 + dh]: e.tensor_tensor(out=xx[:, dh * 512:(dh + 1) * 512], in0=o[:, 0:512], in1=xx[:, dh * 512:(dh + 1) * 512], op=ALU.add), [ob[sub * 2 + dh], xx], [xx])
                kb.dma('sp', x_out[r0:r0 + 128, :], xx[:], reads=[xx], writes=[x_out])

        for _ in routing(0):
            pass
        for stile in range(NST):
            gen = routing(stile + 1, qbank=[kb.banks[4], kb.banks[5]]) if stile + 1 < NST else None
            ggen(stile, gen)
            if OPT_PUMP < 0:
                stageF(stile, None)
                epilogue(stile)
                if gen is not None:
                    for _ in gen:
                        pass
            else:
                stageF(stile, gen)
                epilogue(stile)
        if extra_gen is not None:
            for _ in extra_gen:
                pass
    kb.scope = None
    kb.barrier()
    kb.new_epoch()


def _adv(g):
    if g is None:
        return None
    try:
        next(g)
        return g
    except StopIteration:
        return None


def run_pipelined(genF, genB, n, ratio=1):
    g = genF(0)
    while g is not None:
        g = _adv(g)
    for i in range(n):
        gB = genB(i)
        gF = genF(i + 1) if i + 1 < n else None
        k_ = 0
        while gF is not None or gB is not None:
            gB = _adv(gB)
            if k_ % ratio == ratio - 1 or gB is None:
                gF = _adv(gF)
            k_ += 1


def phase_ple(kb, C, x_in, x_out, p_d, W, kv_out=None, final=False):
    ident = C['c_ident']
    ptr = kb.ptr
    with ExitStack() as sc:
        kb.scope = sc
        gw = kb.sb('gw', [128, 8, 1024], BF16)
        for dc in range(8):
            kb.dma('pool', gw[:, dc, :], W['ple_gate_w'][dc * 128:(dc + 1) * 128, :], reads=[W['ple_gate_w']], writes=[gw])
        pw = kb.sb('pw', [128, 2, 1024], BF16)
        for kc in range(2):
            kb.dma('pool', pw[:, kc, :], W['ple_proj'][kc * 128:(kc + 1) * 128, :], reads=[W['ple_proj']], writes=[pw])
        gbc = bcast_load(kb, 'gbc_ple', W['ple_norm'][0:1, :], 1024, W['ple_norm'])
        if kv_out is not None:
            kvw = kb.sb('kvw', [128, 8, 256], BF16)
            for dc in range(8):
                kb.dma('pool', kvw[:, dc, :], W['kv_w'][dc * 128:(dc + 1) * 128, :], reads=[W['kv_w']], writes=[kvw])
            gkv = bcast_load(kb, 'gbc_kv', W['kv_norm'][0:1, :], 1024, W['kv_norm'])
            kvb = bcast_load(kb, 'kvb', W['kv_b'][0:1, :], 256, W['kv_b'])
        if final:
            gfin = bcast_load(kb, 'gbc_fin', W['final_norm'][0:1, :], 1024, W['final_norm'])
        xt = [kb.sb(f'xtl{i}', [128, 1024], F32) for i in range(3)]
        pt_ = [kb.sb(f'ptl{i}', [128, 256], F32) for i in range(2)]
        pb16 = [kb.sb(f'pb16{i}', [128, 256], BF16) for i in range(2)]
        pT = [kb.sb(f'pTl{i}', [128, 2, 128], BF16) for i in range(2)]
        hb = [kb.sb(f'hbl{i}', [128, 1024], BF16) for i in range(2)]
        hT = [kb.sb(f'hTl{i}', [128, 8, 128], BF16) for i in range(2)]
        junk = kb.sb('junkl', [128, 1024], F32)
        ssq = kb.sb('ssql', [128, 1], F32)
        rstd = kb.sb('rstdl', [128, 1], F32)
        hbK = kb.sb('hbK', [128, 1024], BF16)
        hTK = kb.sb('hTK', [128, 8, 128], BF16)
        junkK = kb.sb('junkK', [128, 1024], F32)
        ssqK = kb.sb('ssqK', [128, 1], F32)
        rstdK = kb.sb('rstdK', [128, 1], F32)
        sg = kb.sb('sgl', [128, 1024], F32)
        x2 = [kb.sb(f'x2l{i}', [128, 1024], F32) for i in range(2)]
        kvt = [kb.sb(f'kvt{i}', [128, 256], F32) for i in range(2)]
        of = [kb.sb(f'ofl{i}', [128, 1024], F32) for i in range(2)]
        rot = kb.banks[0:4]
        rotK = kb.banks[4:6]

        def genF(n):
            r0 = n * 128
            xx, pp = xt[n % 3], pt_[n % 2]
            kb.dma('sp', xx[:], x_in[r0:r0 + 128, :], reads=[x_in], writes=[xx])
            kb.dma('sp', pp[:], p_d[r0:r0 + 128, :], reads=[p_d], writes=[pp])
            yield
            kb.op('act', lambda e, pp=pp: e.copy(out=pb16[n % 2][:], in_=pp[:]), [pp], [pb16[n % 2]])
            kb.op('act', lambda e, xx=xx: e.activation(out=junk[:], in_=xx[:], func=AF.Square, accum_out=ssq[:, 0:1]), [xx], [junk, ssq])
            yield
            kb.op('dve', lambda e: e.tensor_scalar(out=rstd[:, 0:1], in0=ssq[:, 0:1], scalar1=1.0 / 1024, scalar2=1e-6, op0=ALU.mult, op1=ALU.add), [ssq], [rstd])
            kb.op('act', lambda e: e.activation(out=rstd[:, 0:1], in_=rstd[:, 0:1], func=AF.Ln), [rstd], [rstd])
            yield
            kb.op('act', lambda e: e.activation(out=rstd[:, 0:1], in_=rstd[:, 0:1], func=AF.Exp, scale=-0.5), [rstd], [rstd])
            h = hb[n % 2]
            kb.op('dve', lambda e, xx=xx, h=h: e.scalar_tensor_tensor(out=h[:], in0=xx[:], scalar=rstd[:, 0:1], in1=gbc[:], op0=ALU.mult, op1=ALU.mult), [xx, rstd, gbc], [h])
            yield
            transpose_to(kb, h, 1024, hT[n % 2][:], hT[n % 2], ident, ptr[0])
            yield
            transpose_to(kb, pb16[n % 2], 256, pT[n % 2][:], pT[n % 2], ident, ptr[1], evac='dve')
            yield

        def genB(n):
            r0 = n * 128
            xx, xo = xt[n % 3], x2[n % 2]
            hT_, pT_ = hT[n % 2], pT[n % 2]
            for hf in range(2):
                pg = kb.bank(rot)
                for dc in range(8):
                    kb.op('pe', lambda e, pg=pg, dc=dc, hf=hf: e.matmul(pg[:, 0:512], lhsT=hT_[:, dc, :], rhs=gw[:, dc, hf * 512:(hf + 1) * 512], start=(dc == 0), stop=(dc == 7)), [hT_, gw], [pg])
                kb.op('act', lambda e, pg=pg, hf=hf: e.activation(out=sg[:, hf * 512:(hf + 1) * 512], in_=pg[:, 0:512], func=AF.Exp, scale=-1.0), [pg], [sg])
                kb.op('dve', lambda e, hf=hf: e.tensor_scalar(out=sg[:, hf * 512:(hf + 1) * 512], in0=sg[:, hf * 512:(hf + 1) * 512], scalar1=1.0, scalar2=None, op0=ALU.add), [sg], [sg])
                kb.op('dve', lambda e, hf=hf: e.reciprocal(out=sg[:, hf * 512:(hf + 1) * 512], in_=sg[:, hf * 512:(hf + 1) * 512]), [sg], [sg])
                pq = kb.bank(rot)
                for kc in range(2):
                    kb.op('pe', lambda e, pq=pq, kc=kc, hf=hf: e.matmul(pq[:, 0:512], lhsT=pT_[:, kc, :], rhs=pw[:, kc, hf * 512:(hf + 1) * 512], start=(kc == 0), stop=(kc == 1)), [pT_, pw], [pq])
                yield
                kb.op('dve', lambda e, pq=pq, hf=hf: e.tensor_tensor(out=sg[:, hf * 512:(hf + 1) * 512], in0=pq[:, 0:512], in1=sg[:, hf * 512:(hf + 1) * 512], op=ALU.mult), [pq, sg], [sg])
                yield
            kb.op('dve', lambda e, xx=xx, xo=xo: e.tensor_tensor(out=xo[:], in0=xx[:], in1=sg[:], op=ALU.add), [xx, sg], [xo])
            yield
            if kv_out is not None:
                kb.op('act', lambda e, xo=xo: e.activation(out=junkK[:], in_=xo[:], func=AF.Square, accum_out=ssqK[:, 0:1]), [xo], [junkK, ssqK])
                yield
                kb.op('dve', lambda e: e.tensor_scalar(out=rstdK[:, 0:1], in0=ssqK[:, 0:1], scalar1=1.0 / 1024, scalar2=1e-6, op0=ALU.mult, op1=ALU.add), [ssqK], [rstdK])
                kb.op('act', lambda e: e.activation(out=rstdK[:, 0:1], in_=rstdK[:, 0:1], func=AF.Ln), [rstdK], [rstdK])
                yield
                kb.op('act', lambda e: e.activation(out=rstdK[:, 0:1], in_=rstdK[:, 0:1], func=AF.Exp, scale=-0.5), [rstdK], [rstdK])
                kb.op('dve', lambda e, xo=xo: e.scalar_tensor_tensor(out=hbK[:], in0=xo[:], scalar=rstdK[:, 0:1], in1=gkv[:], op0=ALU.mult, op1=ALU.mult), [xo, rstdK, gkv], [hbK])
                yield
                transpose_to(kb, hbK, 1024, hTK[:], hTK, ident, ptr[1])
                yield
                pk = kb.bank(rotK)
                for dc in range(8):
                    kb.op('pe', lambda e, pk=pk, dc=dc: e.matmul(pk[:, 0:256], lhsT=hTK[:, dc, :], rhs=kvw[:, dc, :], start=(dc == 0), stop=(dc == 7)), [hTK, kvw], [pk])
                kv = kvt[n % 2]
                kb.op('dve', lambda e, pk=pk, kv=kv: e.tensor_tensor(out=kv[:], in0=pk[:, 0:256], in1=kvb[:], op=ALU.add), [pk, kvb], [kv])
                kb.dma('sp', kv_out[r0:r0 + 128, :], kv[:], reads=[kv], writes=[kv_out])
                yield
            if final:
                o = of[n % 2]
                kb.op('act', lambda e, xo=xo: e.activation(out=junkK[:], in_=xo[:], func=AF.Square, accum_out=ssqK[:, 0:1]), [xo], [junkK, ssqK])
                yield
                kb.op('dve', lambda e: e.tensor_scalar(out=rstdK[:, 0:1], in0=ssqK[:, 0:1], scalar1=1.0 / 1024, scalar2=1e-6, op0=ALU.mult, op1=ALU.add), [ssqK], [rstdK])
                kb.op('act', lambda e: e.activation(out=rstdK[:, 0:1], in_=rstdK[:, 0:1], func=AF.Ln), [rstdK], [rstdK])
                yield
                kb.op('act', lambda e: e.activation(out=rstdK[:, 0:1], in_=rstdK[:, 0:1], func=AF.Exp, scale=-0.5), [rstdK], [rstdK])
                kb.op('dve', lambda e, xo=xo, o=o: e.scalar_tensor_tensor(out=o[:], in0=xo[:], scalar=rstdK[:, 0:1], in1=gfin[:], op0=ALU.mult, op1=ALU.mult), [xo, rstdK, gfin], [o])
                kb.dma('sp', x_out[r0:r0 + 128, :], o[:], reads=[o], writes=[x_out])
                yield
            else:
                kb.dma('sp', x_out[r0:r0 + 128, :], xo[:], reads=[xo], writes=[x_out])
                yield
        run_pipelined(genF, genB, NCH, ratio=2)
    kb.scope = None
    kb.barrier()
    kb.new_epoch()


MAGIC = 12582912.0


def phase_attn(kb, C, x_in, x_out, kv_d, pos_d, W):
    ident = C['c_ident']
    ptr = kb.ptr
    with ExitStack() as sc:
        kb.scope = sc
        qw = kb.sb('qwa', [128, 8, 1024], BF16)
        ow = kb.sb('owa', [128, 8, 1024], BF16)
        for dc in range(8):
            kb.dma('pool', qw[:, dc, :], W['q_w'][dc * 128:(dc + 1) * 128, :], reads=[W['q_w']], writes=[qw])
            kb.dma('pool', ow[:, dc, :], W['o_w'][dc * 128:(dc + 1) * 128, :], reads=[W['o_w']], writes=[ow])
        gbc = bcast_load(kb, 'gbc_at', W['attn_norm'][0:1, :], 1024, W['attn_norm'])
        qb = bcast_load(kb, 'qb_at', W['q_b'][0:1, :], 1024, W['q_b'])
        obb = bcast_load(kb, 'ob_at', W['o_b'][0:1, :], 1024, W['o_b'])
        esink = bcast_load(kb, 'esink', W['sinks'][0:1, :], 16, W['sinks'])
        kb.op('act', lambda e: e.activation(out=esink[:], in_=esink[:], func=AF.Exp), [esink], [esink])
        invf = bcast_load(kb, 'invf', W['c_invf'][0:1, :], 8, W['c_invf'])
        posi = kb.sb('posi', [128, 32], I32)
        kb.dma('sp', posi[:], pos_d[:, :], reads=[pos_d], writes=[posi])
        posf = kb.sb('posfa', [128, 32], F32)
        kb.op('dve', lambda e: e.tensor_copy(out=posf[:], in_=posi[:]), [posi], [posf])
        yy = kb.sb('yy', [128, 32, 8], F32)
        nn_ = kb.sb('nn_', [128, 32, 8], F32)
        sinT = kb.sb('sinT', [128, 32, 8], F32)
        cosT = kb.sb('cosT', [128, 32, 8], F32)
        kb.op('dve', lambda e: e.tensor_tensor(out=yy[:], in0=posf[:].unsqueeze(2).to_broadcast([128, 32, 8]), in1=invf[:].unsqueeze(1).to_broadcast([128, 32, 8]), op=ALU.mult), [posf, invf], [yy])
        for (dst, shift) in ((sinT, 0.0), (cosT, 0.25)):
            if shift != 0.0:
                kb.op('dve', lambda e, shift=shift: e.tensor_scalar(out=yy[:], in0=yy[:], scalar1=shift, scalar2=None, op0=ALU.add), [yy], [yy])
            kb.op('dve', lambda e: e.tensor_scalar(out=nn_[:], in0=yy[:], scalar1=MAGIC, scalar2=None, op0=ALU.add), [yy], [nn_])
            kb.op('dve', lambda e: e.tensor_scalar(out=nn_[:], in0=nn_[:], scalar1=-MAGIC, scalar2=None, op0=ALU.add), [nn_], [nn_])
            kb.op('dve', lambda e: e.tensor_tensor(out=nn_[:], in0=yy[:], in1=nn_[:], op=ALU.subtract), [yy, nn_], [nn_])
            kb.op('act', lambda e, dst=dst: e.activation(out=dst[:], in_=nn_[:], func=AF.Sin, scale=2.0 * np.pi * (1.0 - 1e-6)), [nn_], [dst])
        xt = [kb.sb(f'xta{i}', [128, 1024], F32) for i in range(3)]
        kvl = [kb.sb(f'kvl{i}', [128, 256], F32) for i in range(2)]
        hb = [kb.sb(f'hba{i}', [128, 1024], BF16) for i in range(2)]
        hT = [kb.sb(f'hTa{i}', [128, 8, 128], BF16) for i in range(2)]
        junk = kb.sb('junka', [128, 1024], F32)
        ssq = kb.sb('ssqa', [128, 1], F32)
        rstd = kb.sb('rstda', [128, 1], F32)
        q = kb.sb('qa', [128, 16, 64], F32)
        rq = [kb.sb(f'rq{i}', [128, 16, 8], F32) for i in range(4)]
        rk = [kb.sb(f'rk{i}', [128, 2, 8], F32) for i in range(4)]
        q16 = kb.sb('q16', [128, 1024], BF16)
        qTz = kb.sb('qTz', [128, 16, 128], BF16)
        kb.op('dve', lambda e: e.memset(qTz[:], 0.0), [], [qTz])
        kdup = kb.sb('kdup', [128, 256], BF16)
        kTd = [kb.sb(f'kTd{i}', [128, 2, 128], BF16) for i in range(3)]
        vaug = [kb.sb(f'vaug{i}', [128, 2, 65], BF16) for i in range(3)]
        for i in range(3):
            kb.op('dve', lambda e, i=i: e.memset(vaug[i][:], 1.0), [], [vaug[i]])
        eT = [kb.sb(f'eT{i}', [128, 4, 128], F32) for i in range(2)]
        pT = [kb.sb(f'pTa{i}', [128, 4, 128], BF16) for i in range(4)]
        den = kb.sb('den', [128, 16], F32)
        o16 = kb.sb('o16', [128, 16, 64], BF16)
        oT = kb.sb('oTa', [128, 8, 128], BF16)
        xo = [kb.sb(f'xoa{i}', [128, 1024], F32) for i in range(2)]
        masks = {0: C['c_gt_f'], 1: C['c_triu_f']}
        obk = kb.banks[0:4]
        rot = kb.banks[4:6]
        pcnt = [0]

        def rope(buf, view, nh, n, R):
            cb_ = cosT[:, n, :].unsqueeze(1).to_broadcast([128, nh, 8])
            sb_ = sinT[:, n, :].unsqueeze(1).to_broadcast([128, nh, 8])
            t1, t2 = view[:, :, 0:8], view[:, :, 8:16]
            ra, rb, rc, rd = R
            kb.op('dve', lambda e: e.tensor_tensor(out=ra[:, 0:nh, :], in0=t1, in1=cb_, op=ALU.mult), [buf, cosT], [ra])
            kb.op('dve', lambda e: e.tensor_tensor(out=rb[:, 0:nh, :], in0=t2, in1=sb_, op=ALU.mult), [buf, sinT], [rb], nosync=True)
            kb.op('dve', lambda e: e.tensor_tensor(out=rc[:, 0:nh, :], in0=t2, in1=cb_, op=ALU.mult), [buf, cosT], [rc], nosync=True)
            kb.op('dve', lambda e: e.tensor_tensor(out=rd[:, 0:nh, :], in0=t1, in1=sb_, op=ALU.mult), [buf, sinT], [rd], nosync=True)
            kb.op('dve', lambda e: e.tensor_tensor(out=t1, in0=ra[:, 0:nh, :], in1=rb[:, 0:nh, :], op=ALU.subtract), [ra, rb, rd], [buf])
            kb.op('dve', lambda e: e.tensor_tensor(out=t2, in0=rc[:, 0:nh, :], in1=rd[:, 0:nh, :], op=ALU.add), [rc, rd], [buf])

        def genF(n):
            r0 = n * 128
            xx, kv = xt[n % 3], kvl[n % 2]
            kb.dma('sp', xx[:], x_in[r0:r0 + 128, :], reads=[x_in], writes=[xx])
            kb.dma('sp', kv[:], kv_d[r0:r0 + 128, :], reads=[kv_d], writes=[kv])
            yield
            h = hb[n % 2]
            rmsnorm_bf(kb, xx, gbc, h, 1024, 1e-6, junk, ssq, rstd)
            yield
            transpose_to(kb, h, 1024, hT[n % 2][:], hT[n % 2], ident, ptr[0])
            yield
            kview = kv[:, 0:128].rearrange('p (g d) -> p g d', d=64)
            rope(kv, kview, 2, n, rk)
            yield
            for dup in range(2):
                kb.op('dve', lambda e, dup=dup, kview=kview: e.tensor_copy(out=kdup[:].rearrange('p (g two d) -> p g two d', two=2, d=64)[:, :, dup, :], in_=kview), [kv], [kdup])
            va = vaug[n % 3]
            kb.op('act', lambda e, va=va, kv=kv: e.copy(out=va[:, :, 0:64], in_=kv[:, 128:256].rearrange('p (g d) -> p g d', d=64)), [kv], [va])
            yield
            kt = kTd[n % 3]
            transpose_to(kb, kdup, 256, kt[:], kt, ident, ptr[0], evac='dve')
            yield

        def genB(n):
            r0 = n * 128
            xx = xt[n % 3]
            hT_ = hT[n % 2]
            qf = q[:].rearrange('p h d -> p (h d)')
            for hf in range(2):
                pq = kb.bank(rot)
                for dc in range(8):
                    kb.op('pe', lambda e, pq=pq, dc=dc, hf=hf: e.matmul(pq[:, 0:512], lhsT=hT_[:, dc, :], rhs=qw[:, dc, hf * 512:(hf + 1) * 512], start=(dc == 0), stop=(dc == 7)), [hT_, qw], [pq])
                kb.op('dve', lambda e, pq=pq, hf=hf: e.tensor_tensor(out=qf[:, hf * 512:(hf + 1) * 512], in0=pq[:, 0:512], in1=qb[:, hf * 512:(hf + 1) * 512], op=ALU.add), [pq, qb], [q])
                yield
            rope(q, q[:], 16, n, rq)
            yield
            kb.op('act', lambda e: e.activation(out=q16[:], in_=qf, func=AF.Copy, scale=0.125), [q], [q16])
            pt = ptr[1]
            for c in range(8):
                kb.op('pe', lambda e, c=c, pt=pt: e.transpose(out=pt[:, c * 128:(c + 1) * 128], in_=q16[:, c * 128:(c + 1) * 128], identity=ident[:]), [q16, ident], [pt])
            for par in range(2):
                src = pt[par * 64:(par + 1) * 64, 0:1024].rearrange('p (c t) -> p c t', t=128)
                dstv = qTz[par * 64:(par + 1) * 64, :, :].rearrange('p (c two) t -> p c two t', two=2)[:, :, par, :]
                kb.op('dve' if par == 0 else 'act', (lambda e, src=src, dstv=dstv: e.tensor_copy(out=dstv, in_=src)) if par == 0 else (lambda e, src=src, dstv=dstv: e.copy(out=dstv, in_=src)), [pt], [qTz])
            yield
            blocks = ([(kTd[(n - 1) % 3], vaug[(n - 1) % 3], 0)] if n > 0 else []) + [(kTd[n % 3], vaug[n % 3], 1)]
            def _scores(hg):
                pts = []
                for (kk, vv, mi) in blocks:
                    ps_ = kb.bank(rot)
                    for hh in range(4):
                        h = hg * 4 + hh
                        g = h // 8
                        kb.op('pe', lambda e, ps_=ps_, hh=hh, h=h, g=g, kk=kk: e.matmul(ps_[:, hh * 128:(hh + 1) * 128], lhsT=kk[:, g, :], rhs=qTz[:, h, :], start=True, stop=True), [kk, qTz], [ps_])
                    et = eT[pcnt[0] % 2]
                    p_ = pT[pcnt[0] % 4]
                    pcnt[0] += 1
                    kb.op('act', lambda e, ps_=ps_, et=et: e.activation(out=et[:].rearrange('p h q -> p (h q)'), in_=ps_[:, 0:512], func=AF.Exp), [ps_], [et])
                    kb.op('dve', lambda e, et=et, p_=p_, mi=mi: e.tensor_tensor(out=p_[:], in0=et[:], in1=masks[mi][:].unsqueeze(1).to_broadcast([128, 4, 128]), op=ALU.mult), [et, masks[mi]], [p_])
                    pts.append((p_, vv))
                return pts
            pts_next = _scores(0)
            yield
            for hg in range(4):
                pts = pts_next
                if hg + 1 < 4:
                    pts_next = _scores(hg + 1)
                for hh in range(4):
                    h = hg * 4 + hh
                    g = h // 8
                    for bi, (p_, vv) in enumerate(pts):
                        kb.op('pe', lambda e, hh=hh, g=g, p_=p_, vv=vv, bi=bi, hg=hg, nb=len(pts): e.matmul(obk[hg][:, hh * 65:(hh + 1) * 65], lhsT=p_[:, hh, :], rhs=vv[:, g, :], start=(bi == 0), stop=(bi == nb - 1)), [p_, vv], [obk[hg]])
                yield
            for hg in range(4):
                ov = obk[hg][:, 0:260].rearrange('p (h d) -> p h d', d=65)
                kb.op('dve', lambda e, ov=ov, hg=hg: e.tensor_tensor(out=den[:, hg * 4:(hg + 1) * 4], in0=ov[:, :, 64], in1=esink[:, hg * 4:(hg + 1) * 4], op=ALU.add), [obk[hg], esink], [den], nosync=(hg > 0))
            kb.op('dve', lambda e: e.reciprocal(out=den[:], in_=den[:]), [den], [den])
            yield
            for hg in range(4):
                ov = obk[hg][:, 0:260].rearrange('p (h d) -> p h d', d=65)
                kb.op('dve', lambda e, ov=ov, hg=hg: e.tensor_tensor(out=o16[:, hg * 4:(hg + 1) * 4, :], in0=ov[:, :, 0:64], in1=den[:, hg * 4:(hg + 1) * 4].unsqueeze(2).to_broadcast([128, 4, 64]), op=ALU.mult), [obk[hg], den], [o16], nosync=(hg > 0))
            yield
            o16f = o16[:].rearrange('p h d -> p (h d)')
            pt = ptr[1]
            for c in range(8):
                kb.op('pe', lambda e, c=c, pt=pt, o16f=o16f: e.transpose(out=pt[:, c * 128:(c + 1) * 128], in_=o16f[:, c * 128:(c + 1) * 128], identity=ident[:]), [o16, ident], [pt])
            kb.op('act', lambda e, pt=pt: e.copy(out=oT[:], in_=pt[:, 0:1024].rearrange('p (c t) -> p c t', t=128)), [pt], [oT])
            yield
            o = xo[n % 2]
            for hf in range(2):
                po = kb.bank(rot)
                for ic in range(8):
                    kb.op('pe', lambda e, po=po, ic=ic, hf=hf: e.matmul(po[:, 0:512], lhsT=oT[:, ic, :], rhs=ow[:, ic, hf * 512:(hf + 1) * 512], start=(ic == 0), stop=(ic == 7)), [oT, ow], [po])
                kb.op('dve', lambda e, po=po, hf=hf, o=o: e.tensor_tensor(out=o[:, hf * 512:(hf + 1) * 512], in0=po[:, 0:512], in1=obb[:, hf * 512:(hf + 1) * 512], op=ALU.add), [po, obb], [o])
                yield
            kb.op('dve', lambda e, o=o, xx=xx: e.tensor_tensor(out=o[:], in0=o[:], in1=xx[:], op=ALU.add), [o, xx], [o])
            kb.dma('sp', x_out[r0:r0 + 128, :], o[:], reads=[o], writes=[x_out])
            yield
        run_pipelined(genF, genB, NCH, ratio=3)
    kb.scope = None
    kb.barrier()
    kb.new_epoch()


WSPEC = [
    ('in_w', [1024, 6176]), ('ssm_norm', [1, 1024]), ('dt_bias', [1, 32]), ('conv_wT', [4096, 4]), ('conv_b2', [128, 32]),
    ('out_w', [2048, 1024]), ('gate_norm', [1, 2048]), ('A_log', [1, 32]), ('D', [1, 32]),
    ('kv_w', [1024, 256]), ('kv_norm', [1, 1024]), ('kv_b', [1, 256]),
    ('q_w', [1024, 1024]), ('o_w', [1024, 1024]), ('attn_norm', [1, 1024]), ('q_b', [1, 1024]), ('o_b', [1, 1024]), ('sinks', [1, 16]), ('c_invf', [1, 8]),
    ('final_norm', [1, 1024]),
]
LSPEC = [('peer_uT', [128, 128, 1024]), ('peer_v', [128, 128, 1024]), ('peer_q_w', [1024, 2048]), ('peer_skT', [2, 128, 128]), ('peer_norm', [1, 1024]),
         ('ple_gate_w', [1024, 1024]), ('ple_proj', [256, 1024]), ('ple_norm', [1, 1024])]


def build_all():
    nc, kb = new_kb()
    C = load_consts(kb)
    x_d = kb.dram('x', [S, 1024], F32, "ExternalInput")
    p0_d = kb.dram('p0', [S, 256], F32, "ExternalInput")
    p1_d = kb.dram('p1', [S, 256], F32, "ExternalInput")
    pos_d = kb.dram('pos', [128, 32], I32, "ExternalInput")
    out_d = kb.dram('out', [S, 1024], F32, "ExternalOutput")
    W = {}
    for name, shape in WSPEC:
        W[name] = kb.dram('w_' + name, shape, F32, "ExternalInput")
    WL = [{}, {}]
    for L in range(2):
        for name, shape in LSPEC:
            WL[L][name] = kb.dram(f'w{L}_' + name, shape, F32, "ExternalInput")
    xs = [kb.scratch(f'sc_x{i}', [S, 1024], F32) for i in range(6)]
    kv_d = kb.scratch('sc_kv', [S, 256], F32)
    UV = []

    lazy1 = []

    def _conv():
        UV.append(peer_convert(kb, WL[0], 0))
        u1, v1, g1 = peer_convert(kb, WL[1], 1, lazy=True)
        UV.append((u1, v1))
        lazy1.append(g1)
    phase1(kb, C, x_d, xs[0], W, after_loads=_conv)
    phase_peer(kb, C, xs[0], xs[1], WL[0], UV[0][0], UV[0][1], extra_gen=lazy1[0])
    Wp = dict(W); Wp.update(WL[0])
    phase_ple(kb, C, xs[1], xs[2], p0_d, Wp, kv_out=kv_d)
    phase_attn(kb, C, xs[2], xs[3], kv_d, pos_d, W)
    phase_peer(kb, C, xs[3], xs[4], WL[1], UV[1][0], UV[1][1])
    Wp = dict(W); Wp.update(WL[1])
    phase_ple(kb, C, xs[4], out_d, p1_d, Wp, final=True)
    kb.finish([out_d.lw])
    kb.emit()
    return nc


def host_inputs(inp):
    f = np.float32
    shared = dict(make_consts())
    shared['w_in_w'] = np.ascontiguousarray(inp['ssm_in_w'][0], f)
    shared['w_ssm_norm'] = np.ascontiguousarray(inp['ssm_norm'], f).reshape(1, 1024)
    shared['w_dt_bias'] = np.ascontiguousarray(inp['ssm_dt_bias'], f).reshape(1, 32)
    shared['w_conv_wT'] = np.ascontiguousarray(np.asarray(inp['ssm_conv_w'][0], f).T)
    shared['w_conv_b2'] = np.ascontiguousarray(np.asarray(inp['ssm_conv_b'][0], f).reshape(32, 128).T)
    shared['w_out_w'] = np.ascontiguousarray(inp['ssm_out_w'][0], f)
    shared['w_gate_norm'] = np.ascontiguousarray(inp['ssm_gate_norm'], f).reshape(1, 2048)
    shared['w_A_log'] = np.ascontiguousarray(inp['ssm_A_log'], f).reshape(1, 32)
    shared['w_D'] = np.ascontiguousarray(inp['ssm_D'], f).reshape(1, 32)
    shared['w_kv_w'] = np.ascontiguousarray(inp['kv_w'], f)
    shared['w_kv_norm'] = np.ascontiguousarray(inp['kv_norm'], f).reshape(1, 1024)
    shared['w_kv_b'] = np.ascontiguousarray(inp['kv_b'], f).reshape(1, 256)
    shared['w_q_w'] = np.ascontiguousarray(inp['q_w'][0], f)
    shared['w_o_w'] = np.ascontiguousarray(inp['o_w'][0], f)
    shared['w_attn_norm'] = np.ascontiguousarray(inp['attn_norm'], f).reshape(1, 1024)
    shared['w_q_b'] = np.ascontiguousarray(inp['q_b'], f).reshape(1, 1024)
    shared['w_o_b'] = np.ascontiguousarray(inp['o_b'], f).reshape(1, 1024)
    shared['w_sinks'] = np.ascontiguousarray(inp['sinks'], f).reshape(1, 16)
    shared['w_c_invf'] = (np.power(500000.0, -np.arange(0, 16, 2, dtype=np.float32) / 16) / (2 * np.pi)).astype(f)[None]
    shared['w_final_norm'] = np.ascontiguousarray(inp['final_norm'], f).reshape(1, 1024)
    for L in range(2):
        u = np.asarray(inp['peer_u'][L], f)
        shared[f'w{L}_peer_uT'] = np.ascontiguousarray(u.reshape(128, 128, 8, 128).transpose(0, 3, 2, 1)).reshape(128, 128, 1024)
        shared[f'w{L}_peer_v'] = np.ascontiguousarray(np.asarray(inp['peer_v'][L], f).reshape(128, 128, 1024))
        shared[f'w{L}_peer_q_w'] = np.ascontiguousarray(inp['peer_q_w'][L], f)
        shared[f'w{L}_peer_skT'] = np.ascontiguousarray(np.asarray(inp['peer_sub_keys'][L], f).transpose(0, 2, 1))
        shared[f'w{L}_peer_norm'] = np.ascontiguousarray(inp['peer_norm'][L], f).reshape(1, 1024)
        shared[f'w{L}_ple_gate_w'] = np.ascontiguousarray(inp['ple_gate_w'][L], f)
        shared[f'w{L}_ple_proj'] = np.ascontiguousarray(inp['ple_proj'][L], f)
        shared[f'w{L}_ple_norm'] = np.ascontiguousarray(inp['ple_norm'][L], f).reshape(1, 1024)
    maps = []
    for b in range(8):
        m = dict(shared)
        m['x'] = np.ascontiguousarray(inp['x'][b], f)
        m['p0'] = np.ascontiguousarray(inp['p'][0, b], f)
        m['p1'] = np.ascontiguousarray(inp['p'][1, b], f)
        m['pos'] = np.ascontiguousarray(np.asarray(inp['positions'][b], np.int32).reshape(32, 128).T)
        maps.append(m)
    return maps


_NC = None


def kernel(**inputs):
    global _NC
    inp = {k: np.asarray(v) for k, v in inputs.items()}
    if _NC is None:
        _NC = build_all()
    maps = host_inputs(inp)
    res = run_bass_kernel_spmd(_NC, maps, core_ids=list(range(8)))
    out = np.stack([np.asarray(r['out'], np.float32) for r in res.results], axis=0)
    return out
```

```python
import numpy as np
from contextlib import ExitStack
import concourse.bass as bass
import concourse.mybir as mybir
from concourse.bass_utils import run_bass_kernel_spmd

F32 = mybir.dt.float32
BF16 = mybir.dt.bfloat16
I32 = mybir.dt.int32
U32 = mybir.dt.uint32
AF = mybir.ActivationFunctionType
ALU = mybir.AluOpType
AX = mybir.AxisListType

SYNC_SAME_ENGINE = True
OPT_BF16_ONEHOT = False
OPT_NOSYNC = True
OPT_PUMP = 1
OPT_TB = 16
OPT_POOLMULT = 0
PAD1A = 3
DEBUG = False
S = 4096
NCH = 32


class Buf:
    def __init__(self, t, name, parent=None):
        self.t = t
        self.name = name
        self.parent = parent
        self._lw = None
        self._rd = []

    @property
    def lw(self):
        return self.parent.lw if self.parent is not None else self._lw

    @lw.setter
    def lw(self, v):
        if self.parent is not None:
            self.parent.lw = v
        else:
            self._lw = v

    @property
    def rd(self):
        return self.parent.rd if self.parent is not None else self._rd

    @rd.setter
    def rd(self, v):
        if self.parent is not None:
            self.parent.rd = v
        else:
            self._rd = v

    def __getitem__(self, idx):
        return self.t[idx]


class KB:
    ENG = ('pe', 'act', 'dve', 'pool', 'sp')

    def __init__(self, nc, n_dma_sems=16):
        self.nc = nc
        self.es = ExitStack()
        self.ops = {e: [] for e in self.ENG}
        self.cnt = {e: 0 for e in self.ENG}
        self.sem = {}
        self.ekey = {}
        self.epoch = 0
        for e in ('pe', 'act', 'dve', 'pool'):
            self.ekey[e] = e + '@0'
            self.sem[self.ekey[e]] = self.es.enter_context(nc.semaphore('s_' + e + '0'))
        self.ekey['sp'] = 'sp@0'
        self.dma_sems = []
        self.dma_pool = {'hw': [], 'sw': []}
        self.dma_pool['cv'] = []
        for i in range(n_dma_sems + 8 + 4):
            s = self.es.enter_context(nc.semaphore(f'd{i}'))
            self.sem[f'd{i}'] = s
            ent = [f'd{i}', 0]
            pk = 'hw' if i < n_dma_sems else ('sw' if i < n_dma_sems + 8 else 'cv')
            if pk != 'cv':
                self.dma_sems.append(ent)
            self.dma_pool[pk].append(ent)
        self.dma_rr = {'hw': 0, 'sw': 0, 'cv': 0}
        self.waited = {e: {} for e in self.ENG}
        self.final_tokens = []
        self.banks = []
        self.bank_rr = 0
        self.scope = None
        self.pending = {e: [] for e in self.ENG}

    def new_epoch(self):
        self.epoch += 1
        for e in ('pe', 'act', 'dve', 'pool'):
            self.ekey[e] = f'{e}@{self.epoch}'
            self.sem[self.ekey[e]] = self.es.enter_context(self.nc.semaphore(f's_{e}{self.epoch}'))
            self.cnt[e] = 0

    def barrier(self):
        cur = {self.ekey[e]: self.cnt[e] for e in ('pe', 'act', 'dve', 'pool')}
        for k, v in self.dma_sems:
            cur[k] = v
        for e in self.ENG:
            for k, v in cur.items():
                if v > 0 and self.waited[e].get(k, 0) < v:
                    self.waited[e][k] = v
                    self.pending[e].append((k, v))

    def sb(self, name, shape, dtype):
        es = self.scope if self.scope is not None else self.es
        self.uid = getattr(self, 'uid', 0) + 1
        name = f'{name}_u{self.uid}'
        t = es.enter_context(self.nc.sbuf_tensor(name, list(shape), dtype))
        return Buf(t, name)

    def ps(self, name, shape, dtype):
        t = self.es.enter_context(self.nc.psum_tensor(name, list(shape), dtype))
        return Buf(t, name)

    def dram(self, name, shape, dtype, kind):
        t = self.nc.dram_tensor(name, list(shape), dtype, kind=kind)
        return Buf(t.ap(), name)

    def scratch(self, name, shape, dtype):
        return self.dram(name, shape, dtype, "ExternalOutput" if DEBUG else "Internal")

    def dbg(self, name, buf, shape, dtype):
        if not DEBUG:
            return
        d = self.dram('dbg_' + name, shape, dtype, "ExternalOutput")
        self.dma('sp', d[:], buf[:], reads=[buf], writes=[d])

    def bank(self, pool=None):
        bs = self.banks if pool is None else pool
        b = bs[self.bank_rr % len(bs)]
        self.bank_rr += 1
        return b

    def _deps(self, eng, reads, writes, nosync=False, after=()):
        toks = {}

        def add(tok, force=False):
            if tok is None:
                return
            k, v = tok
            if k == self.ekey[eng] and not force and (nosync or not (SYNC_SAME_ENGINE and eng in ('act', 'dve', 'pool'))):
                return
            if toks.get(k, 0) < v:
                toks[k] = v
        for b in reads:
            add(b.lw)
        for b in writes:
            add(b.lw)
            for r in b.rd:
                add(r)
        for t in after:
            add(t, True)
        w = self.waited[eng]
        out = []
        for k, v in toks.items():
            if w.get(k, 0) < v:
                w[k] = v
                out.append((k, v))
        return out

    def _commit(self, tok, reads, writes):
        for b in reads:
            b.rd.append(tok)
            if len(b.rd) > 32:
                m = {}
                for k, v in b.rd:
                    m[k] = max(m.get(k, 0), v)
                b.rd = list(m.items())
        for b in writes:
            b.lw = tok
            b.rd = []

    def op(self, eng, fn, reads=(), writes=(), nosync=False, after=()):
        waits = self.pending[eng] + self._deps(eng, reads, writes, nosync and OPT_NOSYNC, after)
        self.pending[eng] = []
        self.cnt[eng] += 1
        tok = (self.ekey[eng], self.cnt[eng])
        self.ops[eng].append((waits, fn, (self.ekey[eng], 1)))
        self._commit(tok, reads, writes)
        return tok

    def dma(self, q, out_ap, in_ap, reads=(), writes=(), **kw):
        pk = kw.pop('sempool', None) or ('sw' if q == 'pool' else 'hw')
        pl = self.dma_pool[pk]
        ent = pl[self.dma_rr[pk] % len(pl)]
        self.dma_rr[pk] += 1
        key = ent[0]
        waits = self.pending[q] + self._deps(q, reads, writes)
        self.pending[q] = []
        if ent[1] > 0 and self.waited[q].get(key, 0) < ent[1]:
            self.waited[q][key] = ent[1]
            waits.append((key, ent[1]))
        ent[1] += 16
        tok = (key, ent[1])

        def fn(e, out_ap=out_ap, in_ap=in_ap, kw=kw):
            return e.dma_start(out=out_ap, in_=in_ap, **kw)
        self.ops[q].append((waits, fn, (key, 16)))
        self._commit(tok, reads, writes)
        return tok

    def finish(self, tokens):
        self.final_tokens = list(tokens)

    def emit(self):
        nc = self.nc
        with nc.Block() as block:
            def body(ename):
                def _f(e):
                    for waits, fn, (sk, inc) in self.ops[ename]:
                        for k, v in waits:
                            e.wait_ge(self.sem[k], v)
                        ins = fn(e)
                        ins.then_inc(self.sem[sk], inc)
                    if ename == 'sp':
                        for k, v in self.final_tokens:
                            e.wait_ge(self.sem[k], v)
                return _f
            block.tensor(body('pe'))
            block.scalar(body('act'))
            block.vector(body('dve'))
            block.gpsimd(body('pool'))
            block.sync(body('sp'))
        self.es.close()


def make_consts():
    k = np.arange(128)
    c = {}
    c['c_ident'] = np.eye(128, dtype=np.float32)
    c['c_triu'] = (k[:, None] <= k[None, :]).astype(np.float32)
    c['c_gt'] = (k[:, None] > k[None, :]).astype(np.float32)
    c['c_ones'] = np.ones((128, 128), np.float32)
    c['c_iota'] = np.broadcast_to(k[None, :].astype(np.float32), (128, 128)).copy()
    return c


def load_consts(kb, names=('c_ident', 'c_triu', 'c_gt', 'c_ones', 'c_iota')):
    C = {}
    for n in names:
        d = kb.dram(n, [128, 128], F32, "ExternalInput")
        sb = kb.sb(n + '_bf', [128, 128], BF16)
        kb.dma('pool', sb[:], d[:], reads=[d], writes=[sb])
        C[n] = sb
        sf = kb.sb(n + '_f', [128, 128], F32)
        kb.dma('sp', sf[:], d[:], reads=[d], writes=[sf])
        C[n + '_f'] = sf
    return C


def bcast_load(kb, name, dram_ap_1xn, n, dbuf):
    sb = kb.sb(name, [128, n], F32)
    kb.dma('sp', sb[:], dram_ap_1xn.partition_broadcast(128), reads=[dbuf], writes=[sb])
    return sb


def rmsnorm_bf(kb, x, gbc, hb, D, eps, junk, ssq, rstd, eng2='dve'):
    kb.op('act', lambda e: e.activation(out=junk[:, 0:D], in_=x[:, 0:D], func=AF.Square, accum_out=ssq[:, 0:1]), [x], [junk, ssq])
    kb.op('dve', lambda e: e.tensor_scalar(out=rstd[:, 0:1], in0=ssq[:, 0:1], scalar1=1.0 / D, scalar2=eps, op0=ALU.mult, op1=ALU.add), [ssq], [rstd])
    kb.op('act', lambda e: e.activation(out=rstd[:, 0:1], in_=rstd[:, 0:1], func=AF.Ln), [rstd], [rstd])
    kb.op('act', lambda e: e.activation(out=rstd[:, 0:1], in_=rstd[:, 0:1], func=AF.Exp, scale=-0.5), [rstd], [rstd])
    kb.op(eng2, lambda e: e.scalar_tensor_tensor(out=hb[:, 0:D], in0=x[:, 0:D], scalar=rstd[:, 0:1], in1=gbc[:, 0:D], op0=ALU.mult, op1=ALU.mult), [x, rstd, gbc], [hb])


def transpose_to(kb, src, ncols, dst_ap, dst, ident, ptr, evac='act'):
    n = ncols // 128
    for c in range(n):
        kb.op('pe', lambda e, c=c: e.transpose(out=ptr[:, c * 128:(c + 1) * 128], in_=src[:, c * 128:(c + 1) * 128], identity=ident[:]), [src, ident], [ptr])
    pv = ptr[:, 0:ncols].rearrange('p (c t) -> p c t', t=128)
    if evac == 'act':
        kb.op('act', lambda e: e.copy(out=dst_ap, in_=pv), [ptr], [dst])
    else:
        kb.op(evac, lambda e: e.tensor_copy(out=dst_ap, in_=pv), [ptr], [dst])


def phase1(kb, C, x_d, x1_d, W, after_loads=None):
    nc = kb.nc
    XB = kb.scratch('sc_XB', [S, 3072], BF16)
    BTs = kb.scratch('sc_BT', [NCH, 128, 8, 128], BF16)
    CTs = kb.scratch('sc_CT', [NCH, 128, 8, 128], BF16)
    ZS = kb.scratch('sc_ZS', [S, 2048], BF16)
    DT = kb.scratch('sc_DT', [S, 32], F32)
    ident = C['c_ident']
    ptr = kb.ptr
    with ExitStack() as sc:
        kb.scope = sc
        inw = kb.sb('inw', [128, 8, 6176], BF16)
        padbank = kb.banks[5]
        for dc in range(8):
            kb.dma('pool', inw[:, dc, :], W['in_w'][dc * 128:(dc + 1) * 128, :], reads=[W['in_w']], writes=[inw])
        gbc = bcast_load(kb, 'gbc1', W['ssm_norm'][0:1, :], 1024, W['ssm_norm'])
        dtb = bcast_load(kb, 'dtb', W['dt_bias'][0:1, :], 32, W['dt_bias'])
        cw = kb.sb('cw', [128, 32, 4], F32)
        kb.dma('sp', cw[:], W['conv_wT'].t.rearrange('(f p) k -> p f k', p=128), reads=[W['conv_wT']], writes=[cw])
        cbias = kb.sb('cbias', [128, 32], F32)
        kb.dma('sp', cbias[:], W['conv_b2'][:, :], reads=[W['conv_b2']], writes=[cbias])
        Uall = kb.sb('Uall', [128, 32, 516], BF16)
        kb.op('dve', lambda e: e.memset(Uall[:], 0.0), [], [Uall])
        Us = [Buf(Uall.t[:, f, :], f'U{f}') for f in range(32)]
        for u_ in Us:
            u_.lw = Uall.lw
        xt = [kb.sb(f'xt{i}', [128, 1024], F32) for i in range(2)]
        junk = kb.sb('junk', [128, 1024], F32)
        ssq = kb.sb('ssq', [128, 1], F32)
        rstd = kb.sb('rstd', [128, 1], F32)
        hb = [kb.sb(f'hb{i}', [128, 1024], BF16) for i in range(2)]
        hT = kb.sb('hT', [128, 8, 512], BF16)
        acc = [kb.sb(f'acc{i}', [128, 512], F32) for i in range(3)]
        xbcf = [kb.sb(f'xbcf{i}', [128, 512], BF16) for i in range(5)]
        XBo = kb.sb('XBo', [128, 4, 3072], BF16)
        zo = [kb.sb(f'zo{i}', [128, 2048], BF16) for i in range(2)]
        dtt = [kb.sb(f'dtt{i}', [128, 32], F32) for i in range(2)]
        if after_loads is not None:
            after_loads()
        for st in range(8):
            for j in range(4):
                xx = xt[j % 2]
                r0 = st * 512 + j * 128
                kb.dma('sp', xx[:], x_d[r0:r0 + 128, :], reads=[x_d], writes=[xx])
                h = hb[j % 2]
                rmsnorm_bf(kb, xx, gbc, h, 1024, 1e-6, junk, ssq, rstd)
                transpose_to(kb, h, 1024, hT[:, :, j * 128:(j + 1) * 128], hT, ident, ptr[j % 2])
            for f in range(32):
                pb = kb.bank(kb.banks[0:5])
                for dc in range(8):
                    kb.op('pe', lambda e, pb=pb, dc=dc, f=f: e.matmul(pb[:, 0:512], lhsT=inw[:, dc, 2048 + f * 128:2048 + (f + 1) * 128], rhs=hT[:, dc, :], start=(dc == 0), stop=(dc == 7)), [inw, hT], [pb])
                for dd in range(PAD1A):
                    kb.op('pe', lambda e, dd=dd, f=f: e.matmul(padbank[:, 0:512], lhsT=inw[:, dd, 2048 + f * 128:2048 + (f + 1) * 128], rhs=hT[:, dd, :], start=True, stop=True), [inw, hT], [padbank])
                U = Us[f]
                kb.op('act', lambda e, U=U, pb=pb: e.copy(out=U[:, 3:515], in_=pb[:, 0:512]), [pb], [U])
                a = acc[f % 3]
                kb.op('act', lambda e, U=U, a=a, f=f: e.activation(out=a[:], in_=U[:, 0:512], func=AF.Identity, scale=cw[:, f, 0:1], bias=cbias[:, f:f + 1]), [U, cw, cbias], [a])
                for k in range(1, 4):
                    kb.op('dve', lambda e, U=U, a=a, f=f, k=k: e.scalar_tensor_tensor(out=a[:], in0=U[:, k:k + 512], scalar=cw[:, f, k:k + 1], in1=a[:], op0=ALU.mult, op1=ALU.add), [U, cw, a], [a])
                kb.op('dve', lambda e, U=U: e.tensor_copy(out=U[:, 0:3], in_=U[:, 512:515]), [U], [U])

                def _silu(ff, st=st):
                    a2 = acc[ff % 3]
                    xo = xbcf[ff % 5]
                    kb.op('act', lambda e, xo=xo, a2=a2: e.activation(out=xo[:], in_=a2[:], func=AF.Silu), [a2], [xo])
                    if ff >= 16:
                        dst = BTs if ff < 24 else CTs
                        g = (ff - 16) % 8
                        kb.dma('sp', dst.t[st * 4:(st + 1) * 4, :, g, :].rearrange('c n t -> n c t'), xo[:].rearrange('p (c t) -> p c t', t=128), reads=[xo], writes=[dst])

                def _tr(ff):
                    xo2 = xbcf[ff % 5]
                    pt = ptr[ff % 2]
                    for j in range(4):
                        kb.op('pe', lambda e, pt=pt, xo2=xo2, j=j: e.transpose(out=pt[:, j * 128:(j + 1) * 128], in_=xo2[:, j * 128:(j + 1) * 128], identity=ident[:]), [xo2, ident], [pt])
                    kb.op('act', lambda e, pt=pt, ff=ff: e.copy(out=XBo[:, :, ff * 128:(ff + 1) * 128], in_=pt[:, 0:512].rearrange('p (j c) -> p j c', c=128)), [pt], [XBo])
                if f >= 1:
                    _silu(f - 1)
                if 3 <= f < 27:
                    _tr(f - 3)
            _silu(31)
            kb.dma('sp', XB.t[st * 512:(st + 1) * 512, :].rearrange('(j p) c -> p j c', p=128), XBo[:], reads=[XBo], writes=[XB])
            for j in range(4):
                z = zo[j % 2]
                for q in range(4):
                    pb = kb.bank(kb.banks[0:5])
                    for dc in range(8):
                        kb.op('pe', lambda e, pb=pb, dc=dc, q=q, j=j: e.matmul(pb[:, 0:512], lhsT=hT[:, dc, j * 128:(j + 1) * 128], rhs=inw[:, dc, q * 512:(q + 1) * 512], start=(dc == 0), stop=(dc == 7)), [inw, hT], [pb])
                    kb.op('act', lambda e, pb=pb, z=z, q=q: e.activation(out=z[:, q * 512:(q + 1) * 512], in_=pb[:, 0:512], func=AF.Silu), [pb], [z])
                r0 = st * 512 + j * 128
                kb.dma('sp', ZS[r0:r0 + 128, :], z[:], reads=[z], writes=[ZS])
                pb = kb.bank(kb.banks[0:5])
                for dc in range(8):
                    kb.op('pe', lambda e, pb=pb, dc=dc, j=j: e.matmul(pb[:, 0:32], lhsT=hT[:, dc, j * 128:(j + 1) * 128], rhs=inw[:, dc, 6144:6176], start=(dc == 0), stop=(dc == 7)), [inw, hT], [pb])
                d = dtt[j % 2]
                kb.op('dve', lambda e, pb=pb, d=d: e.tensor_tensor(out=d[:], in0=pb[:, 0:32], in1=dtb[:], op=ALU.add), [pb, dtb], [d])
                kb.op('act', lambda e, d=d: e.activation(out=d[:], in_=d[:], func=AF.Exp), [d], [d])
                kb.op('act', lambda e, d=d: e.activation(out=d[:], in_=d[:], func=AF.Ln, bias=1.0), [d], [d])
                kb.dma('sp', DT[r0:r0 + 128, :], d[:], reads=[d], writes=[DT])
    kb.barrier()
    kb.new_epoch()
    with ExitStack() as sc:
        kb.scope = sc
        outw = kb.sb('outw', [128, 16, 1024], BF16)
        for ic in range(16):
            kb.dma('pool', outw[:, ic, :], W['out_w'][ic * 128:(ic + 1) * 128, :], reads=[W['out_w']], writes=[outw])
        gnb = bcast_load(kb, 'gnb', W['gate_norm'][0:1, :], 2048, W['gate_norm'])
        Abc = bcast_load(kb, 'Abc', W['A_log'][0:1, :], 32, W['A_log'])
        Dbc = bcast_load(kb, 'Dbc', W['D'][0:1, :], 32, W['D'])
        kb.op('act', lambda e: e.activation(out=Abc[:], in_=Abc[:], func=AF.Exp), [Abc], [Abc])
        kb.op('dve', lambda e: e.tensor_scalar(out=Abc[:], in0=Abc[:], scalar1=-1.0, scalar2=None, op0=ALU.mult), [Abc], [Abc])
        triu, gt, ones = C['c_triu'], C['c_gt'], C['c_ones']
        triu_f, ones_f = C['c_triu_f'], C['c_ones_f']
        xbt = [kb.sb(f'xbt{i}', [128, 3072], BF16) for i in range(2)]
        bt = [kb.sb(f'bt{i}', [128, 8, 128], BF16) for i in range(2)]
        ct = [kb.sb(f'ct{i}', [128, 8, 128], BF16) for i in range(2)]
        dtl = [kb.sb(f'dtl{i}', [128, 32], F32) for i in range(2)]
        zl = [kb.sb(f'zl{i}', [128, 2048], BF16) for i in range(2)]
        xl = [kb.sb(f'xl{i}', [128, 1024], F32) for i in range(2)]
        a_t = kb.sb('a_t', [128, 32], F32)
        acs = kb.sb('acs', [128, 32], F32)
        dte = kb.sb('dte', [128, 32], F32)
        cd = kb.sb('cd', [128, 32], F32)
        Xdt = kb.sb('Xdt', [128, 32, 64], BF16)
        Xd = kb.sb('Xd', [128, 32, 64], BF16)
        rhs_all = kb.sb('rhs_all', [128, 32, 128], BF16)
        cbTm = kb.sb('cbTm', [128, 8, 128], F32)
        LT = [kb.sb(f'LT{i}', [128, 4, 128], F32) for i in range(2)]
        decT = [kb.sb(f'decT{i}', [128, 4, 128], BF16) for i in range(2)]
        MT = [kb.sb(f'MT{i}', [128, 4, 128], BF16) for i in range(2)]
        CsT = [kb.sb(f'CsT{i}', [128, 4, 128], BF16) for i in range(2)]
        prevT = kb.sb('prevT', [128, 32, 64], F32)
        prevB = kb.sb('prevB', [128, 32, 64], BF16)
        kb.op('dve', lambda e: e.memset(prevT[:], 0.0), [], [prevT])
        kb.op('dve', lambda e: e.memset(prevB[:], 0.0), [], [prevB])
        t1s = [kb.sb(f't1_{i}', [128, 2048], F32) for i in range(2)]
        y2 = kb.sb('y2', [128, 2048], F32)
        sq = kb.sb('sq', [128, 2048], F32)
        ssq8 = kb.sb('ssq8', [128, 8], F32)
        y3 = kb.sb('y3', [128, 2048], BF16)
        ynT = kb.sb('ynT', [128, 16, 128], BF16)
        xo = [kb.sb(f'xo{i}', [128, 1024], F32) for i in range(2)]
        def genA(c):
            r0 = c * 128
            xb_, b_, c_, d_, z_, x_ = xbt[c % 2], bt[c % 2], ct[c % 2], dtl[c % 2], zl[c % 2], xl[c % 2]
            kb.dma('sp', xb_[:], XB[r0:r0 + 128, :], reads=[XB], writes=[xb_])
            kb.dma('sp', b_[:], BTs[c], reads=[BTs], writes=[b_])
            kb.dma('sp', c_[:], CTs[c], reads=[CTs], writes=[c_])
            kb.dma('sp', d_[:], DT[r0:r0 + 128, :], reads=[DT], writes=[d_])
            kb.dma('sp', z_[:], ZS[r0:r0 + 128, :], reads=[ZS], writes=[z_])
            kb.dma('sp', x_[:], x_d[r0:r0 + 128, :], reads=[x_d], writes=[x_])
            yield
            kb.op('dve', lambda e, d_=d_: e.tensor_tensor(out=a_t[:], in0=d_[:], in1=Abc[:], op=ALU.mult), [d_, Abc], [a_t])
            pb = kb.bank(kb.banks[4:6])
            kb.op('pe', lambda e, pb=pb: e.matmul(pb[:, 0:32], lhsT=triu_f[:], rhs=a_t[:], start=True, stop=True), [triu_f, a_t], [pb])
            kb.op('pe', lambda e, pb=pb: e.matmul(pb[:, 32:64], lhsT=ones_f[:], rhs=a_t[:], start=True, stop=True), [ones_f, a_t], [pb])
            kb.op('act', lambda e, pb=pb: e.copy(out=acs[:], in_=pb[:, 0:32]), [pb], [acs])
            kb.op('dve', lambda e, pb=pb: e.tensor_tensor(out=dte[:], in0=pb[:, 32:64], in1=acs[:], op=ALU.subtract), [pb, acs], [dte])
            kb.op('act', lambda e: e.activation(out=dte[:], in_=dte[:], func=AF.Exp), [dte], [dte])
            kb.op('act', lambda e, pb=pb: e.activation(out=cd[:], in_=pb[:, 32:64], func=AF.Exp), [pb], [cd])
            yield
            xs3 = xb_[:, 0:2048].rearrange('p (h d) -> p h d', d=64)
            kb.op('dve', lambda e, xs3=xs3, d_=d_: e.tensor_tensor(out=Xdt[:], in0=xs3, in1=d_[:].unsqueeze(2).to_broadcast([128, 32, 64]), op=ALU.mult), [xb_, d_], [Xdt])
            kb.op('dve', lambda e: e.tensor_tensor(out=Xd[:], in0=Xdt[:], in1=dte[:].unsqueeze(2).to_broadcast([128, 32, 64]), op=ALU.mult), [Xdt, dte], [Xd])
            yield
            for h_ in range(32):
                if h_ % 8 == 7:
                    yield
                kb.op('act', lambda e, h_=h_: e.activation(out=rhs_all[:, h_, :], in_=triu_f[:], func=AF.Copy, scale=a_t[:, h_:h_ + 1]), [a_t, triu_f], [rhs_all], nosync=(h_ > 0))
            for half in range(2):
                pb = kb.bank(kb.banks[4:6])
                for gg in range(4):
                    g = half * 4 + gg
                    kb.op('pe', lambda e, pb=pb, g=g, gg=gg, b_=b_, c_=c_: e.matmul(pb[:, gg * 128:(gg + 1) * 128], lhsT=b_[:, g, :], rhs=c_[:, g, :], start=True, stop=True), [b_, c_], [pb])
                kb.op('dve', lambda e, pb=pb, half=half: e.tensor_tensor(out=cbTm[:, half * 4:(half + 1) * 4, :], in0=pb[:, 0:512].rearrange('p (g l) -> p g l', l=128), in1=triu_f[:].unsqueeze(1).to_broadcast([128, 4, 128]), op=ALU.mult), [pb, triu_f], [cbTm])
            yield
            ybanks = kb.banks[0:4]
            sdb = [kb.banks[4], kb.banks[5], kb.xbanks[0], kb.xbanks[1]]

            def _segdec(hg):
                rh = rhs_all[:, hg * 4:(hg + 1) * 4, :].rearrange('p h l -> p (h l)')
                pseg = sdb[(hg % 2) * 2]
                pdec = sdb[(hg % 2) * 2 + 1]
                kb.op('pe', lambda e, pseg=pseg, rh=rh: e.matmul(pseg[:, 0:512], lhsT=gt[:], rhs=rh, start=True, stop=True), [gt, rhs_all], [pseg])
                kb.op('pe', lambda e, pdec=pdec, rh=rh: e.matmul(pdec[:, 0:512], lhsT=ones[:], rhs=rh, start=True, stop=True), [ones, rhs_all], [pdec])
                lt, dc_, mt, cs = LT[hg % 2], decT[hg % 2], MT[hg % 2], CsT[hg % 2]
                g = hg
                kb.op('act', lambda e, pseg=pseg, lt=lt: e.activation(out=lt[:].rearrange('p h l -> p (h l)'), in_=pseg[:, 0:512], func=AF.Exp), [pseg], [lt])
                kb.op('act', lambda e, pdec=pdec, dc_=dc_: e.activation(out=dc_[:].rearrange('p h l -> p (h l)'), in_=pdec[:, 0:512], func=AF.Exp), [pdec], [dc_])
                kb.op('dve', lambda e, lt=lt, mt=mt, g=g: e.tensor_tensor(out=mt[:], in0=lt[:], in1=cbTm[:, g:g + 1, :].to_broadcast([128, 4, 128]), op=ALU.mult), [lt, cbTm], [mt])
                kb.op('dve', lambda e, dc_=dc_, cs=cs, g=g, c_=c_: e.tensor_tensor(out=cs[:], in0=dc_[:], in1=c_[:, g:g + 1, :].to_broadcast([128, 4, 128]), op=ALU.mult), [dc_, c_], [cs])
            _segdec(0)
            for hg in range(8):
                if hg + 1 < 8:
                    _segdec(hg + 1)
                mt, cs = MT[hg % 2], CsT[hg % 2]
                for hh in range(4):
                    h = hg * 4 + hh
                    yb = ybanks[h // 8]
                    col = (h % 8) * 64
                    kb.op('pe', lambda e, yb=yb, col=col, mt=mt, hh=hh, h=h: e.matmul(yb[:, col:col + 64], lhsT=mt[:, hh, :], rhs=Xdt[:, h, :], start=True, stop=False), [mt, Xdt], [yb])
                    kb.op('pe', lambda e, yb=yb, col=col, cs=cs, hh=hh, h=h: e.matmul(yb[:, col:col + 64], lhsT=cs[:, hh, :], rhs=prevB[:, h, :], start=False, stop=True), [cs, prevB], [yb])
                yield
            t1 = t1s[c % 2]
            kb.op('dve', lambda e, xs3=xs3: e.tensor_tensor(out=t1[:].rearrange('p (h d) -> p h d', d=64), in0=xs3, in1=Dbc[:].unsqueeze(2).to_broadcast([128, 32, 64]), op=ALU.mult), [xb_, Dbc], [t1])
            for q in range(4):
                kb.op('dve', lambda e, q=q, yb=ybanks[q]: e.tensor_tensor(out=t1[:, q * 512:(q + 1) * 512], in0=yb[:, 0:512], in1=t1[:, q * 512:(q + 1) * 512], op=ALU.add), [ybanks[q], t1], [t1])
            yield
            sbanks = kb.banks[0:4]
            for h in range(32):
                g = h // 4
                sbk = sbanks[h // 8]
                col = (h % 8) * 64
                kb.op('pe', lambda e, sbk=sbk, col=col, g=g, h=h, xb_=xb_: e.matmul(sbk[:, col:col + 64], lhsT=xb_[:, 2048 + g * 128:2048 + (g + 1) * 128], rhs=Xd[:, h, :], start=True, stop=True), [xb_, Xd], [sbk])
            yield
            kb.op('dve', lambda e: e.tensor_tensor(out=prevT[:], in0=prevT[:], in1=cd[:].unsqueeze(2).to_broadcast([128, 32, 64]), op=ALU.mult), [prevT, cd], [prevT])
            pf = prevT[:].rearrange('p h d -> p (h d)')
            for q in range(4):
                kb.op('dve', lambda e, q=q, sbk=sbanks[q], pf=pf: e.tensor_tensor(out=pf[:, q * 512:(q + 1) * 512], in0=sbk[:, 0:512], in1=pf[:, q * 512:(q + 1) * 512], op=ALU.add), [sbanks[q], prevT], [prevT])
            kb.op('act', lambda e: e.copy(out=prevB[:], in_=prevT[:]), [prevT], [prevB])
            yield

        def genB(c):
            r0 = c * 128
            z_, x_ = zl[c % 2], xl[c % 2]
            t1 = t1s[c % 2]
            kb.op('dve', lambda e, z_=z_: e.tensor_tensor(out=y2[:], in0=t1[:], in1=z_[:], op=ALU.mult), [t1, z_], [y2])
            yield
            for g_ in range(8):
                kb.op('act', lambda e, g_=g_: e.activation(out=sq[:, g_ * 256:(g_ + 1) * 256], in_=y2[:, g_ * 256:(g_ + 1) * 256], func=AF.Square, accum_out=ssq8[:, g_:g_ + 1]), [y2], [sq, ssq8], nosync=(g_ > 0))
            yield
            kb.op('dve', lambda e: e.tensor_scalar(out=ssq8[:], in0=ssq8[:], scalar1=1.0 / 256, scalar2=1e-5, op0=ALU.mult, op1=ALU.add), [ssq8], [ssq8])
            kb.op('act', lambda e: e.activation(out=ssq8[:], in_=ssq8[:], func=AF.Ln), [ssq8], [ssq8])
            kb.op('act', lambda e: e.activation(out=ssq8[:], in_=ssq8[:], func=AF.Exp, scale=-0.5), [ssq8], [ssq8])
            yield
            for g_ in range(8):
                kb.op('dve', lambda e, g_=g_: e.scalar_tensor_tensor(out=y3[:, g_ * 256:(g_ + 1) * 256], in0=y2[:, g_ * 256:(g_ + 1) * 256], scalar=ssq8[:, g_:g_ + 1], in1=gnb[:, g_ * 256:(g_ + 1) * 256], op0=ALU.mult, op1=ALU.mult), [y2, ssq8, gnb], [y3], nosync=(g_ > 0))
            yield
            for hf in range(2):
                pt = ptr[hf]
                for ic in range(8):
                    kb.op('pe', lambda e, pt=pt, ic=ic, hf=hf: e.transpose(out=pt[:, ic * 128:(ic + 1) * 128], in_=y3[:, (hf * 8 + ic) * 128:(hf * 8 + ic + 1) * 128], identity=ident[:]), [y3, ident], [pt])
                kb.op('act', lambda e, pt=pt, hf=hf: e.copy(out=ynT[:, hf * 8:(hf + 1) * 8, :], in_=pt[:, 0:1024].rearrange('p (c t) -> p c t', t=128)), [pt], [ynT])
            yield
            o = xo[c % 2]
            for hf in range(2):
                yield
                pb = kb.bank(kb.banks[4:6])
                for ic in range(16):
                    kb.op('pe', lambda e, pb=pb, ic=ic, hf=hf: e.matmul(pb[:, 0:512], lhsT=ynT[:, ic, :], rhs=outw[:, ic, hf * 512:(hf + 1) * 512], start=(ic == 0), stop=(ic == 15)), [ynT, outw], [pb])
                kb.op('dve', lambda e, pb=pb, hf=hf, o=o, x_=x_: e.tensor_tensor(out=o[:, hf * 512:(hf + 1) * 512], in0=pb[:, 0:512], in1=x_[:, hf * 512:(hf + 1) * 512], op=ALU.add), [pb, x_], [o])
            kb.dma('sp', x1_d[r0:r0 + 128, :], o[:], reads=[o], writes=[x1_d])

        def _adv(g):
            if g is None:
                return None
            try:
                next(g)
                return g
            except StopIteration:
                return None
        gA = genA(0)
        while gA is not None:
            gA = _adv(gA)
        for c in range(NCH):
            gB = genB(c)
            gA = genA(c + 1) if c + 1 < NCH else None
            k_ = 0
            while gA is not None or gB is not None:
                gA = _adv(gA)
                if k_ % 2 == 1 or gA is None:
                    gB = _adv(gB)
                k_ += 1
    kb.scope = None
    kb.barrier()
    kb.new_epoch()


def new_kb():
    nc = bass.Bass("TRN2", target_bir_lowering=False)
    kb = KB(nc)
    allb = [kb.ps(f'bank{i}', [128, 512], F32) for i in range(8)]
    kb.banks = allb[0:6]
    kb.xbanks = allb[6:8]
    kb.ptr = [Buf(b.t[:].bitcast(BF16), f'ptrv{i}', parent=b) for i, b in enumerate(kb.xbanks)]
    return nc, kb


def peer_convert(kb, W, L, lazy=False):
    U16 = kb.scratch(f'sc_U16_{L}', [128, 128, 1024], BF16)
    V16 = kb.scratch(f'sc_V16_{L}', [128, 128, 1024], BF16)

    def gen():
        for q in range(64):
            for (src, dst) in ((W['peer_uT'], U16), (W['peer_v'], V16)):
                kb.dma('pool', dst.t[q * 2:(q + 1) * 2].rearrange('c p n -> p c n'), src.t[q * 2:(q + 1) * 2].rearrange('c p n -> p c n'), reads=[src], writes=[dst], sempool='cv')
                yield
    g = gen()
    if lazy:
        return U16, V16, g
    for _ in g:
        pass
    return U16, V16


def phase_peer(kb, C, x_in, x_out, W, U16, V16, NT=256, nst=None, extra_gen=None):
    ident, ident_f = C['c_ident'], C['c_ident_f']
    iota = C['c_iota_f']
    iota_b = C['c_iota'] if OPT_BF16_ONEHOT else C['c_iota_f']
    ptr = kb.ptr
    nsub = NT // 128
    NST = (S // NT) if nst is None else nst
    with ExitStack() as sc:
        kb.scope = sc
        qw = kb.sb('qw', [128, 8, 2048], BF16)
        for dc in range(8):
            kb.dma('pool', qw[:, dc, :], W['peer_q_w'][dc * 128:(dc + 1) * 128, :], reads=[W['peer_q_w']], writes=[qw])
        skT = kb.sb('skT', [128, 2, 128], BF16)
        kb.dma('pool', skT[:], W['peer_skT'].t.rearrange('c d k -> d c k'), reads=[W['peer_skT']], writes=[skT])
        gbc = bcast_load(kb, 'gbcp', W['peer_norm'][0:1, :], 1024, W['peer_norm'])
        G = kb.sb('G', [128, 128, NT], BF16)
        uvb = [(kb.sb(f'ub{i}', [128, 2, 1024], BF16), kb.sb(f'vb{i}', [128, 2, 1024], BF16)) for i in range(3)]
        arA = kb.sb('arA', [128, 2048], F32)
        arB = kb.sb('arB', [128, 2048], F32)
        arC = kb.sb('arC', [128, 2048], F32)
        qT = kb.sb('qT', [128, 16, NT], BF16)
        hTs = [kb.sb(f'hTp{i}', [128, 8, NT], BF16) for i in range(2)]
        hb = [kb.sb(f'hbp{i}', [128, 1024], BF16) for i in range(1)]
        xt = [kb.sb(f'xtp{i}', [128, 1024], F32) for i in range(1)]
        ssq = kb.sb('ssqp', [128, 1], F32)
        rstd = kb.sb('rstdp', [128, 1], F32)
        v16 = kb.sb('v16', [128, 16, 16], F32)
        idx = kb.sb('idx', [128, 16, 16], U32)
        idxf = kb.sb('idxf', [128, 16, 16], F32)
        c8 = kb.sb('c8', [128, 8, 16], F32)
        pos = kb.sb('pos', [128, 8, 16], U32)
        posf = kb.sb('posf', [128, 8, 16], F32)
        r1f = kb.sb('r1f', [128, 8, 16], F32)
        r2f = kb.sb('r2f', [128, 8, 16], F32)
        ge = kb.sb('ge', [128, 8, 16], F32)
        zz = kb.sb('zz', [128, 8], F32)
        IJG = kb.sb('IJG', [128, 3, 128], F32)
        IJGT = [kb.sb(f'IJGT{i}', [128, 3, 128], BF16 if OPT_BF16_ONEHOT else F32) for i in range(nsub)]
        TB = OPT_TB
        XYb = [kb.sb(f'XYb{i}', [128, TB, 2, 128], BF16) for i in range(2)]
        gl = [kb.sb(f'gl{i}', [128, NT], BF16) for i in range(3)]
        wT = [kb.sb(f'wT{i}', [128, NT], BF16) for i in range(3)]
        thr = kb.sb('thr', [128, 16], F32)
        kb.op('dve', lambda e: e.tensor_scalar(out=thr[:], in0=iota[:, 0:16], scalar1=16.0, scalar2=16.0, op0=ALU.mult, op1=ALU.add), [iota], [thr])
        ob = kb.banks[0:4]
        aslots = [kb.banks[4], kb.banks[5], kb.xbanks[1]]
        rbank = kb.xbanks[0]
        LOOK = 2

        def routing(stile, qbank=None):
            t0 = stile * NT
            hT = hTs[stile % 2]
            for sub in range(nsub):
                xx = xt[0]
                r0 = t0 + sub * 128
                kb.dma('sp', xx[:], x_in[r0:r0 + 128, :], reads=[x_in], writes=[xx])
                h = hb[0]
                rmsnorm_bf(kb, xx, gbc, h, 1024, 1e-6, arC, ssq, rstd)
                yield
                transpose_to(kb, h, 1024, hT[:, :, sub * 128:(sub + 1) * 128], hT, ident, ptr[1])
                yield
            for fc in range(16):
                pb = rbank if qbank is None else qbank[fc % len(qbank)]
                for dc in range(8):
                    kb.op('pe', lambda e, pb=pb, dc=dc, fc=fc: e.matmul(pb[:, 0:NT], lhsT=qw[:, dc, fc * 128:(fc + 1) * 128], rhs=hT[:, dc, :], start=(dc == 0), stop=(dc == 7)), [qw, hT], [pb])
                kb.op('act', lambda e, pb=pb, fc=fc: e.copy(out=qT[:, fc, :], in_=pb[:, 0:NT]), [pb], [qT])
                yield
            for sub in range(nsub):
                s_sb = arA
                for q4 in range(4):
                    pb = rbank
                    for i4 in range(4):
                        fc = q4 * 4 + i4
                        kb.op('pe', lambda e, pb=pb, fc=fc, i4=i4, sub=sub: e.matmul(pb[:, i4 * 128:(i4 + 1) * 128], lhsT=qT[:, fc, sub * 128:(sub + 1) * 128], rhs=skT[:, fc % 2, :], start=True, stop=True), [qT, skT], [pb])
                    kb.op('act', lambda e, pb=pb, q4=q4: e.copy(out=s_sb[:, q4 * 512:(q4 + 1) * 512], in_=pb[:, 0:512]), [pb], [s_sb])
                    yield
                tmp = arC
                last = None
                for sg in range(16):
                    ss = s_sb[:, sg * 128:(sg + 1) * 128]
                    last = kb.op('dve', lambda e, ss=ss, sg=sg: e.max(out=v16[:, sg, 0:8], in_=ss), [s_sb], [v16], nosync=(sg > 0))
                    if sg % 4 == 3:
                        yield
                for sg in range(16):
                    ss = s_sb[:, sg * 128:(sg + 1) * 128]
                    t_ = kb.op('dve', lambda e, ss=ss, sg=sg: e.max_index(out=idx[:, sg, 0:8], in_max=v16[:, sg, 0:8], in_values=ss), [s_sb, v16], [idx], nosync=(sg > 0), after=([last] if sg == 0 else []))
                    if sg == 15:
                        last = t_
                    if sg % 4 == 3:
                        yield
                for sg in range(16):
                    ss = s_sb[:, sg * 128:(sg + 1) * 128]
                    t_ = kb.op('dve', lambda e, ss=ss, sg=sg: e.match_replace(out=tmp[:, sg * 128:(sg + 1) * 128], in_to_replace=v16[:, sg, 0:8], in_values=ss, imm_value=-1e30), [s_sb, v16], [tmp], nosync=(sg > 0), after=([last] if sg == 0 else []))
                    if sg == 15:
                        last = t_
                    if sg % 4 == 3:
                        yield
                for sg in range(16):
                    t_ = kb.op('dve', lambda e, sg=sg: e.max(out=v16[:, sg, 8:16], in_=tmp[:, sg * 128:(sg + 1) * 128]), [tmp], [v16], nosync=(sg > 0), after=([last] if sg == 0 else []))
                    if sg == 15:
                        last = t_
                    if sg % 4 == 3:
                        yield
                for sg in range(16):
                    t_ = kb.op('dve', lambda e, sg=sg: e.max_index(out=idx[:, sg, 8:16], in_max=v16[:, sg, 8:16], in_values=tmp[:, sg * 128:(sg + 1) * 128]), [tmp, v16], [idx], nosync=(sg > 0), after=([last] if sg == 0 else []))
                    if sg == 15:
                        last = t_
                    if sg % 4 == 3:
                        yield
                kb.op('dve', lambda e: e.tensor_copy(out=idxf[:], in_=idx[:]), [idx], [idxf])
                cand = arB
                v4 = v16[:].rearrange('p (h c) r -> p h c r', c=2)
                kb.op('dve', lambda e: e.tensor_tensor(out=cand[:].rearrange('p (h a b) -> p h a b', a=16, b=16), in0=v4[:, :, 0, :].unsqueeze(3).to_broadcast([128, 8, 16, 16]), in1=v4[:, :, 1, :].unsqueeze(2).to_broadcast([128, 8, 16, 16]), op=ALU.add), [v16], [cand])
                yield
                tmp2 = arA
                last = None
                for hh in range(8):
                    cs_ = cand[:, hh * 256:(hh + 1) * 256]
                    last = kb.op('dve', lambda e, cs_=cs_, hh=hh: e.max(out=c8[:, hh, 0:8], in_=cs_), [cand], [c8], nosync=(hh > 0))
                yield
                for hh in range(8):
                    cs_ = cand[:, hh * 256:(hh + 1) * 256]
                    t_ = kb.op('dve', lambda e, cs_=cs_, hh=hh: e.max_index(out=pos[:, hh, 0:8], in_max=c8[:, hh, 0:8], in_values=cs_), [cand, c8], [pos], nosync=(hh > 0), after=([last] if hh == 0 else []))
                    if hh == 7:
                        last = t_
                yield
                for hh in range(8):
                    cs_ = cand[:, hh * 256:(hh + 1) * 256]
                    t_ = kb.op('dve', lambda e, cs_=cs_, hh=hh: e.match_replace(out=tmp2[:, hh * 256:(hh + 1) * 256], in_to_replace=c8[:, hh, 0:8], in_values=cs_, imm_value=-1e30), [cand, c8], [tmp2], nosync=(hh > 0), after=([last] if hh == 0 else []))
                    if hh == 7:
                        last = t_
                yield
                for hh in range(8):
                    t_ = kb.op('dve', lambda e, hh=hh: e.max(out=c8[:, hh, 8:16], in_=tmp2[:, hh * 256:(hh + 1) * 256]), [tmp2], [c8], nosync=(hh > 0), after=([last] if hh == 0 else []))
                    if hh == 7:
                        last = t_
                yield
                for hh in range(8):
                    t_ = kb.op('dve', lambda e, hh=hh: e.max_index(out=pos[:, hh, 8:16], in_max=c8[:, hh, 8:16], in_values=tmp2[:, hh * 256:(hh + 1) * 256]), [tmp2, c8], [pos], nosync=(hh > 0), after=([last] if hh == 0 else []))
                    if hh == 7:
                        last = t_
                yield
                kb.op('dve', lambda e: e.tensor_tensor(out=ge[:], in0=c8[:], in1=c8[:, :, 0:1].to_broadcast([128, 8, 16]), op=ALU.subtract), [c8], [ge])
                kb.op('act', lambda e: e.activation(out=ge[:], in_=ge[:], func=AF.Exp), [ge], [ge])
                kb.op('dve', lambda e: e.tensor_reduce(out=zz[:], in_=ge[:], axis=AX.X, op=ALU.add), [ge], [zz])
                kb.op('dve', lambda e: e.reciprocal(out=zz[:], in_=zz[:]), [zz], [zz])
                kb.op('dve', lambda e: e.tensor_tensor(out=IJG[:, 2, :].rearrange('p (h k) -> p h k', k=16), in0=ge[:], in1=zz[:].unsqueeze(2).to_broadcast([128, 8, 16]), op=ALU.mult), [ge, zz], [IJG])
                yield
                kb.op('dve', lambda e: e.tensor_copy(out=posf[:], in_=pos[:]), [pos], [posf])
                kb.op('dve', lambda e: e.tensor_tensor(out=arA[:].rearrange('p (h k r) -> p h k r', k=16, r=16), in0=posf[:].unsqueeze(3).to_broadcast([128, 8, 16, 16]), in1=thr[:].unsqueeze(1).unsqueeze(1).to_broadcast([128, 8, 16, 16]), op=ALU.is_ge), [posf, thr], [arA])
                yield
                kb.op('dve', lambda e: e.tensor_reduce(out=r1f[:].rearrange('p h k -> p (h k)'), in_=arA[:].rearrange('p (m r) -> p m r', r=16), axis=AX.X, op=ALU.add), [arA], [r1f])
                kb.op('dve', lambda e: e.scalar_tensor_tensor(out=r2f[:], in0=r1f[:], scalar=-16.0, in1=posf[:], op0=ALU.mult, op1=ALU.add), [r1f, posf], [r2f])
                yield
                i4v = idxf[:].rearrange('p (h c) r -> p h c r', c=2)
                for which, rf in ((0, r1f), (1, r2f)):
                    eq = arA[:].rearrange('p (h k r) -> p h k r', k=16, r=16)
                    kb.op('dve', lambda e, rf=rf, eq=eq: e.tensor_tensor(out=eq, in0=iota[:, 0:16].unsqueeze(1).unsqueeze(1).to_broadcast([128, 8, 16, 16]), in1=rf[:].unsqueeze(3).to_broadcast([128, 8, 16, 16]), op=ALU.is_equal), [rf, iota], [arA])
                    yield
                    kb.op('dve', lambda e, eq=eq, which=which: e.tensor_tensor(out=arC[:].rearrange('p (h k r) -> p h k r', k=16, r=16), in0=eq, in1=i4v[:, :, which, :].unsqueeze(2).to_broadcast([128, 8, 16, 16]), op=ALU.mult), [arA, idxf], [arC])
                    yield
                    kb.op('dve', lambda e, which=which: e.tensor_reduce(out=IJG[:, which, :], in_=arC[:].rearrange('p (m r) -> p m r', r=16), axis=AX.X, op=ALU.add), [arC], [IJG])
                    yield
                pb = rbank
                for w3 in range(3):
                    kb.op('pe', lambda e, pb=pb, w3=w3: e.transpose(out=pb[:, w3 * 128:(w3 + 1) * 128], in_=IJG[:, w3, :], identity=ident_f[:]), [IJG, ident_f], [pb])
                ijgt = IJGT[sub]
                kb.op('act', lambda e, pb=pb, ijgt=ijgt: e.copy(out=ijgt[:].rearrange('p a t -> p (a t)'), in_=pb[:, 0:384]), [pb], [ijgt])
                yield

        def ggen(stile, gen=None):
            pbs = [rbank, kb.xbanks[1]]
            k = 0
            npumped = 0
            for sub in range(nsub):
                ijgt = IJGT[sub]
                for blk in range(128 // TB):
                    for _rep in range(TB // 8):
                        if extra_gen is not None and blk * (TB // 8) + _rep < 4:
                            try:
                                next(extra_gen)
                            except StopIteration:
                                pass
                        if gen is not None and npumped < 20:
                            npumped += 1
                            try:
                                next(gen)
                            except StopIteration:
                                pass
                    XY = XYb[blk % 2]
                    tb = blk * TB
                    kb.op('dve', lambda e, XY=XY, tb=tb, ijgt=ijgt: e.tensor_tensor(out=XY[:], in0=iota_b[:].unsqueeze(1).unsqueeze(1).to_broadcast([128, TB, 2, 128]), in1=ijgt[:, 0:2, tb:tb + TB].rearrange('p w t -> p t w').unsqueeze(3).to_broadcast([128, TB, 2, 128]), op=ALU.is_equal), [iota_b, ijgt], [XY], nosync=True)
                    kb.op('dve', lambda e, XY=XY, tb=tb, ijgt=ijgt: e.tensor_tensor(out=XY[:, :, 1, :], in0=XY[:, :, 1, :], in1=ijgt[:, 2, tb:tb + TB].unsqueeze(2).to_broadcast([128, TB, 128]), op=ALU.mult), [XY, ijgt], [XY])
                    for t4 in range(TB // 4):
                        pb = pbs[k % 2]
                        k += 1
                        for ti in range(4):
                            t = t4 * 4 + ti
                            kb.op('pe', lambda e, pb=pb, ti=ti, t=t, XY=XY: e.matmul(pb[:, ti * 128:(ti + 1) * 128], lhsT=XY[:, t, 1, :], rhs=XY[:, t, 0, :], start=True, stop=True), [XY], [pb])
                        tok = sub * 128 + tb + t4 * 4
                        kb.op('act', lambda e, pb=pb, tok=tok: e.copy(out=G[:, :, tok:tok + 4], in_=pb[:, 0:512].rearrange('p (t i) -> p i t', i=128)), [pb], [G])

        def stageF(stile, gen):
            hT = hTs[stile % 2]

            def pump(n):
                if gen is None:
                    return
                for _ in range(n):
                    try:
                        next(gen)
                    except StopIteration:
                        return

            def load(cg):
                ub, vb = uvb[cg % 3]
                kb.dma('sp', ub[:], U16.t[cg * 2:(cg + 1) * 2].rearrange('c p n -> p c n'), reads=[U16], writes=[ub])
                kb.dma('sp', vb[:], V16.t[cg * 2:(cg + 1) * 2].rearrange('c p n -> p c n'), reads=[V16], writes=[vb])

            def emit_a(c):
                ub, vb = uvb[(c // 2) % 3]
                pa = aslots[c % 3]
                ci = c % 2
                for dc in range(8):
                    kb.op('pe', lambda e, pa=pa, dc=dc, ci=ci, ub=ub: e.matmul(pa[:, 0:NT], lhsT=ub[:, ci, dc * 128:(dc + 1) * 128], rhs=hT[:, dc, :], start=(dc == 0), stop=(dc == 7)), [ub, hT], [pa])
                g_ = gl[c % 3]
                w_ = wT[c % 3]
                kb.op('act', lambda e, pa=pa, g_=g_: e.activation(out=g_[:], in_=pa[:, 0:NT], func=AF.Gelu), [pa], [g_])
                kb.op('pool' if (OPT_POOLMULT and c % OPT_POOLMULT == OPT_POOLMULT - 1) else 'dve', lambda e, g_=g_, w_=w_, c=c: e.tensor_tensor(out=w_[:], in0=g_[:], in1=G[:, c, :], op=ALU.mult), [g_, G], [w_])

            def emit_s2(c):
                ub, vb = uvb[(c // 2) % 3]
                w_ = wT[c % 3]
                ci = c % 2
                for tt in range(nsub):
                    for dh in range(2):
                        o = ob[tt * 2 + dh]
                        kb.op('pe', lambda e, o=o, tt=tt, dh=dh, ci=ci, vb=vb, w_=w_, c=c: e.matmul(o[:, 0:512], lhsT=w_[:, tt * 128:(tt + 1) * 128], rhs=vb[:, ci, dh * 512:(dh + 1) * 512], start=(c == 0), stop=(c == 127)), [w_, vb], [o])
            load(0)
            load(1)
            for c0 in range(LOOK):
                emit_a(c0)
            for c in range(128):
                if c % 2 == 0 and c // 2 + 2 < 64:
                    load(c // 2 + 2)
                if c + LOOK < 128:
                    emit_a(c + LOOK)
                emit_s2(c)
                pump(OPT_PUMP)
            pump(100000)

        def epilogue(stile):
            t0 = stile * NT
            for sub in range(nsub):
                xx = xt[0]
                r0 = t0 + sub * 128
                kb.dma('sp', xx[:], x_in[r0:r0 + 128, :], reads=[x_in], writes=[xx])
                for dh in range(2):
                    kb.op('dve', lambda e, xx=xx, dh=dh, o=ob[sub * 2 + dh]: e.tensor_tensor(out=xx[:, dh * 512:(dh + 1) * 512], in0=o[:, 0:512], in1=xx[:, dh * 512:(dh + 1) * 512], op=ALU.add), [ob[sub * 2 + dh], xx], [xx])
                kb.dma('sp', x_out[r0:r0 + 128, :], xx[:], reads=[xx], writes=[x_out])

        for _ in routing(0):
            pass
        for stile in range(NST):
            gen = routing(stile + 1, qbank=[kb.banks[4], kb.banks[5]]) if stile + 1 < NST else None
            ggen(stile, gen)
            if OPT_PUMP < 0:
                stageF(stile, None)
                epilogue(stile)
                if gen is not None:
                    for _ in gen:
                        pass
            else:
                stageF(stile, gen)
                epilogue(stile)
        if extra_gen is not None:
            for _ in extra_gen:
                pass
    kb.scope = None
    kb.barrier()
    kb.new_epoch()


def _adv(g):
    if g is None:
        return None
    try:
        next(g)
        return g
    except StopIteration:
        return None


def run_pipelined(genF, genB, n, ratio=1):
    g = genF(0)
    while g is not None:
        g = _adv(g)
    for i in range(n):
        gB = genB(i)
        gF = genF(i + 1) if i + 1 < n else None
        k_ = 0
        while gF is not None or gB is not None:
            gB = _adv(gB)
            if k_ % ratio == ratio - 1 or gB is None:
                gF = _adv(gF)
            k_ += 1


def phase_ple(kb, C, x_in, x_out, p_d, W, kv_out=None, final=False):
    ident = C['c_ident']
    ptr = kb.ptr
    with ExitStack() as sc:
        kb.scope = sc
        gw = kb.sb('gw', [128, 8, 1024], BF16)
        for dc in range(8):
            kb.dma('pool', gw[:, dc, :], W['ple_gate_w'][dc * 128:(dc + 1) * 128, :], reads=[W['ple_gate_w']], writes=[gw])
        pw = kb.sb('pw', [128, 2, 1024], BF16)
        for kc in range(2):
            kb.dma('pool', pw[:, kc, :], W['ple_proj'][kc * 128:(kc + 1) * 128, :], reads=[W['ple_proj']], writes=[pw])
        gbc = bcast_load(kb, 'gbc_ple', W['ple_norm'][0:1, :], 1024, W['ple_norm'])
        if kv_out is not None:
            kvw = kb.sb('kvw', [128, 8, 256], BF16)
            for dc in range(8):
                kb.dma('pool', kvw[:, dc, :], W['kv_w'][dc * 128:(dc + 1) * 128, :], reads=[W['kv_w']], writes=[kvw])
            gkv = bcast_load(kb, 'gbc_kv', W['kv_norm'][0:1, :], 1024, W['kv_norm'])
            kvb = bcast_load(kb, 'kvb', W['kv_b'][0:1, :], 256, W['kv_b'])
        if final:
            gfin = bcast_load(kb, 'gbc_fin', W['final_norm'][0:1, :], 1024, W['final_norm'])
        xt = [kb.sb(f'xtl{i}', [128, 1024], F32) for i in range(3)]
        pt_ = [kb.sb(f'ptl{i}', [128, 256], F32) for i in range(2)]
        pb16 = [kb.sb(f'pb16{i}', [128, 256], BF16) for i in range(2)]
        pT = [kb.sb(f'pTl{i}', [128, 2, 128], BF16) for i in range(2)]
        hb = [kb.sb(f'hbl{i}', [128, 1024], BF16) for i in range(2)]
        hT = [kb.sb(f'hTl{i}', [128, 8, 128], BF16) for i in range(2)]
        junk = kb.sb('junkl', [128, 1024], F32)
        ssq = kb.sb('ssql', [128, 1], F32)
        rstd = kb.sb('rstdl', [128, 1], F32)
        hbK = kb.sb('hbK', [128, 1024], BF16)
        hTK = kb.sb('hTK', [128, 8, 128], BF16)
        junkK = kb.sb('junkK', [128, 1024], F32)
        ssqK = kb.sb('ssqK', [128, 1], F32)
        rstdK = kb.sb('rstdK', [128, 1], F32)
        sg = kb.sb('sgl', [128, 1024], F32)
        x2 = [kb.sb(f'x2l{i}', [128, 1024], F32) for i in range(2)]
        kvt = [kb.sb(f'kvt{i}', [128, 256], F32) for i in range(2)]
        of = [kb.sb(f'ofl{i}', [128, 1024], F32) for i in range(2)]
        rot = kb.banks[0:4]
        rotK = kb.banks[4:6]

        def genF(n):
            r0 = n * 128
            xx, pp = xt[n % 3], pt_[n % 2]
            kb.dma('sp', xx[:], x_in[r0:r0 + 128, :], reads=[x_in], writes=[xx])
            kb.dma('sp', pp[:], p_d[r0:r0 + 128, :], reads=[p_d], writes=[pp])
            yield
            kb.op('act', lambda e, pp=pp: e.copy(out=pb16[n % 2][:], in_=pp[:]), [pp], [pb16[n % 2]])
            kb.op('act', lambda e, xx=xx: e.activation(out=junk[:], in_=xx[:], func=AF.Square, accum_out=ssq[:, 0:1]), [xx], [junk, ssq])
            yield
            kb.op('dve', lambda e: e.tensor_scalar(out=rstd[:, 0:1], in0=ssq[:, 0:1], scalar1=1.0 / 1024, scalar2=1e-6, op0=ALU.mult, op1=ALU.add), [ssq], [rstd])
            kb.op('act', lambda e: e.activation(out=rstd[:, 0:1], in_=rstd[:, 0:1], func=AF.Ln), [rstd], [rstd])
            yield
            kb.op('act', lambda e: e.activation(out=rstd[:, 0:1], in_=rstd[:, 0:1], func=AF.Exp, scale=-0.5), [rstd], [rstd])
            h = hb[n % 2]
            kb.op('dve', lambda e, xx=xx, h=h: e.scalar_tensor_tensor(out=h[:], in0=xx[:], scalar=rstd[:, 0:1], in1=gbc[:], op0=ALU.mult, op1=ALU.mult), [xx, rstd, gbc], [h])
            yield
            transpose_to(kb, h, 1024, hT[n % 2][:], hT[n % 2], ident, ptr[0])
            yield
            transpose_to(kb, pb16[n % 2], 256, pT[n % 2][:], pT[n % 2], ident, ptr[1], evac='dve')
            yield

        def genB(n):
            r0 = n * 128
            xx, xo = xt[n % 3], x2[n % 2]
            hT_, pT_ = hT[n % 2], pT[n % 2]
            for hf in range(2):
                pg = kb.bank(rot)
                for dc in range(8):
                    kb.op('pe', lambda e, pg=pg, dc=dc, hf=hf: e.matmul(pg[:, 0:512], lhsT=hT_[:, dc, :], rhs=gw[:, dc, hf * 512:(hf + 1) * 512], start=(dc == 0), stop=(dc == 7)), [hT_, gw], [pg])
                kb.op('act', lambda e, pg=pg, hf=hf: e.activation(out=sg[:, hf * 512:(hf + 1) * 512], in_=pg[:, 0:512], func=AF.Exp, scale=-1.0), [pg], [sg])
                kb.op('dve', lambda e, hf=hf: e.tensor_scalar(out=sg[:, hf * 512:(hf + 1) * 512], in0=sg[:, hf * 512:(hf + 1) * 512], scalar1=1.0, scalar2=None, op0=ALU.add), [sg], [sg])
                kb.op('dve', lambda e, hf=hf: e.reciprocal(out=sg[:, hf * 512:(hf + 1) * 512], in_=sg[:, hf * 512:(hf + 1) * 512]), [sg], [sg])
                pq = kb.bank(rot)
                for kc in range(2):
                    kb.op('pe', lambda e, pq=pq, kc=kc, hf=hf: e.matmul(pq[:, 0:512], lhsT=pT_[:, kc, :], rhs=pw[:, kc, hf * 512:(hf + 1) * 512], start=(kc == 0), stop=(kc == 1)), [pT_, pw], [pq])
                yield
                kb.op('dve', lambda e, pq=pq, hf=hf: e.tensor_tensor(out=sg[:, hf * 512:(hf + 1) * 512], in0=pq[:, 0:512], in1=sg[:, hf * 512:(hf + 1) * 512], op=ALU.mult), [pq, sg], [sg])
                yield
            kb.op('dve', lambda e, xx=xx, xo=xo: e.tensor_tensor(out=xo[:], in0=xx[:], in1=sg[:], op=ALU.add), [xx, sg], [xo])
            yield
            if kv_out is not None:
                kb.op('act', lambda e, xo=xo: e.activation(out=junkK[:], in_=xo[:], func=AF.Square, accum_out=ssqK[:, 0:1]), [xo], [junkK, ssqK])
                yield
                kb.op('dve', lambda e: e.tensor_scalar(out=rstdK[:, 0:1], in0=ssqK[:, 0:1], scalar1=1.0 / 1024, scalar2=1e-6, op0=ALU.mult, op1=ALU.add), [ssqK], [rstdK])
                kb.op('act', lambda e: e.activation(out=rstdK[:, 0:1], in_=rstdK[:, 0:1], func=AF.Ln), [rstdK], [rstdK])
                yield
                kb.op('act', lambda e: e.activation(out=rstdK[:, 0:1], in_=rstdK[:, 0:1], func=AF.Exp, scale=-0.5), [rstdK], [rstdK])
                kb.op('dve', lambda e, xo=xo: e.scalar_tensor_tensor(out=hbK[:], in0=xo[:], scalar=rstdK[:, 0:1], in1=gkv[:], op0=ALU.mult, op1=ALU.mult), [xo, rstdK, gkv], [hbK])
                yield
                transpose_to(kb, hbK, 1024, hTK[:], hTK, ident, ptr[1])
                yield
                pk = kb.bank(rotK)
                for dc in range(8):
                    kb.op('pe', lambda e, pk=pk, dc=dc: e.matmul(pk[:, 0:256], lhsT=hTK[:, dc, :], rhs=kvw[:, dc, :], start=(dc == 0), stop=(dc == 7)), [hTK, kvw], [pk])
                kv = kvt[n % 2]
                kb.op('dve', lambda e, pk=pk, kv=kv: e.tensor_tensor(out=kv[:], in0=pk[:, 0:256], in1=kvb[:], op=ALU.add), [pk, kvb], [kv])
                kb.dma('sp', kv_out[r0:r0 + 128, :], kv[:], reads=[kv], writes=[kv_out])
                yield
            if final:
                o = of[n % 2]
                kb.op('act', lambda e, xo=xo: e.activation(out=junkK[:], in_=xo[:], func=AF.Square, accum_out=ssqK[:, 0:1]), [xo], [junkK, ssqK])
                yield
                kb.op('dve', lambda e: e.tensor_scalar(out=rstdK[:, 0:1], in0=ssqK[:, 0:1], scalar1=1.0 / 1024, scalar2=1e-6, op0=ALU.mult, op1=ALU.add), [ssqK], [rstdK])
                kb.op('act', lambda e: e.activation(out=rstdK[:, 0:1], in_=rstdK[:, 0:1], func=AF.Ln), [rstdK], [rstdK])
                yield
                kb.op('act', lambda e: e.activation(out=rstdK[:, 0:1], in_=rstdK[:, 0:1], func=AF.Exp, scale=-0.5), [rstdK], [rstdK])
                kb.op('dve', lambda e, xo=xo, o=o: e.scalar_tensor_tensor(out=o[:], in0=xo[:], scalar=rstdK[:, 0:1], in1=gfin[:], op0=ALU.mult, op1=ALU.mult), [xo, rstdK, gfin], [o])
                kb.dma('sp', x_out[r0:r0 + 128, :], o[:], reads=[o], writes=[x_out])
                yield
            else:
                kb.dma('sp', x_out[r0:r0 + 128, :], xo[:], reads=[xo], writes=[x_out])
                yield
        run_pipelined(genF, genB, NCH, ratio=2)
    kb.scope = None
    kb.barrier()
    kb.new_epoch()


MAGIC = 12582912.0


def phase_attn(kb, C, x_in, x_out, kv_d, pos_d, W):
    ident = C['c_ident']
    ptr = kb.ptr
    with ExitStack() as sc:
        kb.scope = sc
        qw = kb.sb('qwa', [128, 8, 1024], BF16)
        ow = kb.sb('owa', [128, 8, 1024], BF16)
        for dc in range(8):
            kb.dma('pool', qw[:, dc, :], W['q_w'][dc * 128:(dc + 1) * 128, :], reads=[W['q_w']], writes=[qw])
            kb.dma('pool', ow[:, dc, :], W['o_w'][dc * 128:(dc + 1) * 128, :], reads=[W['o_w']], writes=[ow])
        gbc = bcast_load(kb, 'gbc_at', W['attn_norm'][0:1, :], 1024, W['attn_norm'])
        qb = bcast_load(kb, 'qb_at', W['q_b'][0:1, :], 1024, W['q_b'])
        obb = bcast_load(kb, 'ob_at', W['o_b'][0:1, :], 1024, W['o_b'])
        esink = bcast_load(kb, 'esink', W['sinks'][0:1, :], 16, W['sinks'])
        kb.op('act', lambda e: e.activation(out=esink[:], in_=esink[:], func=AF.Exp), [esink], [esink])
        invf = bcast_load(kb, 'invf', W['c_invf'][0:1, :], 8, W['c_invf'])
        posi = kb.sb('posi', [128, 32], I32)
        kb.dma('sp', posi[:], pos_d[:, :], reads=[pos_d], writes=[posi])
        posf = kb.sb('posfa', [128, 32], F32)
        kb.op('dve', lambda e: e.tensor_copy(out=posf[:], in_=posi[:]), [posi], [posf])
        yy = kb.sb('yy', [128, 32, 8], F32)
        nn_ = kb.sb('nn_', [128, 32, 8], F32)
        sinT = kb.sb('sinT', [128, 32, 8], F32)
        cosT = kb.sb('cosT', [128, 32, 8], F32)
        kb.op('dve', lambda e: e.tensor_tensor(out=yy[:], in0=posf[:].unsqueeze(2).to_broadcast([128, 32, 8]), in1=invf[:].unsqueeze(1).to_broadcast([128, 32, 8]), op=ALU.mult), [posf, invf], [yy])
        for (dst, shift) in ((sinT, 0.0), (cosT, 0.25)):
            if shift != 0.0:
                kb.op('dve', lambda e, shift=shift: e.tensor_scalar(out=yy[:], in0=yy[:], scalar1=shift, scalar2=None, op0=ALU.add), [yy], [yy])
            kb.op('dve', lambda e: e.tensor_scalar(out=nn_[:], in0=yy[:], scalar1=MAGIC, scalar2=None, op0=ALU.add), [yy], [nn_])
            kb.op('dve', lambda e: e.tensor_scalar(out=nn_[:], in0=nn_[:], scalar1=-MAGIC, scalar2=None, op0=ALU.add), [nn_], [nn_])
            kb.op('dve', lambda e: e.tensor_tensor(out=nn_[:], in0=yy[:], in1=nn_[:], op=ALU.subtract), [yy, nn_], [nn_])
            kb.op('act', lambda e, dst=dst: e.activation(out=dst[:], in_=nn_[:], func=AF.Sin, scale=2.0 * np.pi * (1.0 - 1e-6)), [nn_], [dst])
        xt = [kb.sb(f'xta{i}', [128, 1024], F32) for i in range(3)]
        kvl = [kb.sb(f'kvl{i}', [128, 256], F32) for i in range(2)]
        hb = [kb.sb(f'hba{i}', [128, 1024], BF16) for i in range(2)]
        hT = [kb.sb(f'hTa{i}', [128, 8, 128], BF16) for i in range(2)]
        junk = kb.sb('junka', [128, 1024], F32)
        ssq = kb.sb('ssqa', [128, 1], F32)
        rstd = kb.sb('rstda', [128, 1], F32)
        q = kb.sb('qa', [128, 16, 64], F32)
        rq = [kb.sb(f'rq{i}', [128, 16, 8], F32) for i in range(4)]
        rk = [kb.sb(f'rk{i}', [128, 2, 8], F32) for i in range(4)]
        q16 = kb.sb('q16', [128, 1024], BF16)
        qTz = kb.sb('qTz', [128, 16, 128], BF16)
        kb.op('dve', lambda e: e.memset(qTz[:], 0.0), [], [qTz])
        kdup = kb.sb('kdup', [128, 256], BF16)
        kTd = [kb.sb(f'kTd{i}', [128, 2, 128], BF16) for i in range(3)]
        vaug = [kb.sb(f'vaug{i}', [128, 2, 65], BF16) for i in range(3)]
        for i in range(3):
            kb.op('dve', lambda e, i=i: e.memset(vaug[i][:], 1.0), [], [vaug[i]])
        eT = [kb.sb(f'eT{i}', [128, 4, 128], F32) for i in range(2)]
        pT = [kb.sb(f'pTa{i}', [128, 4, 128], BF16) for i in range(4)]
        den = kb.sb('den', [128, 16], F32)
        o16 = kb.sb('o16', [128, 16, 64], BF16)
        oT = kb.sb('oTa', [128, 8, 128], BF16)
        xo = [kb.sb(f'xoa{i}', [128, 1024], F32) for i in range(2)]
        masks = {0: C['c_gt_f'], 1: C['c_triu_f']}
        obk = kb.banks[0:4]
        rot = kb.banks[4:6]
        pcnt = [0]

        def rope(buf, view, nh, n, R):
            cb_ = cosT[:, n, :].unsqueeze(1).to_broadcast([128, nh, 8])
            sb_ = sinT[:, n, :].unsqueeze(1).to_broadcast([128, nh, 8])
            t1, t2 = view[:, :, 0:8], view[:, :, 8:16]
            ra, rb, rc, rd = R
            kb.op('dve', lambda e: e.tensor_tensor(out=ra[:, 0:nh, :], in0=t1, in1=cb_, op=ALU.mult), [buf, cosT], [ra])
            kb.op('dve', lambda e: e.tensor_tensor(out=rb[:, 0:nh, :], in0=t2, in1=sb_, op=ALU.mult), [buf, sinT], [rb], nosync=True)
            kb.op('dve', lambda e: e.tensor_tensor(out=rc[:, 0:nh, :], in0=t2, in1=cb_, op=ALU.mult), [buf, cosT], [rc], nosync=True)
            kb.op('dve', lambda e: e.tensor_tensor(out=rd[:, 0:nh, :], in0=t1, in1=sb_, op=ALU.mult), [buf, sinT], [rd], nosync=True)
            kb.op('dve', lambda e: e.tensor_tensor(out=t1, in0=ra[:, 0:nh, :], in1=rb[:, 0:nh, :], op=ALU.subtract), [ra, rb, rd], [buf])
            kb.op('dve', lambda e: e.tensor_tensor(out=t2, in0=rc[:, 0:nh, :], in1=rd[:, 0:nh, :], op=ALU.add), [rc, rd], [buf])

        def genF(n):
            r0 = n * 128
            xx, kv = xt[n % 3], kvl[n % 2]
            kb.dma('sp', xx[:], x_in[r0:r0 + 128, :], reads=[x_in], writes=[xx])
            kb.dma('sp', kv[:], kv_d[r0:r0 + 128, :], reads=[kv_d], writes=[kv])
            yield
            h = hb[n % 2]
            rmsnorm_bf(kb, xx, gbc, h, 1024, 1e-6, junk, ssq, rstd)
            yield
            transpose_to(kb, h, 1024, hT[n % 2][:], hT[n % 2], ident, ptr[0])
            yield
            kview = kv[:, 0:128].rearrange('p (g d) -> p g d', d=64)
            rope(kv, kview, 2, n, rk)
            yield
            for dup in range(2):
                kb.op('dve', lambda e, dup=dup, kview=kview: e.tensor_copy(out=kdup[:].rearrange('p (g two d) -> p g two d', two=2, d=64)[:, :, dup, :], in_=kview), [kv], [kdup])
            va = vaug[n % 3]
            kb.op('act', lambda e, va=va, kv=kv: e.copy(out=va[:, :, 0:64], in_=kv[:, 128:256].rearrange('p (g d) -> p g d', d=64)), [kv], [va])
            yield
            kt = kTd[n % 3]
            transpose_to(kb, kdup, 256, kt[:], kt, ident, ptr[0], evac='dve')
            yield

        def genB(n):
            r0 = n * 128
            xx = xt[n % 3]
            hT_ = hT[n % 2]
            qf = q[:].rearrange('p h d -> p (h d)')
            for hf in range(2):
                pq = kb.bank(rot)
                for dc in range(8):
                    kb.op('pe', lambda e, pq=pq, dc=dc, hf=hf: e.matmul(pq[:, 0:512], lhsT=hT_[:, dc, :], rhs=qw[:, dc, hf * 512:(hf + 1) * 512], start=(dc == 0), stop=(dc == 7)), [hT_, qw], [pq])
                kb.op('dve', lambda e, pq=pq, hf=hf: e.tensor_tensor(out=qf[:, hf * 512:(hf + 1) * 512], in0=pq[:, 0:512], in1=qb[:, hf * 512:(hf + 1) * 512], op=ALU.add), [pq, qb], [q])
                yield
            rope(q, q[:], 16, n, rq)
            yield
            kb.op('act', lambda e: e.activation(out=q16[:], in_=qf, func=AF.Copy, scale=0.125), [q], [q16])
            pt = ptr[1]
            for c in range(8):
                kb.op('pe', lambda e, c=c, pt=pt: e.transpose(out=pt[:, c * 128:(c + 1) * 128], in_=q16[:, c * 128:(c + 1) * 128], identity=ident[:]), [q16, ident], [pt])
            for par in range(2):
                src = pt[par * 64:(par + 1) * 64, 0:1024].rearrange('p (c t) -> p c t', t=128)
                dstv = qTz[par * 64:(par + 1) * 64, :, :].rearrange('p (c two) t -> p c two t', two=2)[:, :, par, :]
                kb.op('dve' if par == 0 else 'act', (lambda e, src=src, dstv=dstv: e.tensor_copy(out=dstv, in_=src)) if par == 0 else (lambda e, src=src, dstv=dstv: e.copy(out=dstv, in_=src)), [pt], [qTz])
            yield
            blocks = ([(kTd[(n - 1) % 3], vaug[(n - 1) % 3], 0)] if n > 0 else []) + [(kTd[n % 3], vaug[n % 3], 1)]
            def _scores(hg):
                pts = []
                for (kk, vv, mi) in blocks:
                    ps_ = kb.bank(rot)
                    for hh in range(4):
                        h = hg * 4 + hh
                        g = h // 8
                        kb.op('pe', lambda e, ps_=ps_, hh=hh, h=h, g=g, kk=kk: e.matmul(ps_[:, hh * 128:(hh + 1) * 128], lhsT=kk[:, g, :], rhs=qTz[:, h, :], start=True, stop=True), [kk, qTz], [ps_])
                    et = eT[pcnt[0] % 2]
                    p_ = pT[pcnt[0] % 4]
                    pcnt[0] += 1
                    kb.op('act', lambda e, ps_=ps_, et=et: e.activation(out=et[:].rearrange('p h q -> p (h q)'), in_=ps_[:, 0:512], func=AF.Exp), [ps_], [et])
                    kb.op('dve', lambda e, et=et, p_=p_, mi=mi: e.tensor_tensor(out=p_[:], in0=et[:], in1=masks[mi][:].unsqueeze(1).to_broadcast([128, 4, 128]), op=ALU.mult), [et, masks[mi]], [p_])
                    pts.append((p_, vv))
                return pts
            pts_next = _scores(0)
            yield
            for hg in range(4):
                pts = pts_next
                if hg + 1 < 4:
                    pts_next = _scores(hg + 1)
                for hh in range(4):
                    h = hg * 4 + hh
                    g = h // 8
                    for bi, (p_, vv) in enumerate(pts):
                        kb.op('pe', lambda e, hh=hh, g=g, p_=p_, vv=vv, bi=bi, hg=hg, nb=len(pts): e.matmul(obk[hg][:, hh * 65:(hh + 1) * 65], lhsT=p_[:, hh, :], rhs=vv[:, g, :], start=(bi == 0), stop=(bi == nb - 1)), [p_, vv], [obk[hg]])
                yield
            for hg in range(4):
                ov = obk[hg][:, 0:260].rearrange('p (h d) -> p h d', d=65)
                kb.op('dve', lambda e, ov=ov, hg=hg: e.tensor_tensor(out=den[:, hg * 4:(hg + 1) * 4], in0=ov[:, :, 64], in1=esink[:, hg * 4:(hg + 1) * 4], op=ALU.add), [obk[hg], esink], [den], nosync=(hg > 0))
            kb.op('dve', lambda e: e.reciprocal(out=den[:], in_=den[:]), [den], [den])
            yield
            for hg in range(4):
                ov = obk[hg][:, 0:260].rearrange('p (h d) -> p h d', d=65)
                kb.op('dve', lambda e, ov=ov, hg=hg: e.tensor_tensor(out=o16[:, hg * 4:(hg + 1) * 4, :], in0=ov[:, :, 0:64], in1=den[:, hg * 4:(hg + 1) * 4].unsqueeze(2).to_broadcast([128, 4, 64]), op=ALU.mult), [obk[hg], den], [o16], nosync=(hg > 0))
            yield
            o16f = o16[:].rearrange('p h d -> p (h d)')
            pt = ptr[1]
            for c in range(8):
                kb.op('pe', lambda e, c=c, pt=pt, o16f=o16f: e.transpose(out=pt[:, c * 128:(c + 1) * 128], in_=o16f[:, c * 128:(c + 1) * 128], identity=ident[:]), [o16, ident], [pt])
            kb.op('act', lambda e, pt=pt: e.copy(out=oT[:], in_=pt[:, 0:1024].rearrange('p (c t) -> p c t', t=128)), [pt], [oT])
            yield
            o = xo[n % 2]
            for hf in range(2):
                po = kb.bank(rot)
                for ic in range(8):
                    kb.op('pe', lambda e, po=po, ic=ic, hf=hf: e.matmul(po[:, 0:512], lhsT=oT[:, ic, :], rhs=ow[:, ic, hf * 512:(hf + 1) * 512], start=(ic == 0), stop=(ic == 7)), [oT, ow], [po])
                kb.op('dve', lambda e, po=po, hf=hf, o=o: e.tensor_tensor(out=o[:, hf * 512:(hf + 1) * 512], in0=po[:, 0:512], in1=obb[:, hf * 512:(hf + 1) * 512], op=ALU.add), [po, obb], [o])
                yield
            kb.op('dve', lambda e, o=o, xx=xx: e.tensor_tensor(out=o[:], in0=o[:], in1=xx[:], op=ALU.add), [o, xx], [o])
            kb.dma('sp', x_out[r0:r0 + 128, :], o[:], reads=[o], writes=[x_out])
            yield
        run_pipelined(genF, genB, NCH, ratio=3)
    kb.scope = None
    kb.barrier()
    kb.new_epoch()


WSPEC = [
    ('in_w', [1024, 6176]), ('ssm_norm', [1, 1024]), ('dt_bias', [1, 32]), ('conv_wT', [4096, 4]), ('conv_b2', [128, 32]),
    ('out_w', [2048, 1024]), ('gate_norm', [1, 2048]), ('A_log', [1, 32]), ('D', [1, 32]),
    ('kv_w', [1024, 256]), ('kv_norm', [1, 1024]), ('kv_b', [1, 256]),
    ('q_w', [1024, 1024]), ('o_w', [1024, 1024]), ('attn_norm', [1, 1024]), ('q_b', [1, 1024]), ('o_b', [1, 1024]), ('sinks', [1, 16]), ('c_invf', [1, 8]),
    ('final_norm', [1, 1024]),
]
LSPEC = [('peer_uT', [128, 128, 1024]), ('peer_v', [128, 128, 1024]), ('peer_q_w', [1024, 2048]), ('peer_skT', [2, 128, 128]), ('peer_norm', [1, 1024]),
         ('ple_gate_w', [1024, 1024]), ('ple_proj', [256, 1024]), ('ple_norm', [1, 1024])]


def build_all():
    nc, kb = new_kb()
    C = load_consts(kb)
    x_d = kb.dram('x', [S, 1024], F32, "ExternalInput")
    p0_d = kb.dram('p0', [S, 256], F32, "ExternalInput")
    p1_d = kb.dram('p1', [S, 256], F32, "ExternalInput")
    pos_d = kb.dram('pos', [128, 32], I32, "ExternalInput")
    out_d = kb.dram('out', [S, 1024], F32, "ExternalOutput")
    W = {}
    for name, shape in WSPEC:
        W[name] = kb.dram('w_' + name, shape, F32, "ExternalInput")
    WL = [{}, {}]
    for L in range(2):
        for name, shape in LSPEC:
            WL[L][name] = kb.dram(f'w{L}_' + name, shape, F32, "ExternalInput")
    xs = [kb.scratch(f'sc_x{i}', [S, 1024], F32) for i in range(6)]
    kv_d = kb.scratch('sc_kv', [S, 256], F32)
    UV = []

    lazy1 = []

    def _conv():
        UV.append(peer_convert(kb, WL[0], 0))
        u1, v1, g1 = peer_convert(kb, WL[1], 1, lazy=True)
        UV.append((u1, v1))
        lazy1.append(g1)
    phase1(kb, C, x_d, xs[0], W, after_loads=_conv)
    phase_peer(kb, C, xs[0], xs[1], WL[0], UV[0][0], UV[0][1], extra_gen=lazy1[0])
    Wp = dict(W); Wp.update(WL[0])
    phase_ple(kb, C, xs[1], xs[2], p0_d, Wp, kv_out=kv_d)
    phase_attn(kb, C, xs[2], xs[3], kv_d, pos_d, W)
    phase_peer(kb, C, xs[3], xs[4], WL[1], UV[1][0], UV[1][1])
    Wp = dict(W); Wp.update(WL[1])
    phase_ple(kb, C, xs[4], out_d, p1_d, Wp, final=True)
    kb.finish([out_d.lw])
    kb.emit()
    return nc


def host_inputs(inp):
    f = np.float32
    shared = dict(make_consts())
    shared['w_in_w'] = np.ascontiguousarray(inp['ssm_in_w'][0], f)
    shared['w_ssm_norm'] = np.ascontiguousarray(inp['ssm_norm'], f).reshape(1, 1024)
    shared['w_dt_bias'] = np.ascontiguousarray(inp['ssm_dt_bias'], f).reshape(1, 32)
    shared['w_conv_wT'] = np.ascontiguousarray(np.asarray(inp['ssm_conv_w'][0], f).T)
    shared['w_conv_b2'] = np.ascontiguousarray(np.asarray(inp['ssm_conv_b'][0], f).reshape(32, 128).T)
    shared['w_out_w'] = np.ascontiguousarray(inp['ssm_out_w'][0], f)
    shared['w_gate_norm'] = np.ascontiguousarray(inp['ssm_gate_norm'], f).reshape(1, 2048)
    shared['w_A_log'] = np.ascontiguousarray(inp['ssm_A_log'], f).reshape(1, 32)
    shared['w_D'] = np.ascontiguousarray(inp['ssm_D'], f).reshape(1, 32)
    shared['w_kv_w'] = np.ascontiguousarray(inp['kv_w'], f)
    shared['w_kv_norm'] = np.ascontiguousarray(inp['kv_norm'], f).reshape(1, 1024)
    shared['w_kv_b'] = np.ascontiguousarray(inp['kv_b'], f).reshape(1, 256)
    shared['w_q_w'] = np.ascontiguousarray(inp['q_w'][0], f)
    shared['w_o_w'] = np.ascontiguousarray(inp['o_w'][0], f)
    shared['w_attn_norm'] = np.ascontiguousarray(inp['attn_norm'], f).reshape(1, 1024)
    shared['w_q_b'] = np.ascontiguousarray(inp['q_b'], f).reshape(1, 1024)
    shared['w_o_b'] = np.ascontiguousarray(inp['o_b'], f).reshape(1, 1024)
    shared['w_sinks'] = np.ascontiguousarray(inp['sinks'], f).reshape(1, 16)
    shared['w_c_invf'] = (np.power(500000.0, -np.arange(0, 16, 2, dtype=np.float32) / 16) / (2 * np.pi)).astype(f)[None]
    shared['w_final_norm'] = np.ascontiguousarray(inp['final_norm'], f).reshape(1, 1024)
    for L in range(2):
        u = np.asarray(inp['peer_u'][L], f)
        shared[f'w{L}_peer_uT'] = np.ascontiguousarray(u.reshape(128, 128, 8, 128).transpose(0, 3, 2, 1)).reshape(128, 128, 1024)
        shared[f'w{L}_peer_v'] = np.ascontiguousarray(np.asarray(inp['peer_v'][L], f).reshape(128, 128, 1024))
        shared[f'w{L}_peer_q_w'] = np.ascontiguousarray(inp['peer_q_w'][L], f)
        shared[f'w{L}_peer_skT'] = np.ascontiguousarray(np.asarray(inp['peer_sub_keys'][L], f).transpose(0, 2, 1))
        shared[f'w{L}_peer_norm'] = np.ascontiguousarray(inp['peer_norm'][L], f).reshape(1, 1024)
        shared[f'w{L}_ple_gate_w'] = np.ascontiguousarray(inp['ple_gate_w'][L], f)
        shared[f'w{L}_ple_proj'] = np.ascontiguousarray(inp['ple_proj'][L], f)
        shared[f'w{L}_ple_norm'] = np.ascontiguousarray(inp['ple_norm'][L], f).reshape(1, 1024)
    maps = []
    for b in range(8):
        m = dict(shared)
        m['x'] = np.ascontiguousarray(inp['x'][b], f)
        m['p0'] = np.ascontiguousarray(inp['p'][0, b], f)
        m['p1'] = np.ascontiguousarray(inp['p'][1, b], f)
        m['pos'] = np.ascontiguousarray(np.asarray(inp['positions'][b], np.int32).reshape(32, 128).T)
        maps.append(m)
    return maps


_NC = None


def kernel(**inputs):
    global _NC
    inp = {k: np.asarray(v) for k, v in inputs.items()}
    if _NC is None:
        _NC = build_all()
    maps = host_inputs(inp)
    res = run_bass_kernel_spmd(_NC, maps, core_ids=list(range(8)))
    out = np.stack([np.asarray(r['out'], np.float32) for r in res.results], axis=0)
    return out
```

```python
import numpy as np
from contextlib import ExitStack
import concourse.bass as bass
import concourse.mybir as mybir
from concourse.bass_utils import run_bass_kernel_spmd

F32 = mybir.dt.float32
BF16 = mybir.dt.bfloat16
I32 = mybir.dt.int32
U32 = mybir.dt.uint32
AF = mybir.ActivationFunctionType
ALU = mybir.AluOpType
AX = mybir.AxisListType

SYNC_SAME_ENGINE = True
OPT_BF16_ONEHOT = False
OPT_NOSYNC = True
OPT_PUMP = 1
OPT_TB = 16
OPT_POOLMULT = 0
PAD1A = 3
DEBUG = False
S = 4096
NCH = 32


class Buf:
    def __init__(self, t, name, parent=None):
        self.t = t
        self.name = name
        self.parent = parent
        self._lw = None
        self._rd = []

    @property
    def lw(self):
        return self.parent.lw if self.parent is not None else self._lw

    @lw.setter
    def lw(self, v):
        if self.parent is not None:
            self.parent.lw = v
        else:
            self._lw = v

    @property
    def rd(self):
        return self.parent.rd if self.parent is not None else self._rd

    @rd.setter
    def rd(self, v):
        if self.parent is not None:
            self.parent.rd = v
        else:
            self._rd = v

    def __getitem__(self, idx):
        return self.t[idx]


class KB:
    ENG = ('pe', 'act', 'dve', 'pool', 'sp')

    def __init__(self, nc, n_dma_sems=16):
        self.nc = nc
        self.es = ExitStack()
        self.ops = {e: [] for e in self.ENG}
        self.cnt = {e: 0 for e in self.ENG}
        self.sem = {}
        self.ekey = {}
        self.epoch = 0
        for e in ('pe', 'act', 'dve', 'pool'):
            self.ekey[e] = e + '@0'
            self.sem[self.ekey[e]] = self.es.enter_context(nc.semaphore('s_' + e + '0'))
        self.ekey['sp'] = 'sp@0'
        self.dma_sems = []
        self.dma_pool = {'hw': [], 'sw': []}
        self.dma_pool['cv'] = []
        for i in range(n_dma_sems + 8 + 4):
            s = self.es.enter_context(nc.semaphore(f'd{i}'))
            self.sem[f'd{i}'] = s
            ent = [f'd{i}', 0]
            pk = 'hw' if i < n_dma_sems else ('sw' if i < n_dma_sems + 8 else 'cv')
            if pk != 'cv':
                self.dma_sems.append(ent)
            self.dma_pool[pk].append(ent)
        self.dma_rr = {'hw': 0, 'sw': 0, 'cv': 0}
        self.waited = {e: {} for e in self.ENG}
        self.final_tokens = []
        self.banks = []
        self.bank_rr = 0
        self.scope = None
        self.pending = {e: [] for e in self.ENG}

    def new_epoch(self):
        self.epoch += 1
        for e in ('pe', 'act', 'dve', 'pool'):
            self.ekey[e] = f'{e}@{self.epoch}'
            self.sem[self.ekey[e]] = self.es.enter_context(self.nc.semaphore(f's_{e}{self.epoch}'))
            self.cnt[e] = 0

    def barrier(self):
        cur = {self.ekey[e]: self.cnt[e] for e in ('pe', 'act', 'dve', 'pool')}
        for k, v in self.dma_sems:
            cur[k] = v
        for e in self.ENG:
            for k, v in cur.items():
                if v > 0 and self.waited[e].get(k, 0) < v:
                    self.waited[e][k] = v
                    self.pending[e].append((k, v))

    def sb(self, name, shape, dtype):
        es = self.scope if self.scope is not None else self.es
        self.uid = getattr(self, 'uid', 0) + 1
        name = f'{name}_u{self.uid}'
        t = es.enter_context(self.nc.sbuf_tensor(name, list(shape), dtype))
        return Buf(t, name)

    def ps(self, name, shape, dtype):
        t = self.es.enter_context(self.nc.psum_tensor(name, list(shape), dtype))
        return Buf(t, name)

    def dram(self, name, shape, dtype, kind):
        t = self.nc.dram_tensor(name, list(shape), dtype, kind=kind)
        return Buf(t.ap(), name)

    def scratch(self, name, shape, dtype):
        return self.dram(name, shape, dtype, "ExternalOutput" if DEBUG else "Internal")

    def dbg(self, name, buf, shape, dtype):
        if not DEBUG:
            return
        d = self.dram('dbg_' + name, shape, dtype, "ExternalOutput")
        self.dma('sp', d[:], buf[:], reads=[buf], writes=[d])

    def bank(self, pool=None):
        bs = self.banks if pool is None else pool
        b = bs[self.bank_rr % len(bs)]
        self.bank_rr += 1
        return b

    def _deps(self, eng, reads, writes, nosync=False, after=()):
        toks = {}

        def add(tok, force=False):
            if tok is None:
                return
            k, v = tok
            if k == self.ekey[eng] and not force and (nosync or not (SYNC_SAME_ENGINE and eng in ('act', 'dve', 'pool'))):
                return
            if toks.get(k, 0) < v:
                toks[k] = v
        for b in reads:
            add(b.lw)
        for b in writes:
            add(b.lw)
            for r in b.rd:
                add(r)
        for t in after:
            add(t, True)
        w = self.waited[eng]
        out = []
        for k, v in toks.items():
            if w.get(k, 0) < v:
                w[k] = v
                out.append((k, v))
        return out

    def _commit(self, tok, reads, writes):
        for b in reads:
            b.rd.append(tok)
            if len(b.rd) > 32:
                m = {}
                for k, v in b.rd:
                    m[k] = max(m.get(k, 0), v)
                b.rd = list(m.items())
        for b in writes:
            b.lw = tok
            b.rd = []

    def op(self, eng, fn, reads=(), writes=(), nosync=False, after=()):
        waits = self.pending[eng] + self._deps(eng, reads, writes, nosync and OPT_NOSYNC, after)
        self.pending[eng] = []
        self.cnt[eng] += 1
        tok = (self.ekey[eng], self.cnt[eng])
        self.ops[eng].append((waits, fn, (self.ekey[eng], 1)))
        self._commit(tok, reads, writes)
        return tok

    def dma(self, q, out_ap, in_ap, reads=(), writes=(), **kw):
        pk = kw.pop('sempool', None) or ('sw' if q == 'pool' else 'hw')
        pl = self.dma_pool[pk]
        ent = pl[self.dma_rr[pk] % len(pl)]
        self.dma_rr[pk] += 1
        key = ent[0]
        waits = self.pending[q] + self._deps(q, reads, writes)
        self.pending[q] = []
        if ent[1] > 0 and self.waited[q].get(key, 0) < ent[1]:
            self.waited[q][key] = ent[1]
            waits.append((key, ent[1]))
        ent[1] += 16
        tok = (key, ent[1])

        def fn(e, out_ap=out_ap, in_ap=in_ap, kw=kw):
            return e.dma_start(out=out_ap, in_=in_ap, **kw)
        self.ops[q].append((waits, fn, (key, 16)))
        self._commit(tok, reads, writes)
        return tok

    def finish(self, tokens):
        self.final_tokens = list(tokens)

    def emit(self):
        nc = self.nc
        with nc.Block() as block:
            def body(ename):
                def _f(e):
                    for waits, fn, (sk, inc) in self.ops[ename]:
                        for k, v in waits:
                            e.wait_ge(self.sem[k], v)
                        ins = fn(e)
                        ins.then_inc(self.sem[sk], inc)
                    if ename == 'sp':
                        for k, v in self.final_tokens:
                            e.wait_ge(self.sem[k], v)
                return _f
            block.tensor(body('pe'))
            block.scalar(body('act'))
            block.vector(body('dve'))
            block.gpsimd(body('pool'))
            block.sync(body('sp'))
        self.es.close()


def make_consts():
    k = np.arange(128)
    c = {}
    c['c_ident'] = np.eye(128, dtype=np.float32)
    c['c_triu'] = (k[:, None] <= k[None, :]).astype(np.float32)
    c['c_gt'] = (k[:, None] > k[None, :]).astype(np.float32)
    c['c_ones'] = np.ones((128, 128), np.float32)
    c['c_iota'] = np.broadcast_to(k[None, :].astype(np.float32), (128, 128)).copy()
    return c


def load_consts(kb, names=('c_ident', 'c_triu', 'c_gt', 'c_ones', 'c_iota')):
    C = {}
    for n in names:
        d = kb.dram(n, [128, 128], F32, "ExternalInput")
        sb = kb.sb(n + '_bf', [128, 128], BF16)
        kb.dma('pool', sb[:], d[:], reads=[d], writes=[sb])
        C[n] = sb
        sf = kb.sb(n + '_f', [128, 128], F32)
        kb.dma('sp', sf[:], d[:], reads=[d], writes=[sf])
        C[n + '_f'] = sf
    return C


def bcast_load(kb, name, dram_ap_1xn, n, dbuf):
    sb = kb.sb(name, [128, n], F32)
    kb.dma('sp', sb[:], dram_ap_1xn.partition_broadcast(128), reads=[dbuf], writes=[sb])
    return sb


def rmsnorm_bf(kb, x, gbc, hb, D, eps, junk, ssq, rstd, eng2='dve'):
    kb.op('act', lambda e: e.activation(out=junk[:, 0:D], in_=x[:, 0:D], func=AF.Square, accum_out=ssq[:, 0:1]), [x], [junk, ssq])
    kb.op('dve', lambda e: e.tensor_scalar(out=rstd[:, 0:1], in0=ssq[:, 0:1], scalar1=1.0 / D, scalar2=eps, op0=ALU.mult, op1=ALU.add), [ssq], [rstd])
    kb.op('act', lambda e: e.activation(out=rstd[:, 0:1], in_=rstd[:, 0:1], func=AF.Ln), [rstd], [rstd])
    kb.op('act', lambda e: e.activation(out=rstd[:, 0:1], in_=rstd[:, 0:1], func=AF.Exp, scale=-0.5), [rstd], [rstd])
    kb.op(eng2, lambda e: e.scalar_tensor_tensor(out=hb[:, 0:D], in0=x[:, 0:D], scalar=rstd[:, 0:1], in1=gbc[:, 0:D], op0=ALU.mult, op1=ALU.mult), [x, rstd, gbc], [hb])


def transpose_to(kb, src, ncols, dst_ap, dst, ident, ptr, evac='act'):
    n = ncols // 128
    for c in range(n):
        kb.op('pe', lambda e, c=c: e.transpose(out=ptr[:, c * 128:(c + 1) * 128], in_=src[:, c * 128:(c + 1) * 128], identity=ident[:]), [src, ident], [ptr])
    pv = ptr[:, 0:ncols].rearrange('p (c t) -> p c t', t=128)
    if evac == 'act':
        kb.op('act', lambda e: e.copy(out=dst_ap, in_=pv), [ptr], [dst])
    else:
        kb.op(evac, lambda e: e.tensor_copy(out=dst_ap, in_=pv), [ptr], [dst])


def phase1(kb, C, x_d, x1_d, W, after_loads=None):
    nc = kb.nc
    XB = kb.scratch('sc_XB', [S, 3072], BF16)
    BTs = kb.scratch('sc_BT', [NCH, 128, 8, 128], BF16)
    CTs = kb.scratch('sc_CT', [NCH, 128, 8, 128], BF16)
    ZS = kb.scratch('sc_ZS', [S, 2048], BF16)
    DT = kb.scratch('sc_DT', [S, 32], F32)
    ident = C['c_ident']
    ptr = kb.ptr
    with ExitStack() as sc:
        kb.scope = sc
        inw = kb.sb('inw', [128, 8, 6176], BF16)
        padbank = kb.banks[5]
        for dc in range(8):
            kb.dma('pool', inw[:, dc, :], W['in_w'][dc * 128:(dc + 1) * 128, :], reads=[W['in_w']], writes=[inw])
        gbc = bcast_load(kb, 'gbc1', W['ssm_norm'][0:1, :], 1024, W['ssm_norm'])
        dtb = bcast_load(kb, 'dtb', W['dt_bias'][0:1, :], 32, W['dt_bias'])
        cw = kb.sb('cw', [128, 32, 4], F32)
        kb.dma('sp', cw[:], W['conv_wT'].t.rearrange('(f p) k -> p f k', p=128), reads=[W['conv_wT']], writes=[cw])
        cbias = kb.sb('cbias', [128, 32], F32)
        kb.dma('sp', cbias[:], W['conv_b2'][:, :], reads=[W['conv_b2']], writes=[cbias])
        Uall = kb.sb('Uall', [128, 32, 516], BF16)
        kb.op('dve', lambda e: e.memset(Uall[:], 0.0), [], [Uall])
        Us = [Buf(Uall.t[:, f, :], f'U{f}') for f in range(32)]
        for u_ in Us:
            u_.lw = Uall.lw
        xt = [kb.sb(f'xt{i}', [128, 1024], F32) for i in range(2)]
        junk = kb.sb('junk', [128, 1024], F32)
        ssq = kb.sb('ssq', [128, 1], F32)
        rstd = kb.sb('rstd', [128, 1], F32)
        hb = [kb.sb(f'hb{i}', [128, 1024], BF16) for i in range(2)]
        hT = kb.sb('hT', [128, 8, 512], BF16)
        acc = [kb.sb(f'acc{i}', [128, 512], F32) for i in range(3)]
        xbcf = [kb.sb(f'xbcf{i}', [128, 512], BF16) for i in range(5)]
        XBo = kb.sb('XBo', [128, 4, 3072], BF16)
        zo = [kb.sb(f'zo{i}', [128, 2048], BF16) for i in range(2)]
        dtt = [kb.sb(f'dtt{i}', [128, 32], F32) for i in range(2)]
        if after_loads is not None:
            after_loads()
        for st in range(8):
            for j in range(4):
                xx = xt[j % 2]
                r0 = st * 512 + j * 128
                kb.dma('sp', xx[:], x_d[r0:r0 + 128, :], reads=[x_d], writes=[xx])
                h = hb[j % 2]
                rmsnorm_bf(kb, xx, gbc, h, 1024, 1e-6, junk, ssq, rstd)
                transpose_to(kb, h, 1024, hT[:, :, j * 128:(j + 1) * 128], hT, ident, ptr[j % 2])
            for f in range(32):
                pb = kb.bank(kb.banks[0:5])
                for dc in range(8):
                    kb.op('pe', lambda e, pb=pb, dc=dc, f=f: e.matmul(pb[:, 0:512], lhsT=inw[:, dc, 2048 + f * 128:2048 + (f + 1) * 128], rhs=hT[:, dc, :], start=(dc == 0), stop=(dc == 7)), [inw, hT], [pb])
                for dd in range(PAD1A):
                    kb.op('pe', lambda e, dd=dd, f=f: e.matmul(padbank[:, 0:512], lhsT=inw[:, dd, 2048 + f * 128:2048 + (f + 1) * 128], rhs=hT[:, dd, :], start=True, stop=True), [inw, hT], [padbank])
                U = Us[f]
                kb.op('act', lambda e, U=U, pb=pb: e.copy(out=U[:, 3:515], in_=pb[:, 0:512]), [pb], [U])
                a = acc[f % 3]
                kb.op('act', lambda e, U=U, a=a, f=f: e.activation(out=a[:], in_=U[:, 0:512], func=AF.Identity, scale=cw[:, f, 0:1], bias=cbias[:, f:f + 1]), [U, cw, cbias], [a])
                for k in range(1, 4):
                    kb.op('dve', lambda e, U=U, a=a, f=f, k=k: e.scalar_tensor_tensor(out=a[:], in0=U[:, k:k + 512], scalar=cw[:, f, k:k + 1], in1=a[:], op0=ALU.mult, op1=ALU.add), [U, cw, a], [a])
                kb.op('dve', lambda e, U=U: e.tensor_copy(out=U[:, 0:3], in_=U[:, 512:515]), [U], [U])

                def _silu(ff, st=st):
                    a2 = acc[ff % 3]
                    xo = xbcf[ff % 5]
                    kb.op('act', lambda e, xo=xo, a2=a2: e.activation(out=xo[:], in_=a2[:], func=AF.Silu), [a2], [xo])
                    if ff >= 16:
                        dst = BTs if ff < 24 else CTs
                        g = (ff - 16) % 8
                        kb.dma('sp', dst.t[st * 4:(st + 1) * 4, :, g, :].rearrange('c n t -> n c t'), xo[:].rearrange('p (c t) -> p c t', t=128), reads=[xo], writes=[dst])

                def _tr(ff):
                    xo2 = xbcf[ff % 5]
                    pt = ptr[ff % 2]
                    for j in range(4):
                        kb.op('pe', lambda e, pt=pt, xo2=xo2, j=j: e.transpose(out=pt[:, j * 128:(j + 1) * 128], in_=xo2[:, j * 128:(j + 1) * 128], identity=ident[:]), [xo2, ident], [pt])
                    kb.op('act', lambda e, pt=pt, ff=ff: e.copy(out=XBo[:, :, ff * 128:(ff + 1) * 128], in_=pt[:, 0:512].rearrange('p (j c) -> p j c', c=128)), [pt], [XBo])
                if f >= 1:
                    _silu(f - 1)
                if 3 <= f < 27:
                    _tr(f - 3)
            _silu(31)
            kb.dma('sp', XB.t[st * 512:(st + 1) * 512, :].rearrange('(j p) c -> p j c', p=128), XBo[:], reads=[XBo], writes=[XB])
            for j in range(4):
                z = zo[j % 2]
                for q in range(4):
                    pb = kb.bank(kb.banks[0:5])
                    for dc in range(8):
                        kb.op('pe', lambda e, pb=pb, dc=dc, q=q, j=j: e.matmul(pb[:, 0:512], lhsT=hT[:, dc, j * 128:(j + 1) * 128], rhs=inw[:, dc, q * 512:(q + 1) * 512], start=(dc == 0), stop=(dc == 7)), [inw, hT], [pb])
                    kb.op('act', lambda e, pb=pb, z=z, q=q: e.activation(out=z[:, q * 512:(q + 1) * 512], in_=pb[:, 0:512], func=AF.Silu), [pb], [z])
                r0 = st * 512 + j * 128
                kb.dma('sp', ZS[r0:r0 + 128, :], z[:], reads=[z], writes=[ZS])
                pb = kb.bank(kb.banks[0:5])
                for dc in range(8):
                    kb.op('pe', lambda e, pb=pb, dc=dc, j=j: e.matmul(pb[:, 0:32], lhsT=hT[:, dc, j * 128:(j + 1) * 128], rhs=inw[:, dc, 6144:6176], start=(dc == 0), stop=(dc == 7)), [inw, hT], [pb])
                d = dtt[j % 2]
                kb.op('dve', lambda e, pb=pb, d=d: e.tensor_tensor(out=d[:], in0=pb[:, 0:32], in1=dtb[:], op=ALU.add), [pb, dtb], [d])
                kb.op('act', lambda e, d=d: e.activation(out=d[:], in_=d[:], func=AF.Exp), [d], [d])
                kb.op('act', lambda e, d=d: e.activation(out=d[:], in_=d[:], func=AF.Ln, bias=1.0), [d], [d])
                kb.dma('sp', DT[r0:r0 + 128, :], d[:], reads=[d], writes=[DT])
    kb.barrier()
    kb.new_epoch()
    with ExitStack() as sc:
        kb.scope = sc
        outw = kb.sb('outw', [128, 16, 1024], BF16)
        for ic in range(16):
            kb.dma('pool', outw[:, ic, :], W['out_w'][ic * 128:(ic + 1) * 128, :], reads=[W['out_w']], writes=[outw])
        gnb = bcast_load(kb, 'gnb', W['gate_norm'][0:1, :], 2048, W['gate_norm'])
        Abc = bcast_load(kb, 'Abc', W['A_log'][0:1, :], 32, W['A_log'])
        Dbc = bcast_load(kb, 'Dbc', W['D'][0:1, :], 32, W['D'])
        kb.op('act', lambda e: e.activation(out=Abc[:], in_=Abc[:], func=AF.Exp), [Abc], [Abc])
        kb.op('dve', lambda e: e.tensor_scalar(out=Abc[:], in0=Abc[:], scalar1=-1.0, scalar2=None, op0=ALU.mult), [Abc], [Abc])
        triu, gt, ones = C['c_triu'], C['c_gt'], C['c_ones']
        triu_f, ones_f = C['c_triu_f'], C['c_ones_f']
        xbt = [kb.sb(f'xbt{i}', [128, 3072], BF16) for i in range(2)]
        bt = [kb.sb(f'bt{i}', [128, 8, 128], BF16) for i in range(2)]
        ct = [kb.sb(f'ct{i}', [128, 8, 128], BF16) for i in range(2)]
        dtl = [kb.sb(f'dtl{i}', [128, 32], F32) for i in range(2)]
        zl = [kb.sb(f'zl{i}', [128, 2048], BF16) for i in range(2)]
        xl = [kb.sb(f'xl{i}', [128, 1024], F32) for i in range(2)]
        a_t = kb.sb('a_t', [128, 32], F32)
        acs = kb.sb('acs', [128, 32], F32)
        dte = kb.sb('dte', [128, 32], F32)
        cd = kb.sb('cd', [128, 32], F32)
        Xdt = kb.sb('Xdt', [128, 32, 64], BF16)
        Xd = kb.sb('Xd', [128, 32, 64], BF16)
        rhs_all = kb.sb('rhs_all', [128, 32, 128], BF16)
        cbTm = kb.sb('cbTm', [128, 8, 128], F32)
        LT = [kb.sb(f'LT{i}', [128, 4, 128], F32) for i in range(2)]
        decT = [kb.sb(f'decT{i}', [128, 4, 128], BF16) for i in range(2)]
        MT = [kb.sb(f'MT{i}', [128, 4, 128], BF16) for i in range(2)]
        CsT = [kb.sb(f'CsT{i}', [128, 4, 128], BF16) for i in range(2)]
        prevT = kb.sb('prevT', [128, 32, 64], F32)
        prevB = kb.sb('prevB', [128, 32, 64], BF16)
        kb.op('dve', lambda e: e.memset(prevT[:], 0.0), [], [prevT])
        kb.op('dve', lambda e: e.memset(prevB[:], 0.0), [], [prevB])
        t1s = [kb.sb(f't1_{i}', [128, 2048], F32) for i in range(2)]
        y2 = kb.sb('y2', [128, 2048], F32)
        sq = kb.sb('sq', [128, 2048], F32)
        ssq8 = kb.sb('ssq8', [128, 8], F32)
        y3 = kb.sb('y3', [128, 2048], BF16)
        ynT = kb.sb('ynT', [128, 16, 128], BF16)
        xo = [kb.sb(f'xo{i}', [128, 1024], F32) for i in range(2)]
        def genA(c):
            r0 = c * 128
            xb_, b_, c_, d_, z_, x_ = xbt[c % 2], bt[c % 2], ct[c % 2], dtl[c % 2], zl[c % 2], xl[c % 2]
            kb.dma('sp', xb_[:], XB[r0:r0 + 128, :], reads=[XB], writes=[xb_])
            kb.dma('sp', b_[:], BTs[c], reads=[BTs], writes=[b_])
            kb.dma('sp', c_[:], CTs[c], reads=[CTs], writes=[c_])
            kb.dma('sp', d_[:], DT[r0:r0 + 128, :], reads=[DT], writes=[d_])
            kb.dma('sp', z_[:], ZS[r0:r0 + 128, :], reads=[ZS], writes=[z_])
            kb.dma('sp', x_[:], x_d[r0:r0 + 128, :], reads=[x_d], writes=[x_])
            yield
            kb.op('dve', lambda e, d_=d_: e.tensor_tensor(out=a_t[:], in0=d_[:], in1=Abc[:], op=ALU.mult), [d_, Abc], [a_t])
            pb = kb.bank(kb.banks[4:6])
            kb.op('pe', lambda e, pb=pb: e.matmul(pb[:, 0:32], lhsT=triu_f[:], rhs=a_t[:], start=True, stop=True), [triu_f, a_t], [pb])
            kb.op('pe', lambda e, pb=pb: e.matmul(pb[:, 32:64], lhsT=ones_f[:], rhs=a_t[:], start=True, stop=True), [ones_f, a_t], [pb])
            kb.op('act', lambda e, pb=pb: e.copy(out=acs[:], in_=pb[:, 0:32]), [pb], [acs])
            kb.op('dve', lambda e, pb=pb: e.tensor_tensor(out=dte[:], in0=pb[:, 32:64], in1=acs[:], op=ALU.subtract), [pb, acs], [dte])
            kb.op('act', lambda e: e.activation(out=dte[:], in_=dte[:], func=AF.Exp), [dte], [dte])
            kb.op('act', lambda e, pb=pb: e.activation(out=cd[:], in_=pb[:, 32:64], func=AF.Exp), [pb], [cd])
            yield
            xs3 = xb_[:, 0:2048].rearrange('p (h d) -> p h d', d=64)
            kb.op('dve', lambda e, xs3=xs3, d_=d_: e.tensor_tensor(out=Xdt[:], in0=xs3, in1=d_[:].unsqueeze(2).to_broadcast([128, 32, 64]), op=ALU.mult), [xb_, d_], [Xdt])
            kb.op('dve', lambda e: e.tensor_tensor(out=Xd[:], in0=Xdt[:], in1=dte[:].unsqueeze(2).to_broadcast([128, 32, 64]), op=ALU.mult), [Xdt, dte], [Xd])
            yield
            for h_ in range(32):
                if h_ % 8 == 7:
                    yield
                kb.op('act', lambda e, h_=h_: e.activation(out=rhs_all[:, h_, :], in_=triu_f[:], func=AF.Copy, scale=a_t[:, h_:h_ + 1]), [a_t, triu_f], [rhs_all], nosync=(h_ > 0))
            for half in range(2):
                pb = kb.bank(kb.banks[4:6])
                for gg in range(4):
                    g = half * 4 + gg
                    kb.op('pe', lambda e, pb=pb, g=g, gg=gg, b_=b_, c_=c_: e.matmul(pb[:, gg * 128:(gg + 1) * 128], lhsT=b_[:, g, :], rhs=c_[:, g, :], start=True, stop=True), [b_, c_], [pb])
                kb.op('dve', lambda e, pb=pb, half=half: e.tensor_tensor(out=cbTm[:, half * 4:(half + 1) * 4, :], in0=pb[:, 0:512].rearrange('p (g l) -> p g l', l=128), in1=triu_f[:].unsqueeze(1).to_broadcast([128, 4, 128]), op=ALU.mult), [pb, triu_f], [cbTm])
            yield
            ybanks = kb.banks[0:4]
            sdb = [kb.banks[4], kb.banks[5], kb.xbanks[0], kb.xbanks[1]]

            def _segdec(hg):
                rh = rhs_all[:, hg * 4:(hg + 1) * 4, :].rearrange('p h l -> p (h l)')
                pseg = sdb[(hg % 2) * 2]
                pdec = sdb[(hg % 2) * 2 + 1]
                kb.op('pe', lambda e, pseg=pseg, rh=rh: e.matmul(pseg[:, 0:512], lhsT=gt[:], rhs=rh, start=True, stop=True), [gt, rhs_all], [pseg])
                kb.op('pe', lambda e, pdec=pdec, rh=rh: e.matmul(pdec[:, 0:512], lhsT=ones[:], rhs=rh, start=True, stop=True), [ones, rhs_all], [pdec])
                lt, dc_, mt, cs = LT[hg % 2], decT[hg % 2], MT[hg % 2], CsT[hg % 2]
                g = hg
                kb.op('act', lambda e, pseg=pseg, lt=lt: e.activation(out=lt[:].rearrange('p h l -> p (h l)'), in_=pseg[:, 0:512], func=AF.Exp), [pseg], [lt])
                kb.op('act', lambda e, pdec=pdec, dc_=dc_: e.activation(out=dc_[:].rearrange('p h l -> p (h l)'), in_=pdec[:, 0:512], func=AF.Exp), [pdec], [dc_])
                kb.op('dve', lambda e, lt=lt, mt=mt, g=g: e.tensor_tensor(out=mt[:], in0=lt[:], in1=cbTm[:, g:g + 1, :].to_broadcast([128, 4, 128]), op=ALU.mult), [lt, cbTm], [mt])
                kb.op('dve', lambda e, dc_=dc_, cs=cs, g=g, c_=c_: e.tensor_tensor(out=cs[:], in0=dc_[:], in1=c_[:, g:g + 1, :].to_broadcast([128, 4, 128]), op=ALU.mult), [dc_, c_], [cs])
            _segdec(0)
            for hg in range(8):
                if hg + 1 < 8:
                    _segdec(hg + 1)
                mt, cs = MT[hg % 2], CsT[hg % 2]
                for hh in range(4):
                    h = hg * 4 + hh
                    yb = ybanks[h // 8]
                    col = (h % 8) * 64
                    kb.op('pe', lambda e, yb=yb, col=col, mt=mt, hh=hh, h=h: e.matmul(yb[:, col:col + 64], lhsT=mt[:, hh, :], rhs=Xdt[:, h, :], start=True, stop=False), [mt, Xdt], [yb])
                    kb.op('pe', lambda e, yb=yb, col=col, cs=cs, hh=hh, h=h: e.matmul(yb[:, col:col + 64], lhsT=cs[:, hh, :], rhs=prevB[:, h, :], start=False, stop=True), [cs, prevB], [yb])
                yield
            t1 = t1s[c % 2]
            kb.op('dve', lambda e, xs3=xs3: e.tensor_tensor(out=t1[:].rearrange('p (h d) -> p h d', d=64), in0=xs3, in1=Dbc[:].unsqueeze(2).to_broadcast([128, 32, 64]), op=ALU.mult), [xb_, Dbc], [t1])
            for q in range(4):
                kb.op('dve', lambda e, q=q, yb=ybanks[q]: e.tensor_tensor(out=t1[:, q * 512:(q + 1) * 512], in0=yb[:, 0:512], in1=t1[:, q * 512:(q + 1) * 512], op=ALU.add), [ybanks[q], t1], [t1])
            yield
            sbanks = kb.banks[0:4]
            for h in range(32):
                g = h // 4
                sbk = sbanks[h // 8]
                col = (h % 8) * 64
                kb.op('pe', lambda e, sbk=sbk, col=col, g=g, h=h, xb_=xb_: e.matmul(sbk[:, col:col + 64], lhsT=xb_[:, 2048 + g * 128:2048 + (g + 1) * 128], rhs=Xd[:, h, :], start=True, stop=True), [xb_, Xd], [sbk])
            yield
            kb.op('dve', lambda e: e.tensor_tensor(out=prevT[:], in0=prevT[:], in1=cd[:].unsqueeze(2).to_broadcast([128, 32, 64]), op=ALU.mult), [prevT, cd], [prevT])
            pf = prevT[:].rearrange('p h d -> p (h d)')
            for q in range(4):
                kb.op('dve', lambda e, q=q, sbk=sbanks[q], pf=pf: e.tensor_tensor(out=pf[:, q * 512:(q + 1) * 512], in0=sbk[:, 0:512], in1=pf[:, q * 512:(q + 1) * 512], op=ALU.add), [sbanks[q], prevT], [prevT])
            kb.op('act', lambda e: e.copy(out=prevB[:], in_=prevT[:]), [prevT], [prevB])
            yield

        def genB(c):
            r0 = c * 128
            z_, x_ = zl[c % 2], xl[c % 2]
            t1 = t1s[c % 2]
            kb.op('dve', lambda e, z_=z_: e.tensor_tensor(out=y2[:], in0=t1[:], in1=z_[:], op=ALU.mult), [t1, z_], [y2])
            yield
            for g_ in range(8):
                kb.op('act', lambda e, g_=g_: e.activation(out=sq[:, g_ * 256:(g_ + 1) * 256], in_=y2[:, g_ * 256:(g_ + 1) * 256], func=AF.Square, accum_out=ssq8[:, g_:g_ + 1]), [y2], [sq, ssq8], nosync=(g_ > 0))
            yield
            kb.op('dve', lambda e: e.tensor_scalar(out=ssq8[:], in0=ssq8[:], scalar1=1.0 / 256, scalar2=1e-5, op0=ALU.mult, op1=ALU.add), [ssq8], [ssq8])
            kb.op('act', lambda e: e.activation(out=ssq8[:], in_=ssq8[:], func=AF.Ln), [ssq8], [ssq8])
            kb.op('act', lambda e: e.activation(out=ssq8[:], in_=ssq8[:], func=AF.Exp, scale=-0.5), [ssq8], [ssq8])
            yield
            for g_ in range(8):
                kb.op('dve', lambda e, g_=g_: e.scalar_tensor_tensor(out=y3[:, g_ * 256:(g_ + 1) * 256], in0=y2[:, g_ * 256:(g_ + 1) * 256], scalar=ssq8[:, g_:g_ + 1], in1=gnb[:, g_ * 256:(g_ + 1) * 256], op0=ALU.mult, op1=ALU.mult), [y2, ssq8, gnb], [y3], nosync=(g_ > 0))
            yield
            for hf in range(2):
                pt = ptr[hf]
                for ic in range(8):
                    kb.op('pe', lambda e, pt=pt, ic=ic, hf=hf: e.transpose(out=pt[:, ic * 128:(ic + 1) * 128], in_=y3[:, (hf * 8 + ic) * 128:(hf * 8 + ic + 1) * 128], identity=ident[:]), [y3, ident], [pt])
                kb.op('act', lambda e, pt=pt, hf=hf: e.copy(out=ynT[:, hf * 8:(hf + 1) * 8, :], in_=pt[:, 0:1024].rearrange('p (c t) -> p c t', t=128)), [pt], [ynT])
            yield
            o = xo[c % 2]
            for hf in range(2):
                yield
                pb = kb.bank(kb.banks[4:6])
                for ic in range(16):
                    kb.op('pe', lambda e, pb=pb, ic=ic, hf=hf: e.matmul(pb[:, 0:512], lhsT=ynT[:, ic, :], rhs=outw[:, ic, hf * 512:(hf + 1) * 512], start=(ic == 0), stop=(ic == 15)), [ynT, outw], [pb])
                kb.op('dve', lambda e, pb=pb, hf=hf, o=o, x_=x_: e.tensor_tensor(out=o[:, hf * 512:(hf + 1) * 512], in0=pb[:, 0:512], in1=x_[:, hf * 512:(hf + 1) * 512], op=ALU.add), [pb, x_], [o])
            kb.dma('sp', x1_d[r0:r0 + 128, :], o[:], reads=[o], writes=[x1_d])

        def _adv(g):
            if g is None:
                return None
            try:
                next(g)
                return g
            except StopIteration:
                return None
        gA = genA(0)
        while gA is not None:
            gA = _adv(gA)
        for c in range(NCH):
            gB = genB(c)
            gA = genA(c + 1) if c + 1 < NCH else None
            k_ = 0
            while gA is not None or gB is not None:
                gA = _adv(gA)
                gB = _adv(gB)
                k_ += 1
    kb.scope = None
    kb.barrier()
    kb.new_epoch()


def new_kb():
    nc = bass.Bass("TRN2", target_bir_lowering=False)
    kb = KB(nc)
    allb = [kb.ps(f'bank{i}', [128, 512], F32) for i in range(8)]
    kb.banks = allb[0:6]
    kb.xbanks = allb[6:8]
    kb.ptr = [Buf(b.t[:].bitcast(BF16), f'ptrv{i}', parent=b) for i, b in enumerate(kb.xbanks)]
    return nc, kb


def peer_convert(kb, W, L, lazy=False):
    U16 = kb.scratch(f'sc_U16_{L}', [128, 128, 1024], BF16)
    V16 = kb.scratch(f'sc_V16_{L}', [128, 128, 1024], BF16)

    def gen():
        for q in range(64):
            for (src, dst) in ((W['peer_uT'], U16), (W['peer_v'], V16)):
                kb.dma('pool', dst.t[q * 2:(q + 1) * 2].rearrange('c p n -> p c n'), src.t[q * 2:(q + 1) * 2].rearrange('c p n -> p c n'), reads=[src], writes=[dst], sempool='cv')
                yield
    g = gen()
    if lazy:
        return U16, V16, g
    for _ in g:
        pass
    return U16, V16


def phase_peer(kb, C, x_in, x_out, W, U16, V16, NT=256, nst=None, extra_gen=None):
    ident, ident_f = C['c_ident'], C['c_ident_f']
    iota = C['c_iota_f']
    iota_b = C['c_iota'] if OPT_BF16_ONEHOT else C['c_iota_f']
    ptr = kb.ptr
    nsub = NT // 128
    NST = (S // NT) if nst is None else nst
    with ExitStack() as sc:
        kb.scope = sc
        qw = kb.sb('qw', [128, 8, 2048], BF16)
        for dc in range(8):
            kb.dma('pool', qw[:, dc, :], W['peer_q_w'][dc * 128:(dc + 1) * 128, :], reads=[W['peer_q_w']], writes=[qw])
        skT = kb.sb('skT', [128, 2, 128], BF16)
        kb.dma('pool', skT[:], W['peer_skT'].t.rearrange('c d k -> d c k'), reads=[W['peer_skT']], writes=[skT])
        gbc = bcast_load(kb, 'gbcp', W['peer_norm'][0:1, :], 1024, W['peer_norm'])
        G = kb.sb('G', [128, 128, NT], BF16)
        uvb = [(kb.sb(f'ub{i}', [128, 2, 1024], BF16), kb.sb(f'vb{i}', [128, 2, 1024], BF16)) for i in range(3)]
        arA = kb.sb('arA', [128, 2048], F32)
        arB = kb.sb('arB', [128, 2048], F32)
        arC = kb.sb('arC', [128, 2048], F32)
        qT = kb.sb('qT', [128, 16, NT], BF16)
        hTs = [kb.sb(f'hTp{i}', [128, 8, NT], BF16) for i in range(2)]
        hb = [kb.sb(f'hbp{i}', [128, 1024], BF16) for i in range(1)]
        xt = [kb.sb(f'xtp{i}', [128, 1024], F32) for i in range(1)]
        ssq = kb.sb('ssqp', [128, 1], F32)
        rstd = kb.sb('rstdp', [128, 1], F32)
        v16 = kb.sb('v16', [128, 16, 16], F32)
        idx = kb.sb('idx', [128, 16, 16], U32)
        idxf = kb.sb('idxf', [128, 16, 16], F32)
        c8 = kb.sb('c8', [128, 8, 16], F32)
        pos = kb.sb('pos', [128, 8, 16], U32)
        posf = kb.sb('posf', [128, 8, 16], F32)
        r1f = kb.sb('r1f', [128, 8, 16], F32)
        r2f = kb.sb('r2f', [128, 8, 16], F32)
        ge = kb.sb('ge', [128, 8, 16], F32)
        zz = kb.sb('zz', [128, 8], F32)
        IJG = kb.sb('IJG', [128, 3, 128], F32)
        IJGT = [kb.sb(f'IJGT{i}', [128, 3, 128], BF16 if OPT_BF16_ONEHOT else F32) for i in range(nsub)]
        TB = OPT_TB
        XYb = [kb.sb(f'XYb{i}', [128, TB, 2, 128], BF16) for i in range(2)]
        gl = [kb.sb(f'gl{i}', [128, NT], BF16) for i in range(3)]
        wT = [kb.sb(f'wT{i}', [128, NT], BF16) for i in range(3)]
        thr = kb.sb('thr', [128, 16], F32)
        kb.op('dve', lambda e: e.tensor_scalar(out=thr[:], in0=iota[:, 0:16], scalar1=16.0, scalar2=16.0, op0=ALU.mult, op1=ALU.add), [iota], [thr])
        ob = kb.banks[0:4]
        aslots = [kb.banks[4], kb.banks[5], kb.xbanks[1]]
        rbank = kb.xbanks[0]
        LOOK = 1

        def routing(stile, qbank=None):
            t0 = stile * NT
            hT = hTs[stile % 2]
            for sub in range(nsub):
                xx = xt[0]
                r0 = t0 + sub * 128
                kb.dma('sp', xx[:], x_in[r0:r0 + 128, :], reads=[x_in], writes=[xx])
                h = hb[0]
                rmsnorm_bf(kb, xx, gbc, h, 1024, 1e-6, arC, ssq, rstd)
                yield
                transpose_to(kb, h, 1024, hT[:, :, sub * 128:(sub + 1) * 128], hT, ident, ptr[1])
                yield
            for fc in range(16):
                pb = rbank if qbank is None else qbank[fc % len(qbank)]
                for dc in range(8):
                    kb.op('pe', lambda e, pb=pb, dc=dc, fc=fc: e.matmul(pb[:, 0:NT], lhsT=qw[:, dc, fc * 128:(fc + 1) * 128], rhs=hT[:, dc, :], start=(dc == 0), stop=(dc == 7)), [qw, hT], [pb])
                kb.op('act', lambda e, pb=pb, fc=fc: e.copy(out=qT[:, fc, :], in_=pb[:, 0:NT]), [pb], [qT])
                yield
            for sub in range(nsub):
                s_sb = arA
                for q4 in range(4):
                    pb = rbank
                    for i4 in range(4):
                        fc = q4 * 4 + i4
                        kb.op('pe', lambda e, pb=pb, fc=fc, i4=i4, sub=sub: e.matmul(pb[:, i4 * 128:(i4 + 1) * 128], lhsT=qT[:, fc, sub * 128:(sub + 1) * 128], rhs=skT[:, fc % 2, :], start=True, stop=True), [qT, skT], [pb])
                    kb.op('act', lambda e, pb=pb, q4=q4: e.copy(out=s_sb[:, q4 * 512:(q4 + 1) * 512], in_=pb[:, 0:512]), [pb], [s_sb])
                    yield
                tmp = arC
                last = None
                for sg in range(16):
                    ss = s_sb[:, sg * 128:(sg + 1) * 128]
                    last = kb.op('dve', lambda e, ss=ss, sg=sg: e.max(out=v16[:, sg, 0:8], in_=ss), [s_sb], [v16], nosync=(sg > 0))
                    if sg % 4 == 3:
                        yield
                for sg in range(16):
                    ss = s_sb[:, sg * 128:(sg + 1) * 128]
                    t_ = kb.op('dve', lambda e, ss=ss, sg=sg: e.max_index(out=idx[:, sg, 0:8], in_max=v16[:, sg, 0:8], in_values=ss), [s_sb, v16], [idx], nosync=(sg > 0), after=([last] if sg == 0 else []))
                    if sg == 15:
                        last = t_
                    if sg % 4 == 3:
                        yield
                for sg in range(16):
                    ss = s_sb[:, sg * 128:(sg + 1) * 128]
                    t_ = kb.op('dve', lambda e, ss=ss, sg=sg: e.match_replace(out=tmp[:, sg * 128:(sg + 1) * 128], in_to_replace=v16[:, sg, 0:8], in_values=ss, imm_value=-1e30), [s_sb, v16], [tmp], nosync=(sg > 0), after=([last] if sg == 0 else []))
                    if sg == 15:
                        last = t_
                    if sg % 4 == 3:
                        yield
                for sg in range(16):
                    t_ = kb.op('dve', lambda e, sg=sg: e.max(out=v16[:, sg, 8:16], in_=tmp[:, sg * 128:(sg + 1) * 128]), [tmp], [v16], nosync=(sg > 0), after=([last] if sg == 0 else []))
                    if sg == 15:
                        last = t_
                    if sg % 4 == 3:
                        yield
                for sg in range(16):
                    t_ = kb.op('dve', lambda e, sg=sg: e.max_index(out=idx[:, sg, 8:16], in_max=v16[:, sg, 8:16], in_values=tmp[:, sg * 128:(sg + 1) * 128]), [tmp, v16], [idx], nosync=(sg > 0), after=([last] if sg == 0 else []))
                    if sg == 15:
                        last = t_
                    if sg % 4 == 3:
                        yield
                kb.op('dve', lambda e: e.tensor_copy(out=idxf[:], in_=idx[:]), [idx], [idxf])
                cand = arB
                v4 = v16[:].rearrange('p (h c) r -> p h c r', c=2)
                kb.op('dve', lambda e: e.tensor_tensor(out=cand[:].rearrange('p (h a b) -> p h a b', a=16, b=16), in0=v4[:, :, 0, :].unsqueeze(3).to_broadcast([128, 8, 16, 16]), in1=v4[:, :, 1, :].unsqueeze(2).to_broadcast([128, 8, 16, 16]), op=ALU.add), [v16], [cand])
                yield
                tmp2 = arA
                last = None
                for hh in range(8):
                    cs_ = cand[:, hh * 256:(hh + 1) * 256]
                    last = kb.op('dve', lambda e, cs_=cs_, hh=hh: e.max(out=c8[:, hh, 0:8], in_=cs_), [cand], [c8], nosync=(hh > 0))
                yield
                for hh in range(8):
                    cs_ = cand[:, hh * 256:(hh + 1) * 256]
                    t_ = kb.op('dve', lambda e, cs_=cs_, hh=hh: e.max_index(out=pos[:, hh, 0:8], in_max=c8[:, hh, 0:8], in_values=cs_), [cand, c8], [pos], nosync=(hh > 0), after=([last] if hh == 0 else []))
                    if hh == 7:
                        last = t_
                yield
                for hh in range(8):
                    cs_ = cand[:, hh * 256:(hh + 1) * 256]
                    t_ = kb.op('dve', lambda e, cs_=cs_, hh=hh: e.match_replace(out=tmp2[:, hh * 256:(hh + 1) * 256], in_to_replace=c8[:, hh, 0:8], in_values=cs_, imm_value=-1e30), [cand, c8], [tmp2], nosync=(hh > 0), after=([last] if hh == 0 else []))
                    if hh == 7:
                        last = t_
                yield
                for hh in range(8):
                    t_ = kb.op('dve', lambda e, hh=hh: e.max(out=c8[:, hh, 8:16], in_=tmp2[:, hh * 256:(hh + 1) * 256]), [tmp2], [c8], nosync=(hh > 0), after=([last] if hh == 0 else []))
                    if hh == 7:
                        last = t_
                yield
                for hh in range(8):
                    t_ = kb.op('dve', lambda e, hh=hh: e.max_index(out=pos[:, hh, 8:16], in_max=c8[:, hh, 8:16], in_values=tmp2[:, hh * 256:(hh + 1) * 256]), [tmp2, c8], [pos], nosync=(hh > 0), after=([last] if hh == 0 else []))
                    if hh == 7:
                        last = t_
                yield
                kb.op('dve', lambda e: e.tensor_tensor(out=ge[:], in0=c8[:], in1=c8[:, :, 0:1].to_broadcast([128, 8, 16]), op=ALU.subtract), [c8], [ge])
                kb.op('act', lambda e: e.activation(out=ge[:], in_=ge[:], func=AF.Exp), [ge], [ge])
                kb.op('dve', lambda e: e.tensor_reduce(out=zz[:], in_=ge[:], axis=AX.X, op=ALU.add), [ge], [zz])
                kb.op('dve', lambda e: e.reciprocal(out=zz[:], in_=zz[:]), [zz], [zz])
                kb.op('dve', lambda e: e.tensor_tensor(out=IJG[:, 2, :].rearrange('p (h k) -> p h k', k=16), in0=ge[:], in1=zz[:].unsqueeze(2).to_broadcast([128, 8, 16]), op=ALU.mult), [ge, zz], [IJG])
                yield
                kb.op('dve', lambda e: e.tensor_copy(out=posf[:], in_=pos[:]), [pos], [posf])
                kb.op('dve', lambda e: e.tensor_tensor(out=arA[:].rearrange('p (h k r) -> p h k r', k=16, r=16), in0=posf[:].unsqueeze(3).to_broadcast([128, 8, 16, 16]), in1=thr[:].unsqueeze(1).unsqueeze(1).to_broadcast([128, 8, 16, 16]), op=ALU.is_ge), [posf, thr], [arA])
                yield
                kb.op('dve', lambda e: e.tensor_reduce(out=r1f[:].rearrange('p h k -> p (h k)'), in_=arA[:].rearrange('p (m r) -> p m r', r=16), axis=AX.X, op=ALU.add), [arA], [r1f])
                kb.op('dve', lambda e: e.scalar_tensor_tensor(out=r2f[:], in0=r1f[:], scalar=-16.0, in1=posf[:], op0=ALU.mult, op1=ALU.add), [r1f, posf], [r2f])
                yield
                i4v = idxf[:].rearrange('p (h c) r -> p h c r', c=2)
                for which, rf in ((0, r1f), (1, r2f)):
                    eq = arA[:].rearrange('p (h k r) -> p h k r', k=16, r=16)
                    kb.op('dve', lambda e, rf=rf, eq=eq: e.tensor_tensor(out=eq, in0=iota[:, 0:16].unsqueeze(1).unsqueeze(1).to_broadcast([128, 8, 16, 16]), in1=rf[:].unsqueeze(3).to_broadcast([128, 8, 16, 16]), op=ALU.is_equal), [rf, iota], [arA])
                    yield
                    kb.op('dve', lambda e, eq=eq, which=which: e.tensor_tensor(out=arC[:].rearrange('p (h k r) -> p h k r', k=16, r=16), in0=eq, in1=i4v[:, :, which, :].unsqueeze(2).to_broadcast([128, 8, 16, 16]), op=ALU.mult), [arA, idxf], [arC])
                    yield
                    kb.op('dve', lambda e, which=which: e.tensor_reduce(out=IJG[:, which, :], in_=arC[:].rearrange('p (m r) -> p m r', r=16), axis=AX.X, op=ALU.add), [arC], [IJG])
                    yield
                pb = rbank
                for w3 in range(3):
                    kb.op('pe', lambda e, pb=pb, w3=w3: e.transpose(out=pb[:, w3 * 128:(w3 + 1) * 128], in_=IJG[:, w3, :], identity=ident_f[:]), [IJG, ident_f], [pb])
                ijgt = IJGT[sub]
                kb.op('act', lambda e, pb=pb, ijgt=ijgt: e.copy(out=ijgt[:].rearrange('p a t -> p (a t)'), in_=pb[:, 0:384]), [pb], [ijgt])
                yield

        def ggen(stile, gen=None):
            pbs = [rbank, kb.xbanks[1]]
            k = 0
            npumped = 0
            for sub in range(nsub):
                ijgt = IJGT[sub]
                for blk in range(128 // TB):
                    for _rep in range(TB // 8):
                        if extra_gen is not None and blk * (TB // 8) + _rep < 4:
                            try:
                                next(extra_gen)
                            except StopIteration:
                                pass
                        if gen is not None and npumped < 20:
                            npumped += 1
                            try:
                                next(gen)
                            except StopIteration:
                                pass
                    XY = XYb[blk % 2]
                    tb = blk * TB
                    kb.op('dve', lambda e, XY=XY, tb=tb, ijgt=ijgt: e.tensor_tensor(out=XY[:], in0=iota_b[:].unsqueeze(1).unsqueeze(1).to_broadcast([128, TB, 2, 128]), in1=ijgt[:, 0:2, tb:tb + TB].rearrange('p w t -> p t w').unsqueeze(3).to_broadcast([128, TB, 2, 128]), op=ALU.is_equal), [iota_b, ijgt], [XY], nosync=True)
                    kb.op('dve', lambda e, XY=XY, tb=tb, ijgt=ijgt: e.tensor_tensor(out=XY[:, :, 1, :], in0=XY[:, :, 1, :], in1=ijgt[:, 2, tb:tb + TB].unsqueeze(2).to_broadcast([128, TB, 128]), op=ALU.mult), [XY, ijgt], [XY])
                    for t4 in range(TB // 4):
                        pb = pbs[k % 2]
                        k += 1
                        for ti in range(4):
                            t = t4 * 4 + ti
                            kb.op('pe', lambda e, pb=pb, ti=ti, t=t, XY=XY: e.matmul(pb[:, ti * 128:(ti + 1) * 128], lhsT=XY[:, t, 1, :], rhs=XY[:, t, 0, :], start=True, stop=True), [XY], [pb])
                        tok = sub * 128 + tb + t4 * 4
                        kb.op('act', lambda e, pb=pb, tok=tok: e.copy(out=G[:, :, tok:tok + 4], in_=pb[:, 0:512].rearrange('p (t i) -> p i t', i=128)), [pb], [G])

        def stageF(stile, gen):
            hT = hTs[stile % 2]

            def pump(n):
                if gen is None:
                    return
                for _ in range(n):
                    try:
                        next(gen)
                    except StopIteration:
                        return

            def load(cg):
                ub, vb = uvb[cg % 3]
                kb.dma('sp', ub[:], U16.t[cg * 2:(cg + 1) * 2].rearrange('c p n -> p c n'), reads=[U16], writes=[ub])
                kb.dma('sp', vb[:], V16.t[cg * 2:(cg + 1) * 2].rearrange('c p n -> p c n'), reads=[V16], writes=[vb])

            def emit_a(c):
                ub, vb = uvb[(c // 2) % 3]
                pa = aslots[c % 3]
                ci = c % 2
                for dc in range(8):
                    kb.op('pe', lambda e, pa=pa, dc=dc, ci=ci, ub=ub: e.matmul(pa[:, 0:NT], lhsT=ub[:, ci, dc * 128:(dc + 1) * 128], rhs=hT[:, dc, :], start=(dc == 0), stop=(dc == 7)), [ub, hT], [pa])
                g_ = gl[c % 3]
                w_ = wT[c % 3]
                kb.op('act', lambda e, pa=pa, g_=g_: e.activation(out=g_[:], in_=pa[:, 0:NT], func=AF.Gelu), [pa], [g_])
                kb.op('pool' if (OPT_POOLMULT and c % OPT_POOLMULT == OPT_POOLMULT - 1) else 'dve', lambda e, g_=g_, w_=w_, c=c: e.tensor_tensor(out=w_[:], in0=g_[:], in1=G[:, c, :], op=ALU.mult), [g_, G], [w_])

            def emit_s2(c):
                ub, vb = uvb[(c // 2) % 3]
                w_ = wT[c % 3]
                ci = c % 2
                for tt in range(nsub):
                    for dh in range(2):
                        o = ob[tt * 2 + dh]
                        kb.op('pe', lambda e, o=o, tt=tt, dh=dh, ci=ci, vb=vb, w_=w_, c=c: e.matmul(o[:, 0:512], lhsT=w_[:, tt * 128:(tt + 1) * 128], rhs=vb[:, ci, dh * 512:(dh + 1) * 512], start=(c == 0), stop=(c == 127)), [w_, vb], [o])
            load(0)
            load(1)
            for c0 in range(LOOK):
                emit_a(c0)
            for c in range(128):
                if c % 2 == 0 and c // 2 + 2 < 64:
                    load(c // 2 + 2)
                if c + LOOK < 128:
                    emit_a(c + LOOK)
                emit_s2(c)
                pump(OPT_PUMP)
            pump(100000)

        def epilogue(stile):
            t0 = stile * NT
            for sub in range(nsub):
                xx = xt[0]
                r0 = t0 + sub * 128
                kb.dma('sp', xx[:], x_in[r0:r0 + 128, :], reads=[x_in], writes=[xx])
                for dh in range(2):
                    kb.op('dve', lambda e, xx=xx, dh=dh, o=ob[sub * 2 + dh]: e.tensor_tensor(out=xx[:, dh * 512:(dh + 1) * 512], in0=o[:, 0:512], in1=xx[:, dh * 512:(dh + 1) * 512], op=ALU.add), [ob[sub * 2 + dh], xx], [xx])
                kb.dma('sp', x_out[r0:r0 + 128, :], xx[:], reads=[xx], writes=[x_out])

        for _ in routing(0):
            pass
        for stile in range(NST):
            gen = routing(stile + 1, qbank=[kb.banks[4], kb.banks[5]]) if stile + 1 < NST else None
            ggen(stile, gen)
            if OPT_PUMP < 0:
                stageF(stile, None)
                epilogue(stile)
                if gen is not None:
                    for _ in gen:
                        pass
            else:
                stageF(stile, gen)
                epilogue(stile)
        if extra_gen is not None:
            for _ in extra_gen:
                pass
    kb.scope = None
    kb.barrier()
    kb.new_epoch()


def _adv(g):
    if g is None:
        return None
    try:
        next(g)
        return g
    except StopIteration:
        return None


def run_pipelined(genF, genB, n, ratio=1):
    g = genF(0)
    while g is not None:
        g = _adv(g)
    for i in range(n):
        gB = genB(i)
        gF = genF(i + 1) if i + 1 < n else None
        k_ = 0
        while gF is not None or gB is not None:
            gB = _adv(gB)
            if k_ % ratio == ratio - 1 or gB is None:
                gF = _adv(gF)
            k_ += 1


def phase_ple(kb, C, x_in, x_out, p_d, W, kv_out=None, final=False):
    ident = C['c_ident']
    ptr = kb.ptr
    with ExitStack() as sc:
        kb.scope = sc
        gw = kb.sb('gw', [128, 8, 1024], BF16)
        for dc in range(8):
            kb.dma('pool', gw[:, dc, :], W['ple_gate_w'][dc * 128:(dc + 1) * 128, :], reads=[W['ple_gate_w']], writes=[gw])
        pw = kb.sb('pw', [128, 2, 1024], BF16)
        for kc in range(2):
            kb.dma('pool', pw[:, kc, :], W['ple_proj'][kc * 128:(kc + 1) * 128, :], reads=[W['ple_proj']], writes=[pw])
        gbc = bcast_load(kb, 'gbc_ple', W['ple_norm'][0:1, :], 1024, W['ple_norm'])
        if kv_out is not None:
            kvw = kb.sb('kvw', [128, 8, 256], BF16)
            for dc in range(8):
                kb.dma('pool', kvw[:, dc, :], W['kv_w'][dc * 128:(dc + 1) * 128, :], reads=[W['kv_w']], writes=[kvw])
            gkv = bcast_load(kb, 'gbc_kv', W['kv_norm'][0:1, :], 1024, W['kv_norm'])
            kvb = bcast_load(kb, 'kvb', W['kv_b'][0:1, :], 256, W['kv_b'])
        if final:
            gfin = bcast_load(kb, 'gbc_fin', W['final_norm'][0:1, :], 1024, W['final_norm'])
        xt = [kb.sb(f'xtl{i}', [128, 1024], F32) for i in range(3)]
        pt_ = [kb.sb(f'ptl{i}', [128, 256], F32) for i in range(2)]
        pb16 = [kb.sb(f'pb16{i}', [128, 256], BF16) for i in range(2)]
        pT = [kb.sb(f'pTl{i}', [128, 2, 128], BF16) for i in range(2)]
        hb = [kb.sb(f'hbl{i}', [128, 1024], BF16) for i in range(2)]
        hT = [kb.sb(f'hTl{i}', [128, 8, 128], BF16) for i in range(2)]
        junk = kb.sb('junkl', [128, 1024], F32)
        ssq = kb.sb('ssql', [128, 1], F32)
        rstd = kb.sb('rstdl', [128, 1], F32)
        hbK = kb.sb('hbK', [128, 1024], BF16)
        hTK = kb.sb('hTK', [128, 8, 128], BF16)
        junkK = kb.sb('junkK', [128, 1024], F32)
        ssqK = kb.sb('ssqK', [128, 1], F32)
        rstdK = kb.sb('rstdK', [128, 1], F32)
        sg = kb.sb('sgl', [128, 1024], F32)
        x2 = [kb.sb(f'x2l{i}', [128, 1024], F32) for i in range(2)]
        kvt = [kb.sb(f'kvt{i}', [128, 256], F32) for i in range(2)]
        of = [kb.sb(f'ofl{i}', [128, 1024], F32) for i in range(2)]
        rot = kb.banks[0:4]
        rotK = kb.banks[4:6]

        def genF(n):
            r0 = n * 128
            xx, pp = xt[n % 3], pt_[n % 2]
            kb.dma('sp', xx[:], x_in[r0:r0 + 128, :], reads=[x_in], writes=[xx])
            kb.dma('sp', pp[:], p_d[r0:r0 + 128, :], reads=[p_d], writes=[pp])
            yield
            kb.op('act', lambda e, pp=pp: e.copy(out=pb16[n % 2][:], in_=pp[:]), [pp], [pb16[n % 2]])
            kb.op('act', lambda e, xx=xx: e.activation(out=junk[:], in_=xx[:], func=AF.Square, accum_out=ssq[:, 0:1]), [xx], [junk, ssq])
            yield
            kb.op('dve', lambda e: e.tensor_scalar(out=rstd[:, 0:1], in0=ssq[:, 0:1], scalar1=1.0 / 1024, scalar2=1e-6, op0=ALU.mult, op1=ALU.add), [ssq], [rstd])
            kb.op('act', lambda e: e.activation(out=rstd[:, 0:1], in_=rstd[:, 0:1], func=AF.Ln), [rstd], [rstd])
            yield
            kb.op('act', lambda e: e.activation(out=rstd[:, 0:1], in_=rstd[:, 0:1], func=AF.Exp, scale=-0.5), [rstd], [rstd])
            h = hb[n % 2]
            kb.op('dve', lambda e, xx=xx, h=h: e.scalar_tensor_tensor(out=h[:], in0=xx[:], scalar=rstd[:, 0:1], in1=gbc[:], op0=ALU.mult, op1=ALU.mult), [xx, rstd, gbc], [h])
            yield
            transpose_to(kb, h, 1024, hT[n % 2][:], hT[n % 2], ident, ptr[0])
            yield
            transpose_to(kb, pb16[n % 2], 256, pT[n % 2][:], pT[n % 2], ident, ptr[1], evac='dve')
            yield

        def genB(n):
            r0 = n * 128
            xx, xo = xt[n % 3], x2[n % 2]
            hT_, pT_ = hT[n % 2], pT[n % 2]
            for hf in range(2):
                pg = kb.bank(rot)
                for dc in range(8):
                    kb.op('pe', lambda e, pg=pg, dc=dc, hf=hf: e.matmul(pg[:, 0:512], lhsT=hT_[:, dc, :], rhs=gw[:, dc, hf * 512:(hf + 1) * 512], start=(dc == 0), stop=(dc == 7)), [hT_, gw], [pg])
                kb.op('act', lambda e, pg=pg, hf=hf: e.activation(out=sg[:, hf * 512:(hf + 1) * 512], in_=pg[:, 0:512], func=AF.Exp, scale=-1.0), [pg], [sg])
                kb.op('dve', lambda e, hf=hf: e.tensor_scalar(out=sg[:, hf * 512:(hf + 1) * 512], in0=sg[:, hf * 512:(hf + 1) * 512], scalar1=1.0, scalar2=None, op0=ALU.add), [sg], [sg])
                kb.op('dve', lambda e, hf=hf: e.reciprocal(out=sg[:, hf * 512:(hf + 1) * 512], in_=sg[:, hf * 512:(hf + 1) * 512]), [sg], [sg])
                pq = kb.bank(rot)
                for kc in range(2):
                    kb.op('pe', lambda e, pq=pq, kc=kc, hf=hf: e.matmul(pq[:, 0:512], lhsT=pT_[:, kc, :], rhs=pw[:, kc, hf * 512:(hf + 1) * 512], start=(kc == 0), stop=(kc == 1)), [pT_, pw], [pq])
                yield
                kb.op('dve', lambda e, pq=pq, hf=hf: e.tensor_tensor(out=sg[:, hf * 512:(hf + 1) * 512], in0=pq[:, 0:512], in1=sg[:, hf * 512:(hf + 1) * 512], op=ALU.mult), [pq, sg], [sg])
                yield
            kb.op('dve', lambda e, xx=xx, xo=xo: e.tensor_tensor(out=xo[:], in0=xx[:], in1=sg[:], op=ALU.add), [xx, sg], [xo])
            yield
            if kv_out is not None:
                kb.op('act', lambda e, xo=xo: e.activation(out=junkK[:], in_=xo[:], func=AF.Square, accum_out=ssqK[:, 0:1]), [xo], [junkK, ssqK])
                yield
                kb.op('dve', lambda e: e.tensor_scalar(out=rstdK[:, 0:1], in0=ssqK[:, 0:1], scalar1=1.0 / 1024, scalar2=1e-6, op0=ALU.mult, op1=ALU.add), [ssqK], [rstdK])
                kb.op('act', lambda e: e.activation(out=rstdK[:, 0:1], in_=rstdK[:, 0:1], func=AF.Ln), [rstdK], [rstdK])
                yield
                kb.op('act', lambda e: e.activation(out=rstdK[:, 0:1], in_=rstdK[:, 0:1], func=AF.Exp, scale=-0.5), [rstdK], [rstdK])
                kb.op('dve', lambda e, xo=xo: e.scalar_tensor_tensor(out=hbK[:], in0=xo[:], scalar=rstdK[:, 0:1], in1=gkv[:], op0=ALU.mult, op1=ALU.mult), [xo, rstdK, gkv], [hbK])
                yield
                transpose_to(kb, hbK, 1024, hTK[:], hTK, ident, ptr[1])
                yield
                pk = kb.bank(rotK)
                for dc in range(8):
                    kb.op('pe', lambda e, pk=pk, dc=dc: e.matmul(pk[:, 0:256], lhsT=hTK[:, dc, :], rhs=kvw[:, dc, :], start=(dc == 0), stop=(dc == 7)), [hTK, kvw], [pk])
                kv = kvt[n % 2]
                kb.op('dve', lambda e, pk=pk, kv=kv: e.tensor_tensor(out=kv[:], in0=pk[:, 0:256], in1=kvb[:], op=ALU.add), [pk, kvb], [kv])
                kb.dma('sp', kv_out[r0:r0 + 128, :], kv[:], reads=[kv], writes=[kv_out])
                yield
            if final:
                o = of[n % 2]
                kb.op('act', lambda e, xo=xo: e.activation(out=junkK[:], in_=xo[:], func=AF.Square, accum_out=ssqK[:, 0:1]), [xo], [junkK, ssqK])
                yield
                kb.op('dve', lambda e: e.tensor_scalar(out=rstdK[:, 0:1], in0=ssqK[:, 0:1], scalar1=1.0 / 1024, scalar2=1e-6, op0=ALU.mult, op1=ALU.add), [ssqK], [rstdK])
                kb.op('act', lambda e: e.activation(out=rstdK[:, 0:1], in_=rstdK[:, 0:1], func=AF.Ln), [rstdK], [rstdK])
                yield
                kb.op('act', lambda e: e.activation(out=rstdK[:, 0:1], in_=rstdK[:, 0:1], func=AF.Exp, scale=-0.5), [rstdK], [rstdK])
                kb.op('dve', lambda e, xo=xo, o=o: e.scalar_tensor_tensor(out=o[:], in0=xo[:], scalar=rstdK[:, 0:1], in1=gfin[:], op0=ALU.mult, op1=ALU.mult), [xo, rstdK, gfin], [o])
                kb.dma('sp', x_out[r0:r0 + 128, :], o[:], reads=[o], writes=[x_out])
                yield
            else:
                kb.dma('sp', x_out[r0:r0 + 128, :], xo[:], reads=[xo], writes=[x_out])
                yield
        run_pipelined(genF, genB, NCH, ratio=2)
    kb.scope = None
    kb.barrier()
    kb.new_epoch()


MAGIC = 12582912.0


def phase_attn(kb, C, x_in, x_out, kv_d, pos_d, W):
    ident = C['c_ident']
    ptr = kb.ptr
    with ExitStack() as sc:
        kb.scope = sc
        qw = kb.sb('qwa', [128, 8, 1024], BF16)
        ow = kb.sb('owa', [128, 8, 1024], BF16)
        for dc in range(8):
            kb.dma('pool', qw[:, dc, :], W['q_w'][dc * 128:(dc + 1) * 128, :], reads=[W['q_w']], writes=[qw])
            kb.dma('pool', ow[:, dc, :], W['o_w'][dc * 128:(dc + 1) * 128, :], reads=[W['o_w']], writes=[ow])
        gbc = bcast_load(kb, 'gbc_at', W['attn_norm'][0:1, :], 1024, W['attn_norm'])
        qb = bcast_load(kb, 'qb_at', W['q_b'][0:1, :], 1024, W['q_b'])
        obb = bcast_load(kb, 'ob_at', W['o_b'][0:1, :], 1024, W['o_b'])
        esink = bcast_load(kb, 'esink', W['sinks'][0:1, :], 16, W['sinks'])
        kb.op('act', lambda e: e.activation(out=esink[:], in_=esink[:], func=AF.Exp), [esink], [esink])
        invf = bcast_load(kb, 'invf', W['c_invf'][0:1, :], 8, W['c_invf'])
        posi = kb.sb('posi', [128, 32], I32)
        kb.dma('sp', posi[:], pos_d[:, :], reads=[pos_d], writes=[posi])
        posf = kb.sb('posfa', [128, 32], F32)
        kb.op('dve', lambda e: e.tensor_copy(out=posf[:], in_=posi[:]), [posi], [posf])
        yy = kb.sb('yy', [128, 32, 8], F32)
        nn_ = kb.sb('nn_', [128, 32, 8], F32)
        sinT = kb.sb('sinT', [128, 32, 8], F32)
        cosT = kb.sb('cosT', [128, 32, 8], F32)
        kb.op('dve', lambda e: e.tensor_tensor(out=yy[:], in0=posf[:].unsqueeze(2).to_broadcast([128, 32, 8]), in1=invf[:].unsqueeze(1).to_broadcast([128, 32, 8]), op=ALU.mult), [posf, invf], [yy])
        for (dst, shift) in ((sinT, 0.0), (cosT, 0.25)):
            if shift != 0.0:
                kb.op('dve', lambda e, shift=shift: e.tensor_scalar(out=yy[:], in0=yy[:], scalar1=shift, scalar2=None, op0=ALU.add), [yy], [yy])
            kb.op('dve', lambda e: e.tensor_scalar(out=nn_[:], in0=yy[:], scalar1=MAGIC, scalar2=None, op0=ALU.add), [yy], [nn_])
            kb.op('dve', lambda e: e.tensor_scalar(out=nn_[:], in0=nn_[:], scalar1=-MAGIC, scalar2=None, op0=ALU.add), [nn_], [nn_])
            kb.op('dve', lambda e: e.tensor_tensor(out=nn_[:], in0=yy[:], in1=nn_[:], op=ALU.subtract), [yy, nn_], [nn_])
            kb.op('act', lambda e, dst=dst: e.activation(out=dst[:], in_=nn_[:], func=AF.Sin, scale=2.0 * np.pi * (1.0 - 1e-6)), [nn_], [dst])
        xt = [kb.sb(f'xta{i}', [128, 1024], F32) for i in range(3)]
        kvl = [kb.sb(f'kvl{i}', [128, 256], F32) for i in range(2)]
        hb = [kb.sb(f'hba{i}', [128, 1024], BF16) for i in range(2)]
        hT = [kb.sb(f'hTa{i}', [128, 8, 128], BF16) for i in range(2)]
        junk = kb.sb('junka', [128, 1024], F32)
        ssq = kb.sb('ssqa', [128, 1], F32)
        rstd = kb.sb('rstda', [128, 1], F32)
        q = kb.sb('qa', [128, 16, 64], F32)
        rq = [kb.sb(f'rq{i}', [128, 16, 8], F32) for i in range(4)]
        rk = [kb.sb(f'rk{i}', [128, 2, 8], F32) for i in range(4)]
        q16 = kb.sb('q16', [128, 1024], BF16)
        qTz = kb.sb('qTz', [128, 16, 128], BF16)
        kb.op('dve', lambda e: e.memset(qTz[:], 0.0), [], [qTz])
        kdup = kb.sb('kdup', [128, 256], BF16)
        kTd = [kb.sb(f'kTd{i}', [128, 2, 128], BF16) for i in range(3)]
        vaug = [kb.sb(f'vaug{i}', [128, 2, 65], BF16) for i in range(3)]
        for i in range(3):
            kb.op('dve', lambda e, i=i: e.memset(vaug[i][:], 1.0), [], [vaug[i]])
        eT = [kb.sb(f'eT{i}', [128, 4, 128], F32) for i in range(2)]
        pT = [kb.sb(f'pTa{i}', [128, 4, 128], BF16) for i in range(4)]
        den = kb.sb('den', [128, 16], F32)
        o16 = kb.sb('o16', [128, 16, 64], BF16)
        oT = kb.sb('oTa', [128, 8, 128], BF16)
        xo = [kb.sb(f'xoa{i}', [128, 1024], F32) for i in range(2)]
        masks = {0: C['c_gt_f'], 1: C['c_triu_f']}
        obk = kb.banks[0:4]
        rot = kb.banks[4:6]
        pcnt = [0]

        def rope(buf, view, nh, n, R):
            cb_ = cosT[:, n, :].unsqueeze(1).to_broadcast([128, nh, 8])
            sb_ = sinT[:, n, :].unsqueeze(1).to_broadcast([128, nh, 8])
            t1, t2 = view[:, :, 0:8], view[:, :, 8:16]
            ra, rb, rc, rd = R
            kb.op('dve', lambda e: e.tensor_tensor(out=ra[:, 0:nh, :], in0=t1, in1=cb_, op=ALU.mult), [buf, cosT], [ra])
            kb.op('dve', lambda e: e.tensor_tensor(out=rb[:, 0:nh, :], in0=t2, in1=sb_, op=ALU.mult), [buf, sinT], [rb], nosync=True)
            kb.op('dve', lambda e: e.tensor_tensor(out=rc[:, 0:nh, :], in0=t2, in1=cb_, op=ALU.mult), [buf, cosT], [rc], nosync=True)
            kb.op('dve', lambda e: e.tensor_tensor(out=rd[:, 0:nh, :], in0=t1, in1=sb_, op=ALU.mult), [buf, sinT], [rd], nosync=True)
            kb.op('dve', lambda e: e.tensor_tensor(out=t1, in0=ra[:, 0:nh, :], in1=rb[:, 0:nh, :], op=ALU.subtract), [ra, rb, rd], [buf])
            kb.op('dve', lambda e: e.tensor_tensor(out=t2, in0=rc[:, 0:nh, :], in1=rd[:, 0:nh, :], op=ALU.add), [rc, rd], [buf])

        def genF(n):
            r0 = n * 128
            xx, kv = xt[n % 3], kvl[n % 2]
            kb.dma('sp', xx[:], x_in[r0:r0 + 128, :], reads=[x_in], writes=[xx])
            kb.dma('sp', kv[:], kv_d[r0:r0 + 128, :], reads=[kv_d], writes=[kv])
            yield
            h = hb[n % 2]
            rmsnorm_bf(kb, xx, gbc, h, 1024, 1e-6, junk, ssq, rstd)
            yield
            transpose_to(kb, h, 1024, hT[n % 2][:], hT[n % 2], ident, ptr[0])
            yield
            kview = kv[:, 0:128].rearrange('p (g d) -> p g d', d=64)
            rope(kv, kview, 2, n, rk)
            yield
            for dup in range(2):
                kb.op('dve', lambda e, dup=dup, kview=kview: e.tensor_copy(out=kdup[:].rearrange('p (g two d) -> p g two d', two=2, d=64)[:, :, dup, :], in_=kview), [kv], [kdup])
            va = vaug[n % 3]
            kb.op('act', lambda e, va=va, kv=kv: e.copy(out=va[:, :, 0:64], in_=kv[:, 128:256].rearrange('p (g d) -> p g d', d=64)), [kv], [va])
            yield
            kt = kTd[n % 3]
            transpose_to(kb, kdup, 256, kt[:], kt, ident, ptr[0], evac='dve')
            yield

        def genB(n):
            r0 = n * 128
            xx = xt[n % 3]
            hT_ = hT[n % 2]
            qf = q[:].rearrange('p h d -> p (h d)')
            for hf in range(2):
                pq = kb.bank(rot)
                for dc in range(8):
                    kb.op('pe', lambda e, pq=pq, dc=dc, hf=hf: e.matmul(pq[:, 0:512], lhsT=hT_[:, dc, :], rhs=qw[:, dc, hf * 512:(hf + 1) * 512], start=(dc == 0), stop=(dc == 7)), [hT_, qw], [pq])
                kb.op('dve', lambda e, pq=pq, hf=hf: e.tensor_tensor(out=qf[:, hf * 512:(hf + 1) * 512], in0=pq[:, 0:512], in1=qb[:, hf * 512:(hf + 1) * 512], op=ALU.add), [pq, qb], [q])
                yield
            rope(q, q[:], 16, n, rq)
            yield
            kb.op('act', lambda e: e.activation(out=q16[:], in_=qf, func=AF.Copy, scale=0.125), [q], [q16])
            pt = ptr[1]
            for c in range(8):
                kb.op('pe', lambda e, c=c, pt=pt: e.transpose(out=pt[:, c * 128:(c + 1) * 128], in_=q16[:, c * 128:(c + 1) * 128], identity=ident[:]), [q16, ident], [pt])
            for par in range(2):
                src = pt[par * 64:(par + 1) * 64, 0:1024].rearrange('p (c t) -> p c t', t=128)
                dstv = qTz[par * 64:(par + 1) * 64, :, :].rearrange('p (c two) t -> p c two t', two=2)[:, :, par, :]
                kb.op('dve' if par == 0 else 'act', (lambda e, src=src, dstv=dstv: e.tensor_copy(out=dstv, in_=src)) if par == 0 else (lambda e, src=src, dstv=dstv: e.copy(out=dstv, in_=src)), [pt], [qTz])
            yield
            blocks = ([(kTd[(n - 1) % 3], vaug[(n - 1) % 3], 0)] if n > 0 else []) + [(kTd[n % 3], vaug[n % 3], 1)]
            def _scores(hg):
                pts = []
                for (kk, vv, mi) in blocks:
                    ps_ = kb.bank(rot)
                    for hh in range(4):
                        h = hg * 4 + hh
                        g = h // 8
                        kb.op('pe', lambda e, ps_=ps_, hh=hh, h=h, g=g, kk=kk: e.matmul(ps_[:, hh * 128:(hh + 1) * 128], lhsT=kk[:, g, :], rhs=qTz[:, h, :], start=True, stop=True), [kk, qTz], [ps_])
                    et = eT[pcnt[0] % 2]
                    p_ = pT[pcnt[0] % 4]
                    pcnt[0] += 1
                    kb.op('act', lambda e, ps_=ps_, et=et: e.activation(out=et[:].rearrange('p h q -> p (h q)'), in_=ps_[:, 0:512], func=AF.Exp), [ps_], [et])
                    kb.op('dve', lambda e, et=et, p_=p_, mi=mi: e.tensor_tensor(out=p_[:], in0=et[:], in1=masks[mi][:].unsqueeze(1).to_broadcast([128, 4, 128]), op=ALU.mult), [et, masks[mi]], [p_])
                    pts.append((p_, vv))
                return pts
            pts_next = _scores(0)
            yield
            for hg in range(4):
                pts = pts_next
                if hg + 1 < 4:
                    pts_next = _scores(hg + 1)
                for hh in range(4):
                    h = hg * 4 + hh
                    g = h // 8
                    for bi, (p_, vv) in enumerate(pts):
                        kb.op('pe', lambda e, hh=hh, g=g, p_=p_, vv=vv, bi=bi, hg=hg, nb=len(pts): e.matmul(obk[hg][:, hh * 65:(hh + 1) * 65], lhsT=p_[:, hh, :], rhs=vv[:, g, :], start=(bi == 0), stop=(bi == nb - 1)), [p_, vv], [obk[hg]])
                yield
            for hg in range(4):
                ov = obk[hg][:, 0:260].rearrange('p (h d) -> p h d', d=65)
                kb.op('dve', lambda e, ov=ov, hg=hg: e.tensor_tensor(out=den[:, hg * 4:(hg + 1) * 4], in0=ov[:, :, 64], in1=esink[:, hg * 4:(hg + 1) * 4], op=ALU.add), [obk[hg], esink], [den], nosync=(hg > 0))
            kb.op('dve', lambda e: e.reciprocal(out=den[:], in_=den[:]), [den], [den])
            yield
            for hg in range(4):
                ov = obk[hg][:, 0:260].rearrange('p (h d) -> p h d', d=65)
                kb.op('dve', lambda e, ov=ov, hg=hg: e.tensor_tensor(out=o16[:, hg * 4:(hg + 1) * 4, :], in0=ov[:, :, 0:64], in1=den[:, hg * 4:(hg + 1) * 4].unsqueeze(2).to_broadcast([128, 4, 64]), op=ALU.mult), [obk[hg], den], [o16], nosync=(hg > 0))
            yield
            o16f = o16[:].rearrange('p h d -> p (h d)')
            pt = ptr[1]
            for c in range(8):
                kb.op('pe', lambda e, c=c, pt=pt, o16f=o16f: e.transpose(out=pt[:, c * 128:(c + 1) * 128], in_=o16f[:, c * 128:(c + 1) * 128], identity=ident[:]), [o16, ident], [pt])
            kb.op('act', lambda e, pt=pt: e.copy(out=oT[:], in_=pt[:, 0:1024].rearrange('p (c t) -> p c t', t=128)), [pt], [oT])
            yield
            o = xo[n % 2]
            for hf in range(2):
                po = kb.bank(rot)
                for ic in range(8):
                    kb.op('pe', lambda e, po=po, ic=ic, hf=hf: e.matmul(po[:, 0:512], lhsT=oT[:, ic, :], rhs=ow[:, ic, hf * 512:(hf + 1) * 512], start=(ic == 0), stop=(ic == 7)), [oT, ow], [po])
                kb.op('dve', lambda e, po=po, hf=hf, o=o: e.tensor_tensor(out=o[:, hf * 512:(hf + 1) * 512], in0=po[:, 0:512], in1=obb[:, hf * 512:(hf + 1) * 512], op=ALU.add), [po, obb], [o])
                yield
            kb.op('dve', lambda e, o=o, xx=xx: e.tensor_tensor(out=o[:], in0=o[:], in1=xx[:], op=ALU.add), [o, xx], [o])
            kb.dma('sp', x_out[r0:r0 + 128, :], o[:], reads=[o], writes=[x_out])
            yield
        run_pipelined(genF, genB, NCH, ratio=3)
    kb.scope = None
    kb.barrier()
    kb.new_epoch()


WSPEC = [
    ('in_w', [1024, 6176]), ('ssm_norm', [1, 1024]), ('dt_bias', [1, 32]), ('conv_wT', [4096, 4]), ('conv_b2', [128, 32]),
    ('out_w', [2048, 1024]), ('gate_norm', [1, 2048]), ('A_log', [1, 32]), ('D', [1, 32]),
    ('kv_w', [1024, 256]), ('kv_norm', [1, 1024]), ('kv_b', [1, 256]),
    ('q_w', [1024, 1024]), ('o_w', [1024, 1024]), ('attn_norm', [1, 1024]), ('q_b', [1, 1024]), ('o_b', [1, 1024]), ('sinks', [1, 16]), ('c_invf', [1, 8]),
    ('final_norm', [1, 1024]),
]
LSPEC = [('peer_uT', [128, 128, 1024]), ('peer_v', [128, 128, 1024]), ('peer_q_w', [1024, 2048]), ('peer_skT', [2, 128, 128]), ('peer_norm', [1, 1024]),
         ('ple_gate_w', [1024, 1024]), ('ple_proj', [256, 1024]), ('ple_norm', [1, 1024])]


def build_all():
    nc, kb = new_kb()
    C = load_consts(kb)
    x_d = kb.dram('x', [S, 1024], F32, "ExternalInput")
    p0_d = kb.dram('p0', [S, 256], F32, "ExternalInput")
    p1_d = kb.dram('p1', [S, 256], F32, "ExternalInput")
    pos_d = kb.dram('pos', [128, 32], I32, "ExternalInput")
    out_d = kb.dram('out', [S, 1024], F32, "ExternalOutput")
    W = {}
    for name, shape in WSPEC:
        W[name] = kb.dram('w_' + name, shape, F32, "ExternalInput")
    WL = [{}, {}]
    for L in range(2):
        for name, shape in LSPEC:
            WL[L][name] = kb.dram(f'w{L}_' + name, shape, F32, "ExternalInput")
    xs = [kb.scratch(f'sc_x{i}', [S, 1024], F32) for i in range(6)]
    kv_d = kb.scratch('sc_kv', [S, 256], F32)
    UV = []

    lazy1 = []

    def _conv():
        UV.append(peer_convert(kb, WL[0], 0))
        u1, v1, g1 = peer_convert(kb, WL[1], 1, lazy=True)
        UV.append((u1, v1))
        lazy1.append(g1)
    phase1(kb, C, x_d, xs[0], W, after_loads=_conv)
    phase_peer(kb, C, xs[0], xs[1], WL[0], UV[0][0], UV[0][1], extra_gen=lazy1[0])
    Wp = dict(W); Wp.update(WL[0])
    phase_ple(kb, C, xs[1], xs[2], p0_d, Wp, kv_out=kv_d)
    phase_attn(kb, C, xs[2], xs[3], kv_d, pos_d, W)
    phase_peer(kb, C, xs[3], xs[4], WL[1], UV[1][0], UV[1][1])
    Wp = dict(W); Wp.update(WL[1])
    phase_ple(kb, C, xs[4], out_d, p1_d, Wp, final=True)
    kb.finish([out_d.lw])
    kb.emit()
    return nc


def host_inputs(inp):
    f = np.float32
    shared = dict(make_consts())
    shared['w_in_w'] = np.ascontiguousarray(inp['ssm_in_w'][0], f)
    shared['w_ssm_norm'] = np.ascontiguousarray(inp['ssm_norm'], f).reshape(1, 1024)
    shared['w_dt_bias'] = np.ascontiguousarray(inp['ssm_dt_bias'], f).reshape(1, 32)
    shared['w_conv_wT'] = np.ascontiguousarray(np.asarray(inp['ssm_conv_w'][0], f).T)
    shared['w_conv_b2'] = np.ascontiguousarray(np.asarray(inp['ssm_conv_b'][0], f).reshape(32, 128).T)
    shared['w_out_w'] = np.ascontiguousarray(inp['ssm_out_w'][0], f)
    shared['w_gate_norm'] = np.ascontiguousarray(inp['ssm_gate_norm'], f).reshape(1, 2048)
    shared['w_A_log'] = np.ascontiguousarray(inp['ssm_A_log'], f).reshape(1, 32)
    shared['w_D'] = np.ascontiguousarray(inp['ssm_D'], f).reshape(1, 32)
    shared['w_kv_w'] = np.ascontiguousarray(inp['kv_w'], f)
    shared['w_kv_norm'] = np.ascontiguousarray(inp['kv_norm'], f).reshape(1, 1024)
    shared['w_kv_b'] = np.ascontiguousarray(inp['kv_b'], f).reshape(1, 256)
    shared['w_q_w'] = np.ascontiguousarray(inp['q_w'][0], f)
    shared['w_o_w'] = np.ascontiguousarray(inp['o_w'][0], f)
    shared['w_attn_norm'] = np.ascontiguousarray(inp['attn_norm'], f).reshape(1, 1024)
    shared['w_q_b'] = np.ascontiguousarray(inp['q_b'], f).reshape(1, 1024)
    shared['w_o_b'] = np.ascontiguousarray(inp['o_b'], f).reshape(1, 1024)
    shared['w_sinks'] = np.ascontiguousarray(inp['sinks'], f).reshape(1, 16)
    shared['w_c_invf'] = (np.power(500000.0, -np.arange(0, 16, 2, dtype=np.float32) / 16) / (2 * np.pi)).astype(f)[None]
    shared['w_final_norm'] = np.ascontiguousarray(inp['final_norm'], f).reshape(1, 1024)
    for L in range(2):
        u = np.asarray(inp['peer_u'][L], f)
        shared[f'w{L}_peer_uT'] = np.ascontiguousarray(u.reshape(128, 128, 8, 128).transpose(0, 3, 2, 1)).reshape(128, 128, 1024)
        shared[f'w{L}_peer_v'] = np.ascontiguousarray(np.asarray(inp['peer_v'][L], f).reshape(128, 128, 1024))
        shared[f'w{L}_peer_q_w'] = np.ascontiguousarray(inp['peer_q_w'][L], f)
        shared[f'w{L}_peer_skT'] = np.ascontiguousarray(np.asarray(inp['peer_sub_keys'][L], f).transpose(0, 2, 1))
        shared[f'w{L}_peer_norm'] = np.ascontiguousarray(inp['peer_norm'][L], f).reshape(1, 1024)
        shared[f'w{L}_ple_gate_w'] = np.ascontiguousarray(inp['ple_gate_w'][L], f)
        shared[f'w{L}_ple_proj'] = np.ascontiguousarray(inp['ple_proj'][L], f)
        shared[f'w{L}_ple_norm'] = np.ascontiguousarray(inp['ple_norm'][L], f).reshape(1, 1024)
    maps = []
    for b in range(8):
        m = dict(shared)
        m['x'] = np.ascontiguousarray(inp['x'][b], f)
        m['p0'] = np.ascontiguousarray(inp['p'][0, b], f)
        m['p1'] = np.ascontiguousarray(inp['p'][1, b], f)
        m['pos'] = np.ascontiguousarray(np.asarray(inp['positions'][b], np.int32).reshape(32, 128).T)
        maps.append(m)
    return maps


_NC = None


def kernel(**inputs):
    global _NC
    inp = {k: np.asarray(v) for k, v in inputs.items()}
    if _NC is None:
        _NC = build_all()
    maps = host_inputs(inp)
    res = run_bass_kernel_spmd(_NC, maps, core_ids=list(range(8)))
    out = np.stack([np.asarray(r['out'], np.float32) for r in res.results], axis=0)
    return out
```
